# Optimizing a Trainium2 kernel written in Bass

```python
import math
import jax, jax.numpy as jnp
from jax import lax
import numpy as np

D_MODEL = 1024
BATCH = 8
SEQ = 4096
DEPTH = 4

GRID_W = 64
CTX_LEN = 256
N_EVEN = (DEPTH + 1) // 2
N_ODD = DEPTH // 2

HEAD_DIM = 64
RET_HEADS = 8
ATT_HEADS = 8
ATT_KV_HEADS = 2
ATT_GROUP = ATT_HEADS // ATT_KV_HEADS
RET_WIDTH = RET_HEADS * HEAD_DIM
ATT_WIDTH = ATT_HEADS * HEAD_DIM
ATT_KV_WIDTH = ATT_KV_HEADS * HEAD_DIM
MIX_WIDTH = RET_WIDTH + ATT_WIDTH
IN_SPLITS = (RET_WIDTH, 2 * RET_WIDTH, 3 * RET_WIDTH, 4 * RET_WIDTH,
             4 * RET_WIDTH + ATT_WIDTH, 4 * RET_WIDTH + ATT_WIDTH + ATT_KV_WIDTH)
IN_WIDTH = 4 * RET_WIDTH + ATT_WIDTH + 2 * ATT_KV_WIDTH
CHUNK = 128
Q_BLOCK = 128
ROPE_THETA = 10000.0
ROPE_AXIS_DIM = HEAD_DIM // 2

S5_GROUP = 16
S5_GROUPS = D_MODEL // S5_GROUP
S5_STATE = 64
S5_DT_MIN = 0.001
S5_DT_MAX = 0.1

N_EXPERTS = 16
CAPACITY_FACTOR = 2
D_FF_EXPERT = 2 * D_MODEL
EPS = 1e-6

kernel_name = "hybrid_retention_gqa_s5_ecmoe_diffusion_trunk"

F32 = jnp.float32


def rms_norm(x, g):
    x32 = x.astype(F32)
    y = x32 * lax.rsqrt(jnp.mean(x32 * x32, axis=-1, keepdims=True) + EPS)
    return y.astype(x.dtype) * g


def modulation(cond, w, b):
    m = jax.nn.silu(cond) @ w + b
    return jnp.split(m[..., None, :], 6, axis=-1)


def axial_rope_tables(n_rows):
    row = jnp.repeat(jnp.arange(n_rows, dtype=F32), GRID_W)
    col = jnp.tile(jnp.arange(GRID_W, dtype=F32), n_rows)
    inv = ROPE_THETA ** (-jnp.arange(0, ROPE_AXIS_DIM, 2, dtype=F32) / ROPE_AXIS_DIM)
    ang_r = row[:, None] * inv[None, :]
    ang_c = col[:, None] * inv[None, :]
    return (jnp.cos(ang_r)[:, None, :], jnp.sin(ang_r)[:, None, :],
            jnp.cos(ang_c)[:, None, :], jnp.sin(ang_c)[:, None, :])


def _rope_1d(x, cos, sin):
    half = x.shape[-1] // 2
    x1, x2 = x[..., :half], x[..., half:]
    return jnp.concatenate([x1 * cos - x2 * sin, x1 * sin + x2 * cos], axis=-1)


def apply_axial_rope(x, rope):
    cr, sr, cc, sc = rope
    xr = _rope_1d(x[..., :ROPE_AXIS_DIM].astype(F32), cr, sr)
    xc = _rope_1d(x[..., ROPE_AXIS_DIM:].astype(F32), cc, sc)
    return jnp.concatenate([xr, xc], axis=-1).astype(x.dtype)


def split_heads(t, n_heads):
    return t.reshape(t.shape[0], t.shape[1], n_heads, HEAD_DIM)


def retention_chunkwise(q, k, v, log_gamma, state0, strict):
    b, h, n, d = q.shape
    nc = n // CHUNK
    pos = jnp.arange(CHUNK, dtype=F32)
    diff = pos[:, None] - pos[None, :]
    mask = (diff > 0) if strict else (diff >= 0)
    lg = log_gamma[:, None, None]
    decay_intra = jnp.where(mask, jnp.exp(jnp.where(mask, diff, 0.0) * lg), 0.0)
    decay_q = jnp.exp((pos + 1.0)[None, :] * log_gamma[:, None])[..., None]
    decay_k = jnp.exp((CHUNK - 1.0 - pos)[None, :] * log_gamma[:, None])[..., None]
    decay_chunk = jnp.exp(CHUNK * log_gamma)[:, None, None]

    def to_chunks(t):
        return jnp.moveaxis(t.reshape(b, h, nc, CHUNK, d), 2, 0)

    def step(state, qkv):
        qc, kc, vc = qkv
        scores = jnp.einsum('bhid,bhjd->bhij', qc, kc) * decay_intra
        out = (jnp.einsum('bhij,bhjd->bhid', scores, vc)
               + jnp.einsum('bhid,bhde->bhie', qc * decay_q, state))
        state = decay_chunk * state + jnp.einsum('bhjd,bhje->bhde', kc * decay_k, vc)
        return state, out

    state, out = lax.scan(step, state0, (to_chunks(q), to_chunks(k), to_chunks(v)))
    return jnp.moveaxis(out, 0, 2).reshape(b, h, n, d), state


def bidir_retention(q, k, v, lg_f, lg_b, s_f, s_b):
    o_f, s_f_new = retention_chunkwise(q, k, v, lg_f, s_f, False)
    flip = lambda t: jnp.flip(t, axis=2)
    o_b, s_b_new = retention_chunkwise(flip(q), flip(k), flip(v), lg_b, s_b, True)
    return o_f + flip(o_b), s_f_new, s_b_new


def retention_output(o, gate_proj, gn_g):
    b, h, n, d = o.shape
    mu = jnp.mean(o, axis=-1, keepdims=True)
    var = jnp.mean(jnp.square(o - mu), axis=-1, keepdims=True)
    o = (o - mu) * lax.rsqrt(var + EPS)
    o = jnp.swapaxes(o, 1, 2).reshape(b, n, h * d).astype(gate_proj.dtype) * gn_g
    return jax.nn.silu(gate_proj) * o


def attend(q, k, v):
    s = jnp.einsum('bkgqd,bkmd->bkgqm', q, k).astype(F32) * (HEAD_DIM ** -0.5)
    p = jax.nn.softmax(s, axis=-1).astype(v.dtype)
    return jnp.einsum('bkgqm,bkmd->bkgqd', p, v)


def even_mixer(h_lat, h_ctx, w_in, w_out, ret_log_rate, ret_gn_g, qk_norm_g, rope, need_ctx):
    def project(h, pos):
        rq, rk, rv, rg, aq, ak, av = jnp.split(h @ w_in, IN_SPLITS, axis=-1)
        rq = split_heads(rq, RET_HEADS)
        rk = split_heads(rk, RET_HEADS) * (HEAD_DIM ** -0.5)
        aq = rms_norm(split_heads(aq, ATT_HEADS), qk_norm_g[0])
        ak = rms_norm(split_heads(ak, ATT_KV_HEADS), qk_norm_g[1])
        if pos is not None:
            rq, rk, aq, ak = [apply_axial_rope(t, pos) for t in (rq, rk, aq, ak)]
        hf = lambda t: jnp.swapaxes(t, 1, 2)
        return (hf(rq).astype(F32), hf(rk).astype(F32), hf(split_heads(rv, RET_HEADS)).astype(F32), rg,
                hf(aq), hf(ak), hf(split_heads(av, ATT_KV_HEADS)))

    b, n_lat, n_ctx = h_lat.shape[0], h_lat.shape[1], h_ctx.shape[1]
    lrq, lrk, lrv, lrg, laq, lak, lav = project(h_lat, rope)
    crq, crk, crv, crg, caq, cak, cav = project(h_ctx, None)

    lg_f = -jnp.exp(ret_log_rate[0].astype(F32))
    lg_b = -jnp.exp(ret_log_rate[1].astype(F32))
    zero = jnp.zeros((b, RET_HEADS, HEAD_DIM, HEAD_DIM), F32)
    o_ctx, s_f, s_b = bidir_retention(crq, crk, crv, lg_f, lg_b, zero, zero)
    o_lat, _, _ = bidir_retention(lrq, lrk, lrv, lg_f, lg_b, s_f, s_b)
    ret_lat = retention_output(o_lat, lrg, ret_gn_g)

    k_all = jnp.concatenate([cak, lak], axis=2)
    v_all = jnp.concatenate([cav, lav], axis=2)
    n_blocks = n_lat // Q_BLOCK
    q_lat = laq.reshape(b, ATT_KV_HEADS, ATT_GROUP, n_blocks, Q_BLOCK, HEAD_DIM)
    q_blocks = jnp.moveaxis(q_lat, 3, 0)
    o_blocks = lax.map(lambda qb: attend(qb, k_all, v_all), q_blocks)
    att_lat = o_blocks.transpose(1, 0, 4, 2, 3, 5).reshape(b, n_lat, ATT_WIDTH)

    y_lat = jnp.concatenate([ret_lat, att_lat], axis=-1) @ w_out
    if not need_ctx:
        return y_lat, None
    ret_ctx = retention_output(o_ctx, crg, ret_gn_g)
    q_ctx = caq.reshape(b, ATT_KV_HEADS, ATT_GROUP, n_ctx, HEAD_DIM)
    att_ctx = attend(q_ctx, cak, cav).transpose(0, 3, 1, 2, 4).reshape(b, n_ctx, ATT_WIDTH)
    y_ctx = jnp.concatenate([ret_ctx, att_ctx], axis=-1) @ w_out
    return y_lat, y_ctx


def _cmul(ar, ai, br, bi):
    return ar * br - ai * bi, ar * bi + ai * br


def _s5_combine(e1, e2):
    a1r, a1i, b1r, b1i = e1
    a2r, a2i, b2r, b2i = e2
    ar, ai = _cmul(a2r, a2i, a1r, a1i)
    br, bi = _cmul(a2r, a2i, b1r, b1i)
    return ar, ai, br + b2r, bi + b2i


def s5_discretize(a_re, a_im, log_dt, b_re, b_im):
    lam_re = jnp.minimum(a_re, -1e-4)
    lam_im = a_im
    dt = jnp.exp(log_dt)[:, None]
    mag = jnp.exp(lam_re * dt)
    bar_re = mag * jnp.cos(lam_im * dt)
    bar_im = mag * jnp.sin(lam_im * dt)
    den = lam_re * lam_re + lam_im * lam_im
    nr, ni = bar_re - 1.0, bar_im
    k_re = (nr * lam_re + ni * lam_im) / den
    k_im = (ni * lam_re - nr * lam_im) / den
    bb_re, bb_im = _cmul(k_re[..., None], k_im[..., None], b_re, b_im)
    return bar_re, bar_im, bb_re, bb_im


def s5_scan(u, bar_re, bar_im, bb_re, bb_im, cc_re, cc_im, x0):
    b, n, _ = u.shape
    nc = n // CHUNK
    u_chunks = jnp.moveaxis(u.reshape(b, nc, CHUNK, S5_GROUPS, S5_GROUP), 1, 0)

    def step(carry, uc):
        xr0, xi0 = carry
        br = jnp.einsum('bcgk,gpk->bcgp', uc, bb_re)
        bi = jnp.einsum('bcgk,gpk->bcgp', uc, bb_im)
        ar = jnp.broadcast_to(bar_re, br.shape)
        ai = jnp.broadcast_to(bar_im, br.shape)
        pr, pi, sr, si = lax.associative_scan(_s5_combine, (ar, ai, br, bi), axis=1)
        xr = pr * xr0[:, None] - pi * xi0[:, None] + sr
        xi = pr * xi0[:, None] + pi * xr0[:, None] + si
        y = jnp.einsum('bcgp,gkp->bcgk', xr, cc_re) - jnp.einsum('bcgp,gkp->bcgk', xi, cc_im)
        return (xr[:, -1], xi[:, -1]), y.reshape(b, CHUNK, D_MODEL)

    state, y = lax.scan(step, x0, u_chunks)
    return jnp.moveaxis(y, 0, 1).reshape(b, n, D_MODEL), state


def s5_mixer(h_lat, h_ctx, a_re, a_im, log_dt, b_re, b_im, c_re, c_im, d_skip, glu_w, glu_b, need_ctx):
    a_re, a_im, log_dt = a_re.astype(F32), a_im.astype(F32), log_dt.astype(F32)
    b_re, b_im, c_re, c_im = b_re.astype(F32), b_im.astype(F32), c_re.astype(F32), c_im.astype(F32)
    d32 = d_skip.astype(F32)
    disc_f = s5_discretize(a_re[0], a_im[0], log_dt[0], b_re, b_im)
    disc_b = s5_discretize(a_re[1], a_im[1], log_dt[1], b_re, b_im)

    def bidir(u, init_f, init_b):
        y_f, s_f = s5_scan(u, *disc_f, c_re[0], c_im[0], init_f)
        y_b, s_b = s5_scan(jnp.flip(u, axis=1), *disc_b, c_re[1], c_im[1], init_b)
        return y_f + jnp.flip(y_b, axis=1) + d32 * u, s_f, s_b

    def glu_out(y, dtype):
        z = jax.nn.gelu(y).astype(dtype)
        a, g = jnp.split(z @ glu_w + glu_b, 2, axis=-1)
        return a * jax.nn.sigmoid(g)

    b = h_lat.shape[0]
    zero = (jnp.zeros((b, S5_GROUPS, S5_STATE), F32), jnp.zeros((b, S5_GROUPS, S5_STATE), F32))
    y_ctx, s_f, s_b = bidir(h_ctx.astype(F32), zero, zero)
    y_lat, _, _ = bidir(h_lat.astype(F32), s_f, s_b)
    out_lat = glu_out(y_lat, h_lat.dtype)
    if not need_ctx:
        return out_lat, None
    return out_lat, glu_out(y_ctx, h_ctx.dtype)


def ec_moe(h, w_router, w1, w3, w2):
    b, n, _ = h.shape
    cap = (CAPACITY_FACTOR * n) // N_EXPERTS
    aff = jax.nn.softmax((h @ w_router).astype(F32), axis=-1)
    gate, idx = lax.top_k(jnp.swapaxes(aff, 1, 2), cap)
    bidx = jnp.arange(b)[:, None, None]
    xs = h[bidx, idx]
    hid = jax.nn.silu(jnp.einsum('becd,edf->becf', xs, w1)) * jnp.einsum('becd,edf->becf', xs, w3)
    ys = jnp.einsum('becf,efd->becd', hid, w2) * gate[..., None].astype(h.dtype)
    return jnp.zeros_like(h).at[bidx, idx].add(ys)


def setup_inputs(seed: int = 0) -> dict:
    key = jax.random.key(seed)
    ks = jax.random.split(key, 32)
    nrm = lambda k, shape, s: jax.random.normal(k, shape, F32) * s
    s5_shape = (N_ODD, 2, S5_GROUPS, S5_STATE)
    base_rate = -jnp.log1p(-(2.0 ** (-5.0 - jnp.arange(RET_HEADS, dtype=F32))))
    n_idx = jnp.arange(S5_STATE, dtype=F32)
    return {
        "x": nrm(ks[0], (BATCH, SEQ, D_MODEL), 1.0),
        "c": nrm(ks[1], (BATCH, D_MODEL), 1.0),
        "ctx": nrm(ks[2], (BATCH, CTX_LEN, D_MODEL), 1.0),
        "c_ctx": nrm(ks[3], (D_MODEL,), 1.0),
        "mod_w": nrm(ks[4], (DEPTH, D_MODEL, 6 * D_MODEL), 0.5 * D_MODEL ** -0.5),
        "mod_b": nrm(ks[5], (DEPTH, 6 * D_MODEL), 0.02),
        "norm_g": 1.0 + nrm(ks[6], (DEPTH, 2, D_MODEL), 0.02),
        "mix_in_w": nrm(ks[7], (N_EVEN, D_MODEL, IN_WIDTH), D_MODEL ** -0.5),
        "mix_out_w": nrm(ks[8], (N_EVEN, MIX_WIDTH, D_MODEL), MIX_WIDTH ** -0.5),
        "ret_log_rate": jnp.log(base_rate) + nrm(ks[9], (N_EVEN, 2, RET_HEADS), 0.05),
        "ret_gn_g": 1.0 + nrm(ks[10], (N_EVEN, RET_WIDTH), 0.02),
        "qk_norm_g": 1.0 + nrm(ks[11], (N_EVEN, 2, HEAD_DIM), 0.02),
        "s5_a_re": -0.5 + nrm(ks[12], s5_shape, 0.01),
        "s5_a_im": math.pi * n_idx + nrm(ks[13], s5_shape, 0.01),
        "s5_log_dt": jax.random.uniform(ks[14], (N_ODD, 2, S5_GROUPS), F32,
                                        math.log(S5_DT_MIN), math.log(S5_DT_MAX)),
        "s5_b_re": nrm(ks[15], (N_ODD, S5_GROUPS, S5_STATE, S5_GROUP), (2 * S5_GROUP) ** -0.5),
        "s5_b_im": nrm(ks[16], (N_ODD, S5_GROUPS, S5_STATE, S5_GROUP), (2 * S5_GROUP) ** -0.5),
        "s5_c_re": nrm(ks[17], (N_ODD, 2, S5_GROUPS, S5_GROUP, S5_STATE), S5_STATE ** -0.5),
        "s5_c_im": nrm(ks[18], (N_ODD, 2, S5_GROUPS, S5_GROUP, S5_STATE), S5_STATE ** -0.5),
        "s5_d": nrm(ks[19], (N_ODD, D_MODEL), 1.0),
        "s5_glu_w": nrm(ks[20], (N_ODD, D_MODEL, 2 * D_MODEL), D_MODEL ** -0.5),
        "s5_glu_b": nrm(ks[21], (N_ODD, 2 * D_MODEL), 0.02),
        "moe_router_w": nrm(ks[22], (DEPTH, D_MODEL, N_EXPERTS), D_MODEL ** -0.5),
        "moe_w1": nrm(ks[23], (DEPTH, N_EXPERTS, D_MODEL, D_FF_EXPERT), D_MODEL ** -0.5),
        "moe_w3": nrm(ks[24], (DEPTH, N_EXPERTS, D_MODEL, D_FF_EXPERT), D_MODEL ** -0.5),
        "moe_w2": nrm(ks[25], (DEPTH, N_EXPERTS, D_FF_EXPERT, D_MODEL), D_FF_EXPERT ** -0.5),
        "final_norm_g": 1.0 + nrm(ks[26], (D_MODEL,), 0.02),
    }


def reference(x, c, ctx, c_ctx, mod_w, mod_b, norm_g, mix_in_w, mix_out_w, ret_log_rate, ret_gn_g,
              qk_norm_g, s5_a_re, s5_a_im, s5_log_dt, s5_b_re, s5_b_im, s5_c_re, s5_c_im, s5_d,
              s5_glu_w, s5_glu_b, moe_router_w, moe_w1, moe_w3, moe_w2, final_norm_g):
    n_rows = x.shape[1] // GRID_W
    rope = axial_rope_tables(n_rows)
    x_lat, x_ctx = x, ctx
    for layer in range(DEPTH):
        last = layer == DEPTH - 1
        sh1, sc1, g1, sh2, sc2, g2 = modulation(c, mod_w[layer], mod_b[layer])
        csh1, csc1, cg1, csh2, csc2, cg2 = modulation(c_ctx, mod_w[layer], mod_b[layer])
        h_lat = rms_norm(x_lat, norm_g[layer, 0]) * (1.0 + sc1) + sh1
        h_ctx = rms_norm(x_ctx, norm_g[layer, 0]) * (1.0 + csc1) + csh1
        i = layer // 2
        if layer % 2 == 0:
            d_lat, d_ctx = even_mixer(h_lat, h_ctx, mix_in_w[i], mix_out_w[i], ret_log_rate[i],
                                      ret_gn_g[i], qk_norm_g[i], rope, not last)
        else:
            d_lat, d_ctx = s5_mixer(h_lat, h_ctx, s5_a_re[i], s5_a_im[i], s5_log_dt[i], s5_b_re[i],
                                    s5_b_im[i], s5_c_re[i], s5_c_im[i], s5_d[i], s5_glu_w[i],
                                    s5_glu_b[i], not last)
        x_lat = x_lat + g1 * d_lat
        f_lat = rms_norm(x_lat, norm_g[layer, 1]) * (1.0 + sc2) + sh2
        x_lat = x_lat + g2 * ec_moe(f_lat, moe_router_w[layer], moe_w1[layer], moe_w3[layer], moe_w2[layer])
        if not last:
            x_ctx = x_ctx + cg1 * d_ctx
            f_ctx = rms_norm(x_ctx, norm_g[layer, 1]) * (1.0 + csc2) + csh2
            x_ctx = x_ctx + cg2 * ec_moe(f_ctx, moe_router_w[layer], moe_w1[layer], moe_w3[layer], moe_w2[layer])
    return rms_norm(x_lat, final_norm_g)
```

```python
import contextlib
import math
import numpy as np
import concourse.bass as bass
import concourse.mybir as mybir
from concourse.bass_utils import run_bass_kernel_spmd

F32 = mybir.dt.float32
BF16 = mybir.dt.bfloat16
I32 = mybir.dt.int32
U32 = mybir.dt.uint32
ALU = mybir.AluOpType
AF = mybir.ActivationFunctionType
AX = mybir.AxisListType

SEM_LIMIT = 30000
NSLOT = 10

D = 1024
NCTX = 256
NLAT = 4096
T = NCTX + NLAT
NT = T // 128
EPS = 1e-6
TWO_PI = 2.0 * math.pi


class KB:
    ENG = ("pe", "act", "dve", "pool", "sp")

    def __init__(self, nc):
        self.nc = nc
        self.stack = contextlib.ExitStack()
        self.q = {e: [] for e in self.ENG}
        self.nsem = 0
        self.cur = {}
        for e in ("pe", "act", "dve", "pool"):
            self.cur[e] = [self._newsem(e), 0]
        self.slots = {}
        self.slot_i = {}
        for e in ("sp", "pool"):
            self.slots[e] = [[self._newsem("d" + e), 0] for _ in range(NSLOT)]
            self.slot_i[e] = 0
        self.known = {e: {} for e in self.ENG}
        self.last_w = {}
        self.reads = {}
        self.pending = {e: [] for e in self.ENG}
        self.ninst = 0

    def _newsem(self, tag):
        self.nsem += 1
        return self.stack.enter_context(self.nc.semaphore(f"s_{tag}_{self.nsem}"))

    def sbuf(self, name, shape, dtype):
        return self.stack.enter_context(self.nc.sbuf_tensor(name, list(shape), dtype))

    def psum(self, name, shape, dtype):
        return self.stack.enter_context(self.nc.psum_tensor(name, list(shape), dtype))

    def all_tokens(self):
        toks = []
        for e in ("pe", "act", "dve", "pool"):
            c = self.cur[e]
            if c[1] > 0:
                toks.append((c[0], c[1]))
        for e in self.slots:
            for s in self.slots[e]:
                if s[1] > 0:
                    toks.append((s[0], s[1]))
        return toks

    def barrier(self):
        toks = self.all_tokens()
        for e in self.ENG:
            self.pending[e] = list(toks)
        self.last_w = {}
        self.reads = {}

    def _deps(self, eng, R, W):
        need = {}

        def add(tok):
            if tok is None:
                return
            sem, val = tok
            k = id(sem)
            if k not in need or need[k][1] < val:
                need[k] = (sem, val)
        for tok in self.pending[eng]:
            add(tok)
        self.pending[eng] = []
        for r in R:
            add(self.last_w.get(r))
        for w in W:
            add(self.last_w.get(w))
            for t in self.reads.get(w, {}).values():
                add(t)
        out = []
        kn = self.known[eng]
        for k, (sem, val) in need.items():
            if kn.get(k, 0) >= val:
                continue
            kn[k] = val
            out.append((sem, val))
        return out

    def _commit(self, tok, R, W, tag):
        for w in W:
            self.last_w[w] = tok
            self.reads[w] = {}
        for r in R:
            if r in W:
                continue
            self.reads.setdefault(r, {})[tag] = tok

    def op(self, eng, fn, R=(), W=()):
        R = tuple(R)
        W = tuple(W)
        waits = self._deps(eng, R, W)
        c = self.cur[eng]
        if c[1] >= SEM_LIMIT:
            c[0] = self._newsem(eng)
            c[1] = 0
        c[1] += 1
        sem, val = c[0], c[1]
        if eng == "pe":
            self.known[eng][id(sem)] = val
        self.q[eng].append((waits, fn, sem, 1))
        self._commit((sem, val), R, W, eng)
        self.ninst += 1

    def dma(self, qeng, fn, R=(), W=()):
        R = tuple(R)
        W = tuple(W)
        waits = self._deps(qeng, R, W)
        i = self.slot_i[qeng]
        self.slot_i[qeng] = (i + 1) % NSLOT
        s = self.slots[qeng][i]
        kn = self.known[qeng]
        if s[1] > 0 and kn.get(id(s[0]), 0) < s[1]:
            waits.append((s[0], s[1]))
            kn[id(s[0])] = s[1]
        s[1] += 16
        self.q[qeng].append((waits, fn, s[0], 16))
        self._commit((s[0], s[1]), R, W, ("dma", qeng, i))
        self.ninst += 1

    def emit(self):
        nc = self.nc
        finals = self.all_tokens()
        with nc.Block() as block:
            def run(engname):
                def body(e):
                    for waits, fn, sem, inc in self.q[engname]:
                        for (ws, wv) in waits:
                            e.wait_ge(ws, wv)
                        fn(e).then_inc(sem, inc)
                    if engname == "sp":
                        for (ws, wv) in finals:
                            e.wait_ge(ws, wv)
                return body
            block.sync(run("sp"))
            block.tensor(run("pe"))
            block.scalar(run("act"))
            block.vector(run("dve"))
            block.gpsimd(run("pool"))


class Arena:
    def __init__(self, kb, words):
        self.kb = kb
        self.words = words
        self.t = kb.sbuf("arena", [128, words], F32)
        self.off = 0
        self.phase = 0

    def reset(self):
        self.kb.barrier()
        self.off = 0
        self.phase += 1

    def alloc(self, name, cols, dtype=F32, parts=128):
        w = cols if dtype in (F32, I32, U32) else (cols + 1) // 2
        w = (w + 7) // 8 * 8
        assert self.off + w <= self.words, f"arena overflow at {name}: {self.off}+{w}>{self.words}"
        ap = self.t[0:parts, self.off:self.off + w]
        self.off += w
        if dtype != F32:
            ap = ap.bitcast(dtype)
        ap = ap[:, 0:cols]
        return ap, f"p{self.phase}.{name}"


class Gen:
    def __init__(self, cfg):
        self.cfg = cfg
        self.nc = bass.Bass("TRN2", target_bir_lowering=False)
        self.kb = KB(self.nc)
        self.dbg = {}

    def mm(self, out, lhsT, rhs, start, stop, R, W):
        self.kb.op("pe", lambda e: e.matmul(out, lhsT=lhsT, rhs=rhs, start=start, stop=stop), R=R, W=W)

    def tr(self, out, in_, ident, R, W):
        self.kb.op("pe", lambda e: e.transpose(out=out, in_=in_, identity=ident), R=R, W=W)

    def act(self, out, in_, func, R, W, **kw):
        self.kb.op("act", lambda e: e.activation(out=out, in_=in_, func=func, **kw), R=R, W=W)

    def tt(self, out, a, b, op, R, W, eng="dve"):
        self.kb.op(eng, lambda e: e.tensor_tensor(out=out, in0=a, in1=b, op=op), R=R, W=W)

    def ts(self, out, in0, s1, s2, op0, op1, R, W, eng="dve", **kw):
        if s2 is None:
            self.kb.op(eng, lambda e: e.tensor_scalar(out=out, in0=in0, scalar1=s1, scalar2=None, op0=op0, **kw), R=R, W=W)
        else:
            self.kb.op(eng, lambda e: e.tensor_scalar(out=out, in0=in0, scalar1=s1, scalar2=s2, op0=op0, op1=op1, **kw), R=R, W=W)

    def stt(self, out, in0, scalar, in1, op0, op1, R, W):
        self.kb.op("dve", lambda e: e.scalar_tensor_tensor(out=out, in0=in0, scalar=scalar, in1=in1, op0=op0, op1=op1), R=R, W=W)

    def cp(self, out, in_, R, W, eng="dve"):
        if eng == "act":
            self.kb.op("act", lambda e: e.copy(out=out, in_=in_), R=R, W=W)
        else:
            self.kb.op(eng, lambda e: e.tensor_copy(out=out, in_=in_), R=R, W=W)

    def memset(self, out, val, W, eng="dve"):
        self.kb.op(eng, lambda e: e.memset(out, val), W=W)

    def ld(self, out, in_, R, W, q="sp"):
        self.kb.dma(q, lambda e: e.dma_start(out=out, in_=in_), R=R, W=W)

    def st(self, out, in_, R, W, q="pool"):
        self.kb.dma(q, lambda e: e.dma_start(out=out, in_=in_), R=R, W=W)

    def red(self, out, in_, op, R, W, axis=AX.X):
        self.kb.op("dve", lambda e: e.tensor_reduce(out=out, in_=in_, axis=axis, op=op), R=R, W=W)

    def recip(self, out, in_, R, W):
        self.kb.op("dve", lambda e: e.reciprocal(out=out, in_=in_), R=R, W=W)

    def rsqrt_small(self, out, in_, scale, tmpkey, R, W):
        self.ts(out, in_, scale, EPS, ALU.mult, ALU.add, R=R, W=W)
        self.act(out, out, AF.Sqrt, R=W, W=W)
        self.recip(out, out, R=W, W=W)

    def dram_in(self, name, shape, dtype=F32):
        return self.nc.dram_tensor(name, list(shape), dtype, kind="ExternalInput").ap()

    def dram_out(self, name, shape, dtype=F32):
        return self.nc.dram_tensor(name, list(shape), dtype, kind="ExternalOutput").ap()

    def dram_tmp(self, name, shape, dtype=F32):
        if self.cfg.get("dbg_" + name):
            ap = self.nc.dram_tensor(name, list(shape), dtype, kind="ExternalOutput").ap()
            self.dbg[name] = ap
            return ap
        return self.nc.dram_tensor(name, list(shape), dtype, kind="Internal").ap()


class Prog(Gen):
    def __init__(self, cfg):
        super().__init__(cfg)
        g = self
        layers = cfg["layers"]
        self.layers = layers
        nl = len(layers)
        ev = [l for l in layers if l % 2 == 0]
        od = [l for l in layers if l % 2 == 1]
        self.ev_idx = {l: i for i, l in enumerate(ev)}
        self.od_idx = {l: i for i, l in enumerate(od)}
        ne, no = max(len(ev), 1), max(len(od), 1)
        self.xin = g.dram_in("xin", [T, D])
        self.cin = g.dram_in("cin", [128, 16])
        self.mod_w = g.dram_in("mod_w", [nl, D, 6 * D])
        self.mod_b = g.dram_in("mod_b", [nl, 6 * D])
        self.norm_g = g.dram_in("norm_g", [nl, 2, D])
        self.mix_in_w = g.dram_in("mix_in_w", [ne, D, 2816])
        self.mix_out_w = g.dram_in("mix_out_w", [ne, D, D])
        self.ret_log_rate = g.dram_in("ret_log_rate", [ne, 16])
        self.ret_gn_g = g.dram_in("ret_gn_g", [ne, 512])
        self.qk_norm_g = g.dram_in("qk_norm_g", [ne, 128])
        self.s5p = g.dram_in("s5p", [no, 2, 3, 128, 32])
        self.s5_b_re = g.dram_in("s5_b_re", [no, 64, 64, 16])
        self.s5_b_im = g.dram_in("s5_b_im", [no, 64, 64, 16])
        self.s5_c_re = g.dram_in("s5_c_re", [no, 2, 64, 64, 16])
        self.s5_c_im = g.dram_in("s5_c_im", [no, 2, 64, 64, 16])
        self.s5_d = g.dram_in("s5_d", [no, 128, 8])
        self.s5_glu_w = g.dram_in("s5_glu_w", [no, D, 2 * D])
        self.s5_glu_b = g.dram_in("s5_glu_b", [no, 2 * D])
        self.moe_router_w = g.dram_in("moe_router_w", [nl, D, 16])
        self.moe_w1 = g.dram_in("moe_w1", [nl, 16, D, 2 * D])
        self.moe_w3 = g.dram_in("moe_w3", [nl, 16, D, 2 * D])
        self.moe_w2 = g.dram_in("moe_w2", [nl, 16, 2 * D, D])
        self.final_norm_g = g.dram_in("final_norm_g", [D])
        self.out = g.dram_out("out", [NLAT, D])
        self.X = g.dram_tmp("X", [T, D])
        self.Fb = g.dram_tmp("Fb", [T, D], BF16)
        self.MOD = g.dram_tmp("MOD", [nl, 2, 6 * D])
        self.AFF = g.dram_tmp("AFF", [16, T])
        kb = self.kb
        self.arena = Arena(kb, cfg.get("arena_words", 40448))
        self.ident_f = kb.sbuf("ident_f", [128, 128], F32)
        self.ident_b = kb.sbuf("ident_b", [128, 128], BF16)
        self.IDXI = kb.sbuf("IDXI", [128, 80], I32)
        self.GVT = kb.sbuf("GVT", [128, 80], F32)
        self.ps = [kb.psum(f"ps{i}", [128, 1024], F32) for i in range(4)]
        self.psk = [f"ps{i}" for i in range(4)]
        kb.op("pool", lambda e: e.iota(self.ident_f[:], pattern=[[1, 128]], base=0, channel_multiplier=-1,
                                       allow_small_or_imprecise_dtypes=True), W=["ident_f"])
        g.kb.op("dve", lambda e: e.tensor_single_scalar(out=self.ident_f[:], in_=self.ident_f[:], scalar=0.0, op=ALU.is_equal),
                R=["ident_f"], W=["ident_f"])
        g.cp(self.ident_b[:], self.ident_f[:], R=["ident_f"], W=["ident_b"])

    def phase0(self):
        g = self
        A = self.arena
        A.reset()
        for i in range(4):
            r0 = i * (T // 4)
            g.ld(self.X[r0:r0 + T // 4, :], self.xin[r0:r0 + T // 4, :], R=[], W=[f"Xinit{i}"])
        cs, kcs = A.alloc("cs", 16)
        g.ld(cs, self.cin, R=[], W=[kcs])
        g.act(cs, cs, AF.Silu, R=[kcs], W=[kcs])
        cs3 = cs.rearrange("p (k c) -> p k c", c=2)
        mb, kmb = A.alloc("mb", 6144, parts=2)
        m2, km2 = A.alloc("m2", 6144, parts=2)
        stg = [A.alloc(f"stg{i}", 4096) for i in range(2)]
        for li in range(len(self.layers)):
            g.ld(mb, self.mod_b[li].partition_broadcast(2), R=[], W=[kmb])
            for n in range(12):
                s, ks = stg[n % 2]
                s3 = s.rearrange("p (k n) -> p k n", n=512)
                g.ld(s3, self.mod_w[li][:, n * 512:(n + 1) * 512].rearrange("(k p) n -> p k n", p=128), R=[], W=[ks])
                for k in range(8):
                    g.mm(self.ps[0][0:2, 0:512], cs3[:, k, :], s3[:, k, :], k == 0, k == 7, R=[kcs, ks], W=["ps0"])
                g.tt(m2[:, n * 512:(n + 1) * 512], self.ps[0][0:2, 0:512], mb[:, n * 512:(n + 1) * 512], ALU.add,
                     R=["ps0", kmb], W=[km2])
            g.st(self.MOD[li], m2, R=[km2], W=[f"MOD{li}"])

    def load_mod(self, li, which, isctx, dst, kdst):
        self.ld(dst, self.MOD[li, isctx, which * D:(which + 1) * D].partition_broadcast(128), R=[], W=[kdst])

    def load_bcast(self, vec_ap, dst, kdst, n=128):
        self.ld(dst, vec_ap.partition_broadcast(n), R=[], W=[kdst])

    def norm_tile(self, x, kx, ssq, kss, junk, kjunk):
        g = self
        g.act(junk, x, AF.Square, R=[kx], W=[kjunk, kss], accum_out=ssq)
        g.rsqrt_small(ssq, ssq, 1.0 / D, None, R=[kss], W=[kss])

    def post_stage(self, li, l, d_fn, alloc_extra=None):
        g = self
        A = self.arena
        G1 = [A.alloc(f"G1_{c}", D) for c in range(2)]
        A2 = [A.alloc(f"A2_{c}", D) for c in range(2)]
        B2 = [A.alloc(f"B2_{c}", D) for c in range(2)]
        ng, kng = A.alloc("ng", D)
        g.load_bcast(self.norm_g[li, 1], ng, kng)
        for c in range(2):
            g.load_mod(li, 2, c, *G1[c])
            g.load_mod(li, 4, c, *A2[c])
            g.load_mod(li, 3, c, *B2[c])
            g.stt(A2[c][0], A2[c][0], 1.0, ng, ALU.add, ALU.mult, R=[A2[c][1], kng], W=[A2[c][1]])
        xt = [A.alloc(f"xt{i}", D) for i in range(2)]
        xn = [A.alloc(f"xn{i}", D) for i in range(2)]
        tmp, ktmp = A.alloc("tmp", D)
        ff = [A.alloc(f"ff{i}", D) for i in range(2)]
        fb = [A.alloc(f"fb{i}", D, BF16) for i in range(2)]
        fT, kfT = A.alloc("fT32", D)
        wr, kwr = A.alloc("wr", 128)
        st_, kst = A.alloc("stat", 8)
        ex, kex = A.alloc("ex", 16)
        aft, kaft = A.alloc("AFFT", T, parts=16)
        g.ld(wr.rearrange("p (k e) -> p k e", e=16), self.moe_router_w[li].rearrange("(k p) e -> p k e", p=128), R=[], W=[kwr])
        wr3 = wr.rearrange("p (k e) -> p k e", e=16)
        psd, pst, psl = self.ps[0], self.ps[1], self.ps[2]
        for tt in range(NT):
            c = 1 if tt < 2 else 0
            b = tt % 2
            x, kx = xt[b]
            g.ld(x, self.X[tt * 128:(tt + 1) * 128, :], R=[f"X{tt}"], W=[kx])
            d, kd = d_fn(tt)
            g.tt(tmp, d, G1[c][0], ALU.mult, R=[kd, G1[c][1]], W=[ktmp])
            xo, kxo = xn[b]
            g.tt(xo, x, tmp, ALU.add, R=[kx, ktmp], W=[kxo])
            g.st(self.X[tt * 128:(tt + 1) * 128, :], xo, R=[kxo], W=[f"X{tt}"])
            ssq = st_[:, 0:1]
            g.act(tmp, xo, AF.Square, R=[kxo], W=[ktmp, kst], accum_out=ssq)
            g.rsqrt_small(ssq, ssq, 1.0 / D, None, R=[kst], W=[kst])
            f, kf = ff[b]
            g.stt(f, xo, ssq, A2[c][0], ALU.mult, ALU.mult, R=[kxo, kst, A2[c][1]], W=[kf])
            g.tt(f, f, B2[c][0], ALU.add, R=[kf, B2[c][1]], W=[kf])
            fbt, kfb = fb[b]
            g.cp(fbt, f, R=[kf], W=[kfb], eng="act")
            g.st(self.Fb[tt * 128:(tt + 1) * 128, :], fbt, R=[kfb], W=[f"Fb{tt}"])
            for k in range(8):
                g.tr(pst[:, k * 128:(k + 1) * 128], f[:, k * 128:(k + 1) * 128], self.ident_f[:], R=[kf, "ident_f"], W=["ps1"])
            g.cp(fT, pst[:, :], R=["ps1"], W=[kfT], eng="act")
            for k in range(8):
                g.mm(psl[:, 0:16], fT[:, k * 128:(k + 1) * 128], wr3[:, k, :], k == 0, k == 7, R=[kfT, kwr], W=["ps2"])
            mx = st_[:, 1:2]
            sm = st_[:, 2:3]
            g.red(mx, psl[:, 0:16], ALU.max, R=["ps2"], W=[kst])
            g.ts(mx, mx, -1.0, None, ALU.mult, None, R=[kst], W=[kst])
            g.act(ex, psl[:, 0:16], AF.Exp, R=["ps2", kst], W=[kex, kst], bias=mx, accum_out=sm)
            g.recip(sm, sm, R=[kst], W=[kst])
            g.ts(ex, ex, sm, None, ALU.mult, None, R=[kex, kst], W=[kex])
            g.tr(psl[0:16, 512:640], ex, self.ident_f[:], R=[kex, "ident_f"], W=["ps2"])
            g.cp(aft[:, tt * 128:(tt + 1) * 128], psl[0:16, 512:640], R=["ps2"], W=[kaft], eng="act")
        g.st(self.AFF, aft, R=[kaft], W=["AFF"])

    def moe_topk(self):
        g = self
        A = self.arena
        A.reset()
        af, kaf = A.alloc("af", T, parts=16)
        wk = [A.alloc(f"wk{i}", NLAT, parts=16) for i in range(2)]
        mxv, kmx = A.alloc("mxv", 544, parts=16)
        ixv, kix = A.alloc("ixv", 544, U32, parts=16)
        idf, kidf = A.alloc("idf", 544, parts=16)
        tf, ktf = A.alloc("tf", 80)
        g.ld(af, self.AFF, R=["AFF"], W=[kaf])

        def rounds(src0, ksrc0, n, col0, nr):
            src, ksrc = src0, ksrc0
            for r in range(nr):
                c0 = col0 + 8 * r
                g.kb.op("dve", lambda e, c0=c0, src=src: e.max(out=mxv[:, c0:c0 + 8], in_=src), R=[ksrc], W=[kmx])
                g.kb.op("dve", lambda e, c0=c0, src=src: e.max_index(out=ixv[:, c0:c0 + 8], in_max=mxv[:, c0:c0 + 8], in_values=src),
                        R=[ksrc, kmx], W=[kix])
                if r < nr - 1:
                    dst, kdst = wk[r % 2]
                    dstv = dst[:, 0:n]
                    g.kb.op("dve", lambda e, c0=c0, src=src, dstv=dstv: e.match_replace(out=dstv, in_to_replace=mxv[:, c0:c0 + 8],
                                                                                         in_values=src, imm_value=-1.0),
                            R=[ksrc, kmx], W=[kdst])
                    src, ksrc = dstv, kdst
        rounds(af[:, NCTX:T], kaf, NLAT, 0, 64)
        rounds(af[:, 0:NCTX], kaf, NCTX, 512, 4)
        g.cp(idf, ixv, R=[kix], W=[kidf])
        g.ts(idf[:, 0:512], idf[:, 0:512], float(NCTX), None, ALU.add, None, R=[kidf], W=[kidf])
        psl = self.ps[2]
        for sc in range(5):
            n = 128 if sc < 4 else 32
            g.tr(psl[0:n, 0:16], idf[:, sc * 128:sc * 128 + n], self.ident_f[0:16, 0:16], R=[kidf, "ident_f"], W=["ps2"])
            g.cp(self.IDXI[0:n, sc * 16:(sc + 1) * 16], psl[0:n, 0:16], R=["ps2"], W=["IDXI"])
            g.tr(psl[0:n, 16:32], mxv[:, sc * 128:sc * 128 + n], self.ident_f[0:16, 0:16], R=[kmx, "ident_f"], W=["ps2"])
            g.cp(self.GVT[0:n, sc * 16:(sc + 1) * 16], psl[0:n, 16:32], R=["ps2"], W=["GVT"])

    def moe_experts(self, li):
        g = self
        A = self.arena
        A.reset()
        W1b, _ = A.alloc("W1b", 8 * 2048, BF16)
        W3b, _ = A.alloc("W3b", 8 * 2048, BF16)
        W2b, _ = A.alloc("W2b", 16 * 1024, BF16)
        W1v = W1b.rearrange("p (k f) -> p k f", f=2048)
        W3v = W3b.rearrange("p (k f) -> p k f", f=2048)
        W2v = W2b.rearrange("p (k f) -> p k f", f=1024)
        XH, kXH = A.alloc("XSHID", 16 * 544, BF16)
        XS = XH[:, 0:5 * 1024].rearrange("p (s d) -> p s d", d=1024)
        HID = XH.rearrange("p (f s) -> p f s", s=544)
        XST, kXST = A.alloc("XST", 8 * 544, BF16)
        XSTv = XST.rearrange("p (k s) -> p k s", s=544)
        SIL = [A.alloc(f"sil{i}", 544) for i in range(2)]
        YSb = [A.alloc(f"YS{i}", 1024) for i in range(2)]
        G2 = [A.alloc(f"G2_{c}", D) for c in range(2)]
        for c in range(2):
            g.load_mod(li, 5, c, *G2[c])

        def load13(e):
            for (wsrc, wv, nm) in ((self.moe_w1, W1v, "W1"), (self.moe_w3, W3v, "W3")):
                for k in range(8):
                    g.ld(wv[:, k, :], wsrc[li, e, k * 128:(k + 1) * 128, :], R=[], W=[f"{nm}.{k}"], q="pool")

        def load2(e):
            for fc in range(0, 16, 2):
                g.ld(W2v[:, fc:fc + 2, :], self.moe_w2[li, e, fc * 128:(fc + 2) * 128, :].rearrange("(a p) n -> p a n", p=128),
                     R=[], W=[f"W2.{fc}", f"W2.{fc + 1}"], q="pool")
        load13(0)
        load2(0)
        ysi = 0
        for e in range(16):
            for sc in range(5):
                n = 128 if sc < 4 else 32
                col = sc * 16 + e
                g.kb.dma("pool", lambda en, n=n, sc=sc, col=col: en.indirect_dma_start(
                    out=XS[0:n, sc, :], out_offset=None, in_=self.Fb,
                    in_offset=bass.IndirectOffsetOnAxis(ap=self.IDXI[0:n, col:col + 1], axis=0)),
                    R=["IDXI", "Fball"], W=[kXH])
            pT = self.ps[3].bitcast(BF16)
            for sc in range(5):
                n = 128 if sc < 4 else 32
                for k in range(8):
                    g.tr(pT[:, k * 128:k * 128 + n], XS[0:n, sc, k * 128:(k + 1) * 128], self.ident_b[0:n, 0:n],
                         R=[kXH, "ident_b"], W=["ps3"])
                g.cp(XSTv[:, :, sc * 128:sc * 128 + n], pT[:, 0:1024].rearrange("p (k s) -> p k s", s=128)[:, :, 0:n],
                     R=["ps3"], W=[kXST], eng="act" if sc % 2 == 0 else "dve")
            for fc in range(16):
                p1, k1 = (self.ps[0], "ps0") if fc % 2 == 0 else (self.ps[2], "ps2")
                p3, k3 = (self.ps[1], "ps1") if fc % 2 == 0 else (self.ps[3], "ps3")
                for (wv, nm, pp, kp) in ((W1v, "W1", p1, k1), (W3v, "W3", p3, k3)):
                    for (n0, n1) in ((0, 512), (512, 544)):
                        for k in range(8):
                            g.mm(pp[:, n0:n1], wv[:, k, fc * 128:(fc + 1) * 128], XSTv[:, k, n0:n1], k == 0, k == 7,
                                 R=[f"{nm}.{k}", kXST], W=[kp])
                sil, ksil = SIL[fc % 2]
                g.act(sil, p1[:, 0:544], AF.Silu, R=[k1], W=[ksil])
                g.tt(HID[:, fc, :], sil, p3[:, 0:544], ALU.mult, R=[ksil, k3, kXST], W=[kXH])
            if e + 1 < 16:
                load13(e + 1)
            for sc in range(5):
                n = 128 if sc < 4 else 32
                c = 0 if sc < 4 else 1
                col = sc * 16 + e
                py, ky = (self.ps[0], "ps0") if sc % 2 == 0 else (self.ps[1], "ps1")
                for hf in range(2):
                    for fc in range(16):
                        g.mm(py[0:n, hf * 512:(hf + 1) * 512], HID[:, fc, sc * 128:sc * 128 + n], W2v[:, fc, hf * 512:(hf + 1) * 512],
                             fc == 0, fc == 15, R=[kXH, f"W2.{fc}"], W=[ky])
                YS, kYS = YSb[ysi % 2]
                ysi += 1
                g.stt(YS[0:n, :], py[0:n, :], self.GVT[0:n, col:col + 1], G2[c][0][0:n, :], ALU.mult, ALU.mult,
                      R=[ky, "GVT", G2[c][1]], W=[kYS])
                g.kb.dma("pool", lambda en, n=n, col=col, YS=YS: en.indirect_dma_start(
                    out=self.X, out_offset=bass.IndirectOffsetOnAxis(ap=self.IDXI[0:n, col:col + 1], axis=0),
                    in_=YS[0:n, :], in_offset=None, compute_op=ALU.add),
                    R=["IDXI", kYS, "Xsc"], W=["Xsc"])
            if e + 1 < 16:
                load2(e + 1)

    def final_norm(self):
        g = self
        A = self.arena
        A.reset()
        fg, kfg = A.alloc("fg", D)
        g.load_bcast(self.final_norm_g, fg, kfg)
        xt = [A.alloc(f"xt{i}", D) for i in range(2)]
        ot = [A.alloc(f"ot{i}", D) for i in range(2)]
        junk, kj = A.alloc("junk", D)
        st_, kst = A.alloc("stat", 8)
        for tt in range(2, NT):
            b = tt % 2
            x, kx = xt[b]
            o, ko = ot[b]
            g.ld(x, self.X[tt * 128:(tt + 1) * 128, :], R=[f"X{tt}"], W=[kx])
            g.norm_tile(x, kx, st_[:, 0:1], kst, junk, kj)
            g.stt(o, x, st_[:, 0:1], fg, ALU.mult, ALU.mult, R=[kx, kst, kfg], W=[ko])
            g.st(self.out[(tt - 2) * 128:(tt - 1) * 128, :], o, R=[ko], W=[f"out{tt}"])

    def build(self):
        g = self
        cfg = self.cfg
        self.phase0()
        if cfg.get("mixer", True) and any(l % 2 == 0 for l in self.layers):
            self.setup_rope()
        for li, l in enumerate(self.layers):
            if cfg.get("mixer", True):
                if l % 2 == 0:
                    self.even_mixer(li, l)
                else:
                    self.s5_mixer(li, l)
            else:
                A = self.arena
                A.reset()
                z, kz = A.alloc("zero", D)
                g.memset(z, 0.0, W=[kz])
                self.post_stage(li, l, lambda tt: (z, kz))
            if cfg.get("moe", True):
                self.moe_topk()
                self.moe_experts(li)
        self.final_norm()
        self.kb.emit()
        return self.nc


def bc(ap, axis, n):
    a = ap.unsqueeze(axis)
    shp = list(a.shape)
    shp[axis] = n
    return a.broadcast_to(shp)


def setup_rope(self):
    g = self
    kb = self.kb
    self.cosT = kb.sbuf("cosT", [128, 1024], F32)
    self.sinT = kb.sbuf("sinT", [128, 1024], F32)
    A = self.arena
    A.reset()
    fi, kfi = A.alloc("fi", 16)
    inv, kinv = A.alloc("inv", 16)
    pidx, kp = A.alloc("pidx", 1)
    ph, kph = A.alloc("ph", 1)
    colv, kcol = A.alloc("colv", 1)
    rowv, krow = A.alloc("rowv", 32)
    ang, kang = A.alloc("ang", 1024)
    ri, kri = A.alloc("ri", 1024, I32)
    ab, kab = A.alloc("ab", 1024)
    kb.op("pool", lambda e: e.iota(fi, pattern=[[1, 16]], base=0, channel_multiplier=0, allow_small_or_imprecise_dtypes=True), W=[kfi])
    g.act(inv, fi, AF.Exp, R=[kfi], W=[kinv], scale=-(2.0 / 32.0) * math.log(10000.0))
    kb.op("pool", lambda e: e.iota(pidx, pattern=[[0, 1]], base=0, channel_multiplier=1, allow_small_or_imprecise_dtypes=True), W=[kp])
    g.ts(ph, pidx, 64.0, None, ALU.is_ge, None, R=[kp], W=[kph])
    g.stt(colv, ph, -64.0, pidx, ALU.mult, ALU.add, R=[kph, kp], W=[kcol])
    kb.op("pool", lambda e: e.iota(rowv, pattern=[[2, 32]], base=0, channel_multiplier=0, allow_small_or_imprecise_dtypes=True), W=[krow])
    g.ts(rowv, rowv, ph, None, ALU.add, None, R=[krow, kph], W=[krow])
    a4 = ang.rearrange("p (t a i) -> p t a i", a=2, i=16)
    g.tt(a4[:, :, 0, :], bc(rowv, 2, 16), bc(inv, 1, 32), ALU.mult, R=[krow, kinv], W=[kang])
    g.ts(a4[:, :, 1, :], bc(inv, 1, 32), colv, None, ALU.mult, None, R=[kinv, kcol, kang], W=[kang])
    g.ts(ang, ang, 1.0 / TWO_PI, None, ALU.mult, None, R=[kang], W=[kang])
    g.cp(ri, ang, R=[kang], W=[kri])
    g.tt(ang, ang, ri, ALU.subtract, R=[kang, kri], W=[kang])
    g.act(self.sinT[:], ang, AF.Sin, R=[kang], W=["sinT"], scale=TWO_PI)
    g.act(ab, ang, AF.Abs, R=[kang], W=[kab])
    hp, khp = A.alloc("halfpi", 1)
    g.memset(hp, math.pi / 2.0, W=[khp])
    g.act(self.cosT[:], ab, AF.Sin, R=[kab, khp], W=["cosT"], scale=-TWO_PI, bias=hp)


def rope(self, src, ksrc, dst, kdst, H, tt, tmps):
    g = self
    t = tt - 2
    sv = src.rearrange("p (h a s i) -> p h a s i", a=2, s=2, i=16)
    dv = dst.rearrange("p (h a s i) -> p h a s i", a=2, s=2, i=16)
    x1, x2 = sv[:, :, :, 0, :], sv[:, :, :, 1, :]
    cos = bc(self.cosT[:, t * 32:(t + 1) * 32].rearrange("p (a i) -> p a i", i=16), 1, H)
    sin = bc(self.sinT[:, t * 32:(t + 1) * 32].rearrange("p (a i) -> p a i", i=16), 1, H)
    (t1, k1), (t2, k2), (t3, k3), (t4, k4) = tmps
    v = lambda a: a[:, 0:H * 32].rearrange("p (h a i) -> p h a i", a=2, i=16)
    g.tt(v(t1), x1, cos, ALU.mult, R=[ksrc, "cosT"], W=[k1])
    g.tt(v(t2), x2, sin, ALU.mult, R=[ksrc, "sinT"], W=[k2])
    g.tt(dv[:, :, :, 0, :], v(t1), v(t2), ALU.subtract, R=[k1, k2], W=[kdst])
    g.tt(v(t3), x1, sin, ALU.mult, R=[ksrc, "sinT"], W=[k3], eng="pool")
    g.tt(v(t4), x2, cos, ALU.mult, R=[ksrc, "cosT"], W=[k4], eng="pool")
    g.tt(dv[:, :, :, 1, :], v(t3), v(t4), ALU.add, R=[k3, k4], W=[kdst], eng="pool")


def even_mixer(self, li, l):
    g = self
    kb = self.kb
    A = self.arena
    ei = self.ev_idx[l]
    if not hasattr(self, "QT"):
        self.QT = g.dram_tmp("QT", [1664, T], BF16)
        self.TMd = g.dram_tmp("TMd", [T, 2176], BF16)
        self.OF = g.dram_tmp("OF", [T, 512])
        self.MIXT = g.dram_tmp("MIXT", [1024, T], BF16)
    QTv = self.QT.rearrange("(c p) t -> p c t", p=128)
    MIXTv = self.MIXT.rearrange("(c p) t -> p c t", p=128)
    A.reset()
    Wb, _ = A.alloc("Wb", 8 * 2816, BF16)
    Wv = Wb.rearrange("p (k n) -> p k n", n=2816)
    stg = [A.alloc(f"stg{i}", 1408) for i in range(2)]
    ce = ["pool", "act", "dve"]
    for k in range(8):
        for hf in range(2):
            s, ks = stg[(2 * k + hf) % 2]
            g.ld(s, self.mix_in_w[ei, k * 128:(k + 1) * 128, hf * 1408:(hf + 1) * 1408], R=[], W=[ks])
            g.cp(Wv[:, k, hf * 1408:(hf + 1) * 1408], s, R=[ks], W=["Wb"], eng=ce[(2 * k + hf) % 3])
    A1 = [A.alloc(f"A1_{c}", D) for c in range(2)]
    B1 = [A.alloc(f"B1_{c}", D) for c in range(2)]
    ng, kng = A.alloc("ng", D)
    g.load_bcast(self.norm_g[li, 0], ng, kng)
    for c in range(2):
        g.load_mod(li, 1, c, *A1[c])
        g.load_mod(li, 0, c, *B1[c])
        g.stt(A1[c][0], A1[c][0], 1.0, ng, ALU.add, ALU.mult, R=[A1[c][1], kng], W=[A1[c][1]])
    gq, kgq = A.alloc("gq", 64)
    gk, kgk = A.alloc("gk", 64)
    g.load_bcast(self.qk_norm_g[ei, 0:64], gq, kgq)
    g.load_bcast(self.qk_norm_g[ei, 64:128], gk, kgk)
    lgt, klg = A.alloc("lgt", 16)
    g.load_bcast(self.ret_log_rate[ei], lgt, klg)
    g.act(lgt, lgt, AF.Exp, R=[klg], W=[klg])
    g.ts(lgt, lgt, -1.0, None, ALU.mult, None, R=[klg], W=[klg])
    pidx, kp = A.alloc("pidx", 1)
    pr_, kpr = A.alloc("prev", 1)
    kb.op("pool", lambda e: e.iota(pidx, pattern=[[0, 1]], base=0, channel_multiplier=1, allow_small_or_imprecise_dtypes=True), W=[kp])
    g.ts(pr_, pidx, -1.0, 127.0, ALU.mult, ALU.add, R=[kp], W=[kpr])
    DK, kDK = A.alloc("DK", 16)
    g.ts(DK[:, 0:8], lgt[:, 0:8], pr_, None, ALU.mult, None, R=[klg, kpr], W=[kDK])
    g.ts(DK[:, 8:16], lgt[:, 8:16], pidx, None, ALU.mult, None, R=[klg, kp, kDK], W=[kDK])
    g.act(DK, DK, AF.Exp, R=[kDK], W=[kDK])
    xt = [A.alloc(f"xt{i}", D) for i in range(2)]
    tmp, ktmp = A.alloc("tmp", D)
    hb, khb = A.alloc("hb", D, BF16)
    hT, khT = A.alloc("hT", D, BF16)
    P, kP = A.alloc("P", 2816)
    sqt, ksq = A.alloc("sqt", 640)
    st_, kst = A.alloc("stat", 16)
    QKb = [A.alloc(f"QKb{i}", 1664, BF16) for i in range(2)]
    TM = [A.alloc(f"TM{i}", 2176, BF16) for i in range(2)]
    QTs = [A.alloc(f"QTs{i}", 1664, BF16) for i in range(2)]
    tmps = [A.alloc(f"rt{i}", 512) for i in range(4)]
    psT = self.ps[3].bitcast(BF16)
    for tt in range(NT):
        c = 1 if tt < 2 else 0
        b = tt % 2
        x, kx = xt[b]
        g.ld(x, self.X[tt * 128:(tt + 1) * 128, :], R=[f"X{tt}"], W=[kx])
        ssq = st_[:, 0:1]
        g.act(tmp, x, AF.Square, R=[kx], W=[ktmp, kst], accum_out=ssq)
        g.rsqrt_small(ssq, ssq, 1.0 / D, None, R=[kst], W=[kst])
        g.stt(tmp, x, ssq, A1[c][0], ALU.mult, ALU.mult, R=[kx, kst, A1[c][1]], W=[ktmp])
        g.tt(hb, tmp, B1[c][0], ALU.add, R=[ktmp, B1[c][1]], W=[khb])
        for k in range(8):
            g.tr(psT[:, k * 128:(k + 1) * 128], hb[:, k * 128:(k + 1) * 128], self.ident_b[:], R=[khb, "ident_b"], W=["ps3"])
        g.cp(hT, psT[:, 0:1024], R=["ps3"], W=[khT], eng="act")
        for (n0, w, pp, kp_, off) in ((0, 512, 0, "ps0", 0), (512, 512, 0, "ps0", 512), (1024, 512, 1, "ps1", 0),
                                      (1536, 512, 1, "ps1", 512), (2048, 512, 2, "ps2", 0), (2560, 256, 2, "ps2", 512)):
            for k in range(8):
                g.mm(self.ps[pp][:, off:off + w], hT[:, k * 128:(k + 1) * 128], Wv[:, k, n0:n0 + w], k == 0, k == 7,
                     R=[khT, "Wb"], W=[kp_])
        g.cp(P[:, 0:1024], self.ps[0][:, :], R=["ps0"], W=[kP], eng="act")
        g.cp(P[:, 1024:2048], self.ps[1][:, :], R=["ps1"], W=[kP], eng="dve")
        g.cp(P[:, 2048:2816], self.ps[2][:, 0:768], R=["ps2"], W=[kP], eng="act")
        qa = P[:, 2048:2688]
        qa3 = qa.rearrange("p (h d) -> p h d", d=64)
        g.act(sqt, qa, AF.Square, R=[kP], W=[ksq])
        ss10 = st_[:, 4:14]
        g.red(ss10, sqt.rearrange("p (h d) -> p h d", d=64), ALU.add, R=[ksq], W=[kst])
        g.rsqrt_small(ss10, ss10, 1.0 / 64.0, None, R=[kst], W=[kst])
        g.tt(qa3, qa3, bc(ss10, 2, 64), ALU.mult, R=[kP, kst], W=[kP])
        g.tt(qa3[:, 0:8, :], qa3[:, 0:8, :], bc(gq, 1, 8), ALU.mult, R=[kP, kgq], W=[kP])
        g.tt(qa3[:, 8:10, :], qa3[:, 8:10, :], bc(gk, 1, 2), ALU.mult, R=[kP, kgk], W=[kP])
        g.ts(P[:, 512:1024], P[:, 512:1024], 0.125, None, ALU.mult, None, R=[kP], W=[kP], eng="pool")
        qk, kqk = QKb[b]
        if c == 0:
            rope(self, P[:, 0:1024], kP, qk[:, 0:1024], kqk, 16, tt, tmps)
            rope(self, P[:, 2048:2688], kP, qk[:, 1024:1664], kqk, 10, tt, tmps)
        else:
            g.cp(qk[:, 0:1024], P[:, 0:1024], R=[kP], W=[kqk], eng="dve")
            g.cp(qk[:, 1024:1664], P[:, 2048:2688], R=[kP], W=[kqk], eng="pool")
        tm, ktm = TM[b]
        rk3 = qk[:, 512:1024].rearrange("p (h d) -> p h d", d=64)
        g.tt(tm[:, 0:512].rearrange("p (h d) -> p h d", d=64), rk3, bc(DK[:, 0:8], 2, 64), ALU.mult, R=[kqk, kDK], W=[ktm])
        g.tt(tm[:, 512:1024].rearrange("p (h d) -> p h d", d=64), rk3, bc(DK[:, 8:16], 2, 64), ALU.mult, R=[kqk, kDK], W=[ktm], eng="pool")
        g.cp(tm[:, 1024:1536], P[:, 1024:1536], R=[kP], W=[ktm], eng="pool")
        g.act(tm[:, 1536:2048], P[:, 1536:2048], AF.Silu, R=[kP], W=[ktm])
        g.cp(tm[:, 2048:2176], P[:, 2688:2816], R=[kP], W=[ktm], eng="act")
        g.st(self.TMd[tt * 128:(tt + 1) * 128, :], tm, R=[ktm], W=[f"TMd{tt}"])
        for ch in range(13):
            g.tr(psT[:, ch * 128:(ch + 1) * 128], qk[:, ch * 128:(ch + 1) * 128], self.ident_b[:], R=[kqk, "ident_b"], W=["ps3"])
        qs, kqs = QTs[b]
        g.cp(qs, psT[:, 0:1664], R=["ps3"], W=[kqs], eng="act")
        g.st(QTv[:, :, tt * 128:(tt + 1) * 128], qs.rearrange("p (c t) -> p c t", t=128), R=[kqs], W=[f"QT{tt}"])

    if self.cfg.get('even_stop') == 1:
        return
    A.reset()
    lgt, klg = A.alloc("lgt", 16)
    g.load_bcast(self.ret_log_rate[ei], lgt, klg)
    g.act(lgt, lgt, AF.Exp, R=[klg], W=[klg])
    g.ts(lgt, lgt, -1.0, None, ALU.mult, None, R=[klg], W=[klg])
    diff, kdf = A.alloc("diff", 128)
    ndiff, kndf = A.alloc("ndiff", 128)
    mk, kmk = A.alloc("mk", 128)
    i1, ki1 = A.alloc("i1", 128)
    i2, ki2 = A.alloc("i2", 128)
    kb.op("pool", lambda e: e.iota(diff, pattern=[[1, 128]], base=0, channel_multiplier=-1, allow_small_or_imprecise_dtypes=True), W=[kdf])
    kb.op("pool", lambda e: e.iota(ndiff, pattern=[[-1, 128]], base=0, channel_multiplier=1, allow_small_or_imprecise_dtypes=True), W=[kndf])
    kb.op("pool", lambda e: e.iota(i1, pattern=[[1, 128]], base=1, channel_multiplier=0, allow_small_or_imprecise_dtypes=True), W=[ki1])
    kb.op("pool", lambda e: e.iota(i2, pattern=[[-1, 128]], base=128, channel_multiplier=0, allow_small_or_imprecise_dtypes=True), W=[ki2])
    DT = [A.alloc(f"DT{d}", 1024) for d in range(2)]
    DQ = [A.alloc(f"DQ{d}", 512) for d in range(2)]
    dc = [A.alloc(f"dc{d}", 4) for d in range(2)]
    lgh = [A.alloc(f"lgh{d}", 4) for d in range(2)]
    for d in range(2):
        src_d, ksd = (diff, kdf) if d == 0 else (ndiff, kndf)
        g.ts(mk, diff, 0.0, None, ALU.is_ge if d == 0 else ALU.is_lt, None, R=[kdf], W=[kmk])
        dt_, kdt = DT[d]
        for h in range(8):
            sl = (h % 2) * 4 + h // 2
            g.act(dt_[:, sl * 128:(sl + 1) * 128], src_d, AF.Exp, R=[ksd, klg], W=[kdt], scale=lgt[:, d * 8 + h:d * 8 + h + 1])
        g.tt(dt_.rearrange("p (h i) -> p h i", i=128), dt_.rearrange("p (h i) -> p h i", i=128), bc(mk, 1, 8), ALU.mult,
             R=[kdt, kmk], W=[kdt])
        lh, klh = lgh[d]
        lsel = lgt[:, d * 8:(d + 1) * 8].rearrange("p (q two) -> p q two", two=2)
        g.cp(lh[0:64, :], lsel[0:64, :, 0], R=[klg], W=[klh])
        g.cp(lh[64:128, :], lsel[64:128, :, 1], R=[klg], W=[klh])
        dq, kdq = DQ[d]
        isrc, kis = (i1, ki1) if d == 0 else (i2, ki2)
        for q in range(4):
            g.act(dq[:, q * 128:(q + 1) * 128], isrc, AF.Exp, R=[kis, klh], W=[kdq], scale=lh[:, q:q + 1])
        g.act(dc[d][0], lh, AF.Exp, R=[klh], W=[dc[d][1]], scale=128.0)
    gng, kgng = A.alloc("gng", 512)
    g.load_bcast(self.ret_gn_g[ei], gng, kgng)
    QKT = [A.alloc(f"QKT{i}", 1024, BF16) for i in range(2)]
    TMt = [A.alloc(f"TMt{i}", 2176, BF16) for i in range(2)]
    PT, kPT = A.alloc("PT", 1024, BF16)
    qtl, kqtl = A.alloc("qtl", 512, BF16)
    S, kS = A.alloc("S", 256)
    Sb, kSb = A.alloc("Sb", 256, BF16)
    o32 = [A.alloc(f"o32_{i}", 512) for i in range(2)]
    oft = [A.alloc(f"of{i}", 512) for i in range(2)]
    sq2, ksq2 = A.alloc("sq2", 512)
    gs, kgs = A.alloc("gs", 32)
    mr = [A.alloc(f"mr{i}", 512, BF16) for i in range(2)]
    mrT = [A.alloc(f"mrT{i}", 512, BF16) for i in range(2)]
    psS, psO = self.ps[0], self.ps[1]
    S3 = S.rearrange("p (q e) -> p q e", e=64)
    if self.cfg.get('even_stop') == 21:
        return
    for d in range(2):
        if d == 1 and self.cfg.get('even_stop') == 22:
            return
        order = list(range(NT)) if d == 0 else [1, 0] + list(range(NT - 1, 1, -1))
        g.memset(S, 0.0, W=[kS])
        g.memset(Sb, 0.0, W=[kSb])
        koff = 0 if d == 0 else 512
        for n_i, tt in enumerate(order):
            b = n_i % 2
            qkt, kq = QKT[b]
            tm, ktm = TMt[b]
            qk3 = qkt.rearrange("p (c t) -> p c t", t=128)
            g.ld(qk3, QTv[:, 0:8, tt * 128:(tt + 1) * 128], R=[f"QT{tt}"], W=[kq])
            g.ld(tm, self.TMd[tt * 128:(tt + 1) * 128, :], R=[f"TMd{tt}"], W=[ktm])
            g.tt(qtl, qkt[:, 0:512], DQ[d][0], ALU.mult, R=[kq, DQ[d][1]], W=[kqtl])
            for h in range(8):
                par, pr = h % 2, h // 2
                sl = par * 4 + pr
                g.mm(psS[:, sl * 128:(sl + 1) * 128], qk3[par * 64:(par + 1) * 64, 4 + pr, :], qk3[par * 64:(par + 1) * 64, pr, :],
                     True, True, R=[kq], W=["ps0"])
            g.tt(PT, psS[:, :], DT[d][0], ALU.mult, R=["ps0", DT[d][1]], W=[kPT])
            for h in range(8):
                par, pr = h % 2, h // 2
                sl = par * 4 + pr
                g.mm(psO[:, h * 64:(h + 1) * 64], PT[:, sl * 128:(sl + 1) * 128], tm[:, 1024 + h * 64:1024 + (h + 1) * 64],
                     True, bool(self.cfg.get('no_acc')), R=[kPT, ktm], W=["ps1"])
                if self.cfg.get('no_acc'):
                    continue
                g.mm(psO[:, h * 64:(h + 1) * 64], qtl[par * 64:(par + 1) * 64, pr * 128:(pr + 1) * 128],
                     Sb[par * 64:(par + 1) * 64, pr * 64:(pr + 1) * 64], False, True, R=[kqtl, kSb], W=["ps1"])
            for pr in range(4):
                g.mm(psO[:, 512 + pr * 128:512 + (pr + 1) * 128], tm[:, koff + pr * 128:koff + (pr + 1) * 128],
                     tm[:, 1024 + pr * 128:1024 + (pr + 1) * 128], True, True, R=[ktm], W=["ps1u"])
            g.tt(S3, S3, bc(dc[d][0], 2, 64), ALU.mult, R=[kS, dc[d][1]], W=[kS])
            U3 = psO[:, 512:1024].rearrange("p (q e) -> p q e", e=128)
            g.tt(S3[0:64], S3[0:64], U3[0:64, :, 0:64], ALU.add, R=[kS, "ps1u"], W=[kS])
            g.tt(S3[64:128], S3[64:128], U3[64:128, :, 64:128], ALU.add, R=[kS, "ps1u"], W=[kS])
            g.cp(Sb, S, R=[kS], W=[kSb], eng="act")
            o, ko = o32[b]
            if d == 0:
                g.cp(o, psO[:, 0:512], R=["ps1"], W=[ko], eng="act")
                g.st(self.OF[tt * 128:(tt + 1) * 128, :], o, R=[ko], W=[f"OF{tt}"])
            else:
                of, kof = oft[b]
                g.ld(of, self.OF[tt * 128:(tt + 1) * 128, :], R=[f"OF{tt}"], W=[kof])
                g.tt(o, psO[:, 0:512], of, ALU.add, R=["ps1", kof], W=[ko])
                o3 = o.rearrange("p (h e) -> p h e", e=64)
                s1, s2, mean, msq, var = gs[:, 0:8], gs[:, 8:16], gs[:, 16:24], gs[:, 24:32], gs[:, 8:16]
                g.red(s1, o3, ALU.add, R=[ko], W=[kgs])
                g.act(sq2, o, AF.Square, R=[ko], W=[ksq2])
                g.red(s2, sq2.rearrange("p (h e) -> p h e", e=64), ALU.add, R=[ksq2], W=[kgs])
                g.ts(mean, s1, 1.0 / 64.0, None, ALU.mult, None, R=[kgs], W=[kgs])
                g.tt(msq, mean, mean, ALU.mult, R=[kgs], W=[kgs])
                g.stt(var, s2, 1.0 / 64.0, msq, ALU.mult, ALU.subtract, R=[kgs], W=[kgs])
                g.rsqrt_small(var, var, 1.0, None, R=[kgs], W=[kgs])
                g.tt(o3, o3, bc(mean, 2, 64), ALU.subtract, R=[ko, kgs], W=[ko])
                g.tt(o3, o3, bc(var, 2, 64), ALU.mult, R=[ko, kgs], W=[ko])
                g.tt(o, o, gng, ALU.mult, R=[ko, kgng], W=[ko], eng="pool")
                m_, km = mr[b]
                g.tt(m_, o, tm[:, 1536:2048], ALU.mult, R=[ko, ktm], W=[km], eng="pool")
                pT2 = self.ps[3].bitcast(BF16)
                for ch in range(4):
                    g.tr(pT2[:, ch * 128:(ch + 1) * 128], m_[:, ch * 128:(ch + 1) * 128], self.ident_b[:], R=[km, "ident_b"], W=["ps3"])
                mt, kmt = mrT[b]
                g.cp(mt, pT2[:, 0:512], R=["ps3"], W=[kmt], eng="act")
                g.st(MIXTv[:, 0:4, tt * 128:(tt + 1) * 128], mt.rearrange("p (c t) -> p c t", t=128), R=[kmt], W=[f"MIXT{tt}"])

    if self.cfg.get('even_stop') == 2:
        return
    A.reset()
    Kstd, kKs = A.alloc("Kstd", T, BF16)
    Kswp, kKw = A.alloc("Kswp", T, BF16)
    g.ld(Kstd, self.QT[1536:1664, :], R=["QTall"], W=[kKs])
    g.ld(Kswp[0:64, :], self.QT[1600:1664, :], R=["QTall"], W=[kKw])
    g.ld(Kswp[64:128, :], self.QT[1536:1600, :], R=["QTall"], W=[kKw])
    VA, kVA = A.alloc("VA", NT * 130, BF16)
    VA4 = VA.rearrange("p (t kv d) -> p t kv d", kv=2, d=65)
    g.memset(VA4[:, :, :, 64:65], 1.0, W=[kVA])
    for t in range(NT):
        g.ld(VA4[:, t, :, 0:64], self.TMd[t * 128:(t + 1) * 128, 2048:2176].rearrange("p (kv d) -> p kv d", kv=2), R=["TMdall"], W=[kVA])
    onesf, kon = A.alloc("onesf", 64)
    g.memset(onesf, 1.0, W=[kon])
    Qb = [A.alloc(f"Qb{i}", 2048, BF16) for i in range(2)]
    PTa = [A.alloc(f"PTa{i}", 512, BF16) for i in range(2)]
    rd, krd = A.alloc("rd", 512)
    rdb, krdb = A.alloc("rdb", 512)
    aT = [A.alloc(f"aT{i}", 512, BF16) for i in range(2)]
    blocks = [(0, 256, [0, 1])] + [(NCTX + qb * 512, 512, list(range(NT))) for qb in range(8)]
    for bi, (q0, nq, ktiles) in enumerate(blocks):
        qb_, kqb = Qb[bi % 2]
        qb3 = qb_.rearrange("p (c t) -> p c t", t=512)
        g.ld(qb3[:, :, 0:nq], QTv[:, 8:12, q0:q0 + nq], R=["QTall"], W=[kqb])
        for h in range(8):
            par, kv, pr = h % 2, h // 4, h // 2
            K, kK = (Kstd, kKs) if par == kv else (Kswp, kKw)
            psOt, kpo = (self.ps[2], "ps2") if h % 2 == 0 else (self.ps[3], "ps3")
            for ki, kt in enumerate(ktiles):
                psSt, kps = (self.ps[0], "ps0") if ki % 2 == 0 else (self.ps[1], "ps1")
                g.mm(psSt[:, 0:nq], K[par * 64:(par + 1) * 64, kt * 128:(kt + 1) * 128], qb3[par * 64:(par + 1) * 64, pr, 0:nq],
                     True, True, R=[kK, kqb], W=[kps])
                pa, kpa = PTa[ki % 2]
                g.act(pa[:, 0:nq], psSt[:, 0:nq], AF.Exp, R=[kps], W=[kpa], scale=0.125)
                g.mm(psOt[0:65, 0:nq], VA4[:, kt, kv, :], pa[:, 0:nq], ki == 0, ki == len(ktiles) - 1, R=[kVA, kpa], W=[kpo])
            g.recip(rd[64:65, 0:nq], psOt[64:65, 0:nq], R=[kpo], W=[krd])
            g.mm(psOt[0:64, 512:512 + nq], onesf[64:65, 0:64], rd[64:65, 0:nq], True, True, R=[kon, krd], W=[kpo + "b"])
            g.cp(rdb[0:64, 0:nq], psOt[0:64, 512:512 + nq], R=[kpo + "b"], W=[krdb], eng="act")
            at, kat = aT[h % 2]
            g.tt(at[0:64, 0:nq], psOt[0:64, 0:nq], rdb[0:64, 0:nq], ALU.mult, R=[kpo, krdb], W=[kat])
            g.st(self.MIXT[512 + h * 64:512 + (h + 1) * 64, q0:q0 + nq], at[0:64, 0:nq], R=[kat], W=[f"MIXTa{bi}_{h}"])

    if self.cfg.get('even_stop') == 3:
        return
    A.reset()
    Wo, kWo = A.alloc("Wo", 8 * 1024, BF16)
    Wov = Wo.rearrange("p (k n) -> p k n", n=1024)
    stg2 = [A.alloc(f"stgo{i}", 1024) for i in range(2)]
    for k in range(8):
        s, ks = stg2[k % 2]
        g.ld(s, self.mix_out_w[ei, k * 128:(k + 1) * 128, :], R=[], W=[ks])
        g.cp(Wov[:, k, :], s, R=[ks], W=[kWo], eng=ce[k % 3])
    mixT = [A.alloc(f"mixT{i}", 1024, BF16) for i in range(2)]

    def d_fn(tt):
        m_, km = mixT[tt % 2]
        m3 = m_.rearrange("p (c t) -> p c t", t=128)
        g.ld(m3, MIXTv[:, :, tt * 128:(tt + 1) * 128], R=["MIXTall"], W=[km])
        for hf in range(2):
            for k in range(8):
                g.mm(self.ps[0][:, hf * 512:(hf + 1) * 512], m3[:, k, :], Wov[:, k, hf * 512:(hf + 1) * 512], k == 0, k == 7,
                     R=[km, kWo], W=["ps0"])
        return self.ps[0][:, :], "ps0"
    self.post_stage(li, l, d_fn)


Prog.setup_rope = setup_rope
Prog.even_mixer = even_mixer


def rev(ap, lo, hi):
    return ap[:, lo:hi][:, ::-1]


def s5_mixer(self, li, l):
    g = self
    kb = self.kb
    A = self.arena
    oi = self.od_idx[l]
    if not hasattr(self, "UT"):
        self.UT = g.dram_tmp("UT", [D, T], BF16)
        self.ZT = g.dram_tmp("ZT", [D, T], BF16)
    UTv = self.UT.rearrange("(c p) t -> p c t", p=128)
    ZTv = self.ZT.rearrange("(c p) t -> p c t", p=128)
    ce = ["pool", "act", "dve"]
    A.reset()
    A1 = [A.alloc(f"A1_{c}", D) for c in range(2)]
    B1 = [A.alloc(f"B1_{c}", D) for c in range(2)]
    ng, kng = A.alloc("ng", D)
    g.load_bcast(self.norm_g[li, 0], ng, kng)
    for c in range(2):
        g.load_mod(li, 1, c, *A1[c])
        g.load_mod(li, 0, c, *B1[c])
        g.stt(A1[c][0], A1[c][0], 1.0, ng, ALU.add, ALU.mult, R=[A1[c][1], kng], W=[A1[c][1]])
    xt = [A.alloc(f"xt{i}", D) for i in range(2)]
    tmp, ktmp = A.alloc("tmp", D)
    hb, khb = A.alloc("hb", D, BF16)
    hT = [A.alloc(f"hT{i}", D, BF16) for i in range(2)]
    st_, kst = A.alloc("stat", 8)
    psT = self.ps[3].bitcast(BF16)
    for tt in range(NT):
        c = 1 if tt < 2 else 0
        b = tt % 2
        x, kx = xt[b]
        g.ld(x, self.X[tt * 128:(tt + 1) * 128, :], R=[f"X{tt}"], W=[kx])
        ssq = st_[:, 0:1]
        g.act(tmp, x, AF.Square, R=[kx], W=[ktmp, kst], accum_out=ssq)
        g.rsqrt_small(ssq, ssq, 1.0 / D, None, R=[kst], W=[kst])
        g.stt(tmp, x, ssq, A1[c][0], ALU.mult, ALU.mult, R=[kx, kst, A1[c][1]], W=[ktmp])
        g.tt(hb, tmp, B1[c][0], ALU.add, R=[ktmp, B1[c][1]], W=[khb])
        for k in range(8):
            g.tr(psT[:, k * 128:(k + 1) * 128], hb[:, k * 128:(k + 1) * 128], self.ident_b[:], R=[khb, "ident_b"], W=["ps3"])
        h_, kh = hT[b]
        g.cp(h_, psT[:, 0:1024], R=["ps3"], W=[kh], eng="act")
        g.st(UTv[:, :, tt * 128:(tt + 1) * 128], h_.rearrange("p (c t) -> p c t", t=128), R=[kh], W=[f"UT{tt}"])

    A.reset()
    BT, kBT = A.alloc("BT", 64 * 128, BF16)
    BTv = BT.rearrange("p (a s) -> p a s", s=128)
    CX, kCX = A.alloc("CX", 6 * 32 * 64, BF16)
    CXv = CX.rearrange("p (a g c) -> p a g c", g=32, c=64)
    rho = [A.alloc(f"rho{d}", 32) for d in range(2)]
    rph = [A.alloc(f"rph{d}", 32) for d in range(2)]
    CN = [[A.alloc(f"cn{d}_{n}", 32) for n in range(2)] for d in range(2)]
    SN = [[A.alloc(f"sn{d}_{n}", 32) for n in range(2)] for d in range(2)]
    NSN = [[A.alloc(f"nsn{d}_{n}", 32) for n in range(2)] for d in range(2)]
    dcol, kdcol = A.alloc("dcol", 8)
    g.ld(dcol, self.s5_d[oi], R=[], W=[kdcol])
    hp, khp = A.alloc("halfpi", 1)
    g.memset(hp, math.pi / 2.0, W=[khp])
    mark = A.off
    g.memset(CX, 0.0, W=[kCX])
    brt, kbr = A.alloc("brt", 512)
    bit, kbi = A.alloc("bit", 512)
    g.ld(brt.rearrange("p (g j) -> p g j", j=16), self.s5_b_re[oi].rearrange("(gp two) p j -> (two p) gp j", two=2), R=[], W=[kbr])
    g.ld(bit.rearrange("p (g j) -> p g j", j=16), self.s5_b_im[oi].rearrange("(gp two) p j -> (two p) gp j", two=2), R=[], W=[kbi])
    br3 = brt.rearrange("p (g j) -> p g j", j=16)
    bi3 = bit.rearrange("p (g j) -> p g j", j=16)
    pt = {}
    for nm in ("are", "aim", "ldt", "lre", "dt", "mag", "ang", "r", "r2", "sn", "cs", "bre", "bim", "den", "nr", "t1", "t2", "kre", "kim"):
        pt[nm] = A.alloc("pp_" + nm, 32)
    ri, kri = A.alloc("pp_ri", 32, I32)
    bbr, kbbr = A.alloc("bbr", 512)
    bbi, kbbi = A.alloc("bbi", 512)
    tb1, ktb1 = A.alloc("tb1", 512)
    tb2, ktb2 = A.alloc("tb2", 512)
    MX, kMX = A.alloc("MX", 32 * 32, BF16)
    crt, kcr = A.alloc("crt", 512)
    cit, kci = A.alloc("cit", 512)
    psT = self.ps[3].bitcast(BF16)
    P_ = lambda n: pt[n][0]
    Kk = lambda n: pt[n][1]

    def T2(out, a, b, op):
        g.tt(P_(out), P_(a), P_(b), op, R=[Kk(a), Kk(b)], W=[Kk(out)])
    for d in range(2):
        for j, nm in enumerate(("are", "aim", "ldt")):
            g.ld(P_(nm), self.s5p[oi, d, j], R=[], W=[Kk(nm)])
        g.ts(P_("lre"), P_("are"), -1e-4, None, ALU.min, None, R=[Kk("are")], W=[Kk("lre")])
        g.act(P_("dt"), P_("ldt"), AF.Exp, R=[Kk("ldt")], W=[Kk("dt")])
        T2("mag", "lre", "dt", ALU.mult)
        g.act(P_("mag"), P_("mag"), AF.Exp, R=[Kk("mag")], W=[Kk("mag")])
        T2("ang", "aim", "dt", ALU.mult)
        g.ts(P_("r"), P_("ang"), 1.0 / TWO_PI, None, ALU.mult, None, R=[Kk("ang")], W=[Kk("r")])
        g.cp(ri, P_("r"), R=[Kk("r")], W=[kri])
        g.tt(P_("r"), P_("r"), ri, ALU.subtract, R=[Kk("r"), kri], W=[Kk("r")])
        g.act(P_("sn"), P_("r"), AF.Sin, R=[Kk("r")], W=[Kk("sn")], scale=TWO_PI)
        g.act(P_("r2"), P_("r"), AF.Abs, R=[Kk("r")], W=[Kk("r2")])
        g.act(P_("cs"), P_("r2"), AF.Sin, R=[Kk("r2"), khp], W=[Kk("cs")], scale=-TWO_PI, bias=hp)
        T2("bre", "mag", "cs", ALU.mult)
        T2("bim", "mag", "sn", ALU.mult)
        T2("den", "lre", "lre", ALU.mult)
        T2("t1", "aim", "aim", ALU.mult)
        T2("den", "den", "t1", ALU.add)
        g.recip(P_("den"), P_("den"), R=[Kk("den")], W=[Kk("den")])
        g.ts(P_("nr"), P_("bre"), -1.0, None, ALU.add, None, R=[Kk("bre")], W=[Kk("nr")])
        T2("t1", "nr", "lre", ALU.mult)
        T2("t2", "bim", "aim", ALU.mult)
        T2("kre", "t1", "t2", ALU.add)
        T2("kre", "kre", "den", ALU.mult)
        T2("t1", "bim", "lre", ALU.mult)
        T2("t2", "nr", "aim", ALU.mult)
        T2("kim", "t1", "t2", ALU.subtract)
        T2("kim", "kim", "den", ALU.mult)
        g.cp(rho[d][0], P_("mag"), R=[Kk("mag")], W=[rho[d][1]])
        g.cp(rph[d][0], P_("r"), R=[Kk("r")], W=[rph[d][1]])
        for ni, nn in enumerate((256, 512)):
            g.ts(P_("t1"), P_("r"), float(nn), None, ALU.mult, None, R=[Kk("r")], W=[Kk("t1")])
            g.cp(ri, P_("t1"), R=[Kk("t1")], W=[kri])
            g.tt(P_("t1"), P_("t1"), ri, ALU.subtract, R=[Kk("t1"), kri], W=[Kk("t1")])
            g.act(SN[d][ni][0], P_("t1"), AF.Sin, R=[Kk("t1")], W=[SN[d][ni][1]], scale=TWO_PI)
            g.act(P_("t2"), P_("t1"), AF.Abs, R=[Kk("t1")], W=[Kk("t2")])
            g.act(CN[d][ni][0], P_("t2"), AF.Sin, R=[Kk("t2"), khp], W=[CN[d][ni][1]], scale=-TWO_PI, bias=hp)
            g.ts(NSN[d][ni][0], SN[d][ni][0], -1.0, None, ALU.mult, None, R=[SN[d][ni][1]], W=[NSN[d][ni][1]])
        bb3r = bbr.rearrange("p (g j) -> p g j", j=16)
        bb3i = bbi.rearrange("p (g j) -> p g j", j=16)
        t13 = tb1.rearrange("p (g j) -> p g j", j=16)
        t23 = tb2.rearrange("p (g j) -> p g j", j=16)
        g.tt(t13, br3, bc(P_("kre"), 2, 16), ALU.mult, R=[kbr, Kk("kre")], W=[ktb1])
        g.tt(t23, bi3, bc(P_("kim"), 2, 16), ALU.mult, R=[kbi, Kk("kim")], W=[ktb2])
        g.tt(bbr, tb1, tb2, ALU.subtract, R=[ktb1, ktb2], W=[kbbr])
        g.tt(t13, bi3, bc(P_("kre"), 2, 16), ALU.mult, R=[kbi, Kk("kre")], W=[ktb1])
        g.tt(t23, br3, bc(P_("kim"), 2, 16), ALU.mult, R=[kbr, Kk("kim")], W=[ktb2])
        g.tt(bbi, tb1, tb2, ALU.add, R=[ktb1, ktb2], W=[kbbi])
        for part, (bsrc, kbs) in enumerate(((bbr, kbbr), (bbi, kbbi))):
            b4 = bsrc.rearrange("p (g two j) -> p g two j", two=2, j=16)
            for par in range(2):
                g.memset(MX, 0.0, W=[kMX])
                MX4 = MX.rearrange("p (g two c) -> p g two c", two=2, c=32)
                g.cp(MX4[0:64, :, par, 0:16], b4[0:64, :, par, :], R=[kbs], W=[kMX])
                g.cp(MX4[64:128, :, par, 16:32], b4[64:128, :, par, :], R=[kbs], W=[kMX])
                for q in range(8):
                    g.tr(psT[:, q * 128:(q + 1) * 128], MX[:, q * 128:(q + 1) * 128], self.ident_b[:], R=[kMX, "ident_b"], W=["ps3"])
                a0 = ((d * 2 + part) * 2 + par) * 8
                g.cp(BT[:, a0 * 128:(a0 + 8) * 128], psT[:, 0:1024], R=["ps3"], W=[kBT], eng="act")
        g.ld(crt.rearrange("p (g k) -> p g k", k=16), self.s5_c_re[oi, d].rearrange("(gp two) p k -> (two p) gp k", two=2), R=[], W=[kcr])
        g.ld(cit.rearrange("p (g k) -> p g k", k=16), self.s5_c_im[oi, d].rearrange("(gp two) p k -> (two p) gp k", two=2), R=[], W=[kci])
        for part, (csrc, kcs, sgn) in enumerate(((crt, kcr, 1.0), (cit, kci, -1.0), (crt, kcr, -1.0))):
            c4 = csrc.rearrange("p (g two k) -> p g two k", two=2, k=16)
            Cv = CXv[:, d * 3 + part].rearrange("p (g two) c -> p g two c", two=2)
            for gpar in range(2):
                g.ts(Cv[0:64, :, gpar, gpar * 32:gpar * 32 + 16], c4[0:64, :, gpar, :], sgn, None, ALU.mult, None, R=[kcs], W=[kCX])
                g.ts(Cv[64:128, :, gpar, gpar * 32 + 16:gpar * 32 + 32], c4[64:128, :, gpar, :], sgn, None, ALU.mult, None, R=[kcs], W=[kCX])
    if self.cfg.get("s5_stop") == 1:
        return
    kb.barrier()
    A.off = mark
    iot, kio = A.alloc("iota", T)
    kb.op("pool", lambda e: e.iota(iot, pattern=[[1, T]], base=0, channel_multiplier=0, allow_small_or_imprecise_dtypes=True), W=[kio])
    uT = [A.alloc("uT0", T, BF16)] * 2
    ysb, kys = A.alloc("ysb", T)
    NB = 512
    TI, kti = A.alloc("TI", NB, I32)
    TFt = [A.alloc(f"TF{i}", NB) for i in range(2)]
    ST = [A.alloc(f"ST{i}", NB) for i in range(2)]
    CT = [A.alloc(f"CT{i}", NB) for i in range(2)]
    W1 = [A.alloc(f"w1_{i}", NB, BF16) for i in range(2)]
    W2 = [A.alloc(f"w2_{i}", NB, BF16) for i in range(2)]
    W3 = [A.alloc(f"w3_{i}", NB, BF16) for i in range(2)]
    W4 = [A.alloc(f"w4_{i}", NB, BF16) for i in range(2)]
    BTR = [A.alloc(f"btr{i}", NB, BF16) for i in range(2)]
    BTI = [A.alloc(f"bti{i}", NB, BF16) for i in range(2)]
    WR = [A.alloc(f"wr{i}", NB) for i in range(2)]
    WI = [A.alloc(f"wi{i}", NB) for i in range(2)]
    P1 = [A.alloc(f"p1_{i}", NB, BF16) for i in range(2)]
    P2 = [A.alloc(f"p2_{i}", NB, BF16) for i in range(2)]
    P3 = [A.alloc(f"p3_{i}", NB, BF16) for i in range(2)]
    P4 = [A.alloc(f"p4_{i}", NB, BF16) for i in range(2)]
    YE = [A.alloc(f"ye{i}", NB) for i in range(2)]
    CAR = [A.alloc(f"carry{i}", 4) for i in range(2)]
    zt, kzt = uT[0]
    lat = [(NCTX + i * NB, NCTX + (i + 1) * NB) for i in range(NLAT // NB)]
    fblocks = [(0, NCTX, False)] + [(lo, hi, False) for (lo, hi) in lat]
    bblocks = [(0, NCTX, True)] + [(lo, hi, True) for (lo, hi) in reversed(lat)]
    gcount = [0]
    for q in range(8):
        u_, ku = uT[q % 2]
        g.ld(u_, self.UT[q * 128:(q + 1) * 128, :], R=["UTall"], W=[ku])
        g.ts(ysb, u_, dcol[:, q:q + 1], None, ALU.mult, None, R=[ku, kdcol], W=[kys])
        blist = []
        for gl in range(4):
            for d in range(2):
                gi = gcount[0]
                gcount[0] += 1
                for bidx, (lo, hi, rv) in enumerate(fblocks if d == 0 else bblocks):
                    blist.append(dict(gl=gl, d=d, gi=gi, bidx=bidx, lo=lo, hi=hi, rv=rv, i=len(blist)))
        for i_, bl in enumerate(blist):
            bl["prev"] = blist[i_ - 1] if bl["bidx"] > 0 else None

        def tokf(bl):
            lo, hi = bl["lo"], bl["hi"]
            return (lambda ap: rev(ap, lo, hi)) if bl["rv"] else (lambda ap: ap[:, lo:hi])

        def stageA(bl):
            gl, d, gi, b = bl["gl"], bl["d"], bl["gi"], bl["i"] % 2
            gp = 4 * q + gl
            half, par = gl // 2, gl % 2
            rows = slice(64 * half, 64 * half + 64)
            n = bl["hi"] - bl["lo"]
            tb = gi % 2
            st, kst2 = ST[tb]
            ct, kct = CT[tb]
            if bl["bidx"] == 0:
                tf, ktf = TFt[tb]
                rcol = rph[d][0][:, gp:gp + 1]
                g.ts(TI, iot[:, 0:NB], rcol, None, ALU.mult, None, R=[kio, rph[d][1]], W=[kti])
                g.stt(tf, iot[:, 0:NB], rcol, TI, ALU.mult, ALU.subtract, R=[kio, rph[d][1], kti], W=[ktf])
                g.act(st, tf, AF.Sin, R=[ktf], W=[kst2], scale=TWO_PI)
                g.act(tf, tf, AF.Abs, R=[ktf], W=[ktf])
                g.act(ct, tf, AF.Sin, R=[ktf, khp], W=[kct], scale=-TWO_PI, bias=hp)
            psBr = self.ps[0][:, b * 512:b * 512 + 512]
            psBi = self.ps[1][:, b * 512:b * 512 + 512]
            k0, k1 = f"ps0.{b}", f"ps1.{b}"
            tok = tokf(bl)
            for part, (pp, kpp) in enumerate(((psBr, k0), (psBi, k1))):
                a0 = ((d * 2 + part) * 2 + par) * 8 + q
                g.mm(pp[:, 0:n], BTv[rows, a0, :], tok(u_[rows, :]), True, True, R=[kBT, ku], W=[kpp])
            brs, kbrs = psBr, k0
            bis, kbis = psBi, k1
            w1, kw1 = W1[b]
            w2, kw2 = W2[b]
            w3, kw3 = W3[b]
            w4, kw4 = W4[b]
            btr, kbtr = BTR[b]
            bti, kbti = BTI[b]
            g.tt(w1[:, 0:n], brs[:, 0:n], ct[:, 0:n], ALU.mult, R=[kbrs, kct], W=[kw1])
            g.tt(w2[:, 0:n], bis[:, 0:n], st[:, 0:n], ALU.mult, R=[kbis, kst2], W=[kw2])
            g.tt(btr[:, 0:n], w1[:, 0:n], w2[:, 0:n], ALU.add, R=[kw1, kw2], W=[kbtr])
            g.tt(w3[:, 0:n], bis[:, 0:n], ct[:, 0:n], ALU.mult, R=[kbis, kct], W=[kw3])
            g.tt(w4[:, 0:n], brs[:, 0:n], st[:, 0:n], ALU.mult, R=[kbrs, kst2], W=[kw4])
            g.tt(bti[:, 0:n], w3[:, 0:n], w4[:, 0:n], ALU.subtract, R=[kw3, kw4], W=[kbti])

        def stageB(bl):
            gl, d, gi, b = bl["gl"], bl["d"], bl["gi"], bl["i"] % 2
            gp = 4 * q + gl
            half = gl // 2
            rows = slice(64 * half, 64 * half + 64)
            n = bl["hi"] - bl["lo"]
            tb = gi % 2
            st, kst2 = ST[tb]
            ct, kct = CT[tb]
            btr, kbtr = BTR[b]
            bti, kbti = BTI[b]
            wr, kwr = WR[b]
            wi, kwi = WI[b]
            car, kcar = CAR[b]
            pv = bl["prev"]
            if pv is None:
                g.cp(self.ps[3][:, 0:512], st, R=[kst2], W=["ps3.S"], eng="act")
                g.cp(self.ps[3][:, 512:1024], ct, R=[kct], W=["ps3.C"], eng="act")
                ini_r, ini_i, Rc = 0.0, 0.0, []
            else:
                pb = pv["i"] % 2
                npv = pv["hi"] - pv["lo"]
                ni = 0 if npv == 256 else 1
                wrl = WR[pb][0][:, npv - 1:npv]
                wil = WI[pb][0][:, npv - 1:npv]
                cn = CN[d][ni][0][:, gp:gp + 1]
                sn = SN[d][ni][0][:, gp:gp + 1]
                nsn = NSN[d][ni][0][:, gp:gp + 1]
                Rk = [WR[pb][1], WI[pb][1], CN[d][ni][1], SN[d][ni][1], NSN[d][ni][1]]
                g.ts(car[:, 2:3], wrl, cn, None, ALU.mult, None, R=Rk, W=[kcar])
                g.stt(car[:, 0:1], wil, nsn, car[:, 2:3], ALU.mult, ALU.add, R=Rk + [kcar], W=[kcar])
                g.ts(car[:, 3:4], wrl, sn, None, ALU.mult, None, R=Rk + [kcar], W=[kcar])
                g.stt(car[:, 1:2], wil, cn, car[:, 3:4], ALU.mult, ALU.add, R=Rk + [kcar], W=[kcar])
                ini_r, ini_i, Rc = car[:, 0:1], car[:, 1:2], [kcar]
            rb = rho[d][0][:, gp:gp + 1].to_broadcast([128, n])
            kb.op("dve", lambda e: e.tensor_tensor_scan(out=wr[:, 0:n], data0=rb, data1=btr[:, 0:n], initial=ini_r,
                                                        op0=ALU.mult, op1=ALU.add), R=[rho[d][1], kbtr] + Rc, W=[kwr])
            kb.op("dve", lambda e: e.tensor_tensor_scan(out=wi[:, 0:n], data0=rb, data1=bti[:, 0:n], initial=ini_i,
                                                        op0=ALU.mult, op1=ALU.add), R=[rho[d][1], kbti] + Rc, W=[kwi])
            p1, kp1 = P1[b]
            p2, kp2 = P2[b]
            p3, kp3 = P3[b]
            p4, kp4 = P4[b]
            pS, pC = self.ps[3][:, 0:512], self.ps[3][:, 512:1024]
            g.tt(p1[:, 0:n], wr[:, 0:n], pC[:, 0:n], ALU.mult, R=[kwr, "ps3.C"], W=[kp1])
            g.tt(p2[:, 0:n], wi[:, 0:n], pS[:, 0:n], ALU.mult, R=[kwi, "ps3.S"], W=[kp2])
            g.tt(p3[:, 0:n], wr[:, 0:n], pS[:, 0:n], ALU.mult, R=[kwr, "ps3.S"], W=[kp3])
            g.tt(p4[:, 0:n], wi[:, 0:n], pC[:, 0:n], ALU.mult, R=[kwi, "ps3.C"], W=[kp4])
            psY = self.ps[2][:, b * 512:b * 512 + 512]
            k2 = f"ps2.{b}"
            g.mm(psY[rows, 0:n], CXv[:, d * 3 + 0, gp, :], p1[:, 0:n], True, False, R=[kCX, kp1], W=[k2])
            g.mm(psY[rows, 0:n], CXv[:, d * 3 + 2, gp, :], p2[:, 0:n], False, False, R=[kCX, kp2], W=[k2])
            g.mm(psY[rows, 0:n], CXv[:, d * 3 + 1, gp, :], p3[:, 0:n], False, False, R=[kCX, kp3], W=[k2])
            g.mm(psY[rows, 0:n], CXv[:, d * 3 + 1, gp, :], p4[:, 0:n], False, True, R=[kCX, kp4], W=[k2])
            tok = tokf(bl)
            if True:
                g.tt(tok(ysb[rows, :]), tok(ysb[rows, :]), psY[rows, 0:n], ALU.add, R=[kys, k2], W=[kys])
            else:
                ye, kye = YE[b]
                g.cp(ye[rows, 0:n], psY[rows, 0:n], R=[k2], W=[kye], eng="act")
                g.tt(tok(ysb[rows, :]), tok(ysb[rows, :]), ye[rows, 0:n], ALU.add, R=[kys, kye], W=[kys], eng="pool")

        stageA(blist[0])
        for i_ in range(len(blist)):
            if i_ + 1 < len(blist):
                stageA(blist[i_ + 1])
            stageB(blist[i_])
        g.act(iot, ysb, AF.Square, R=[kys], W=[kio + "g"])
        g.ts(iot, iot, 0.044715, 1.0, ALU.mult, ALU.add, R=[kio + "g"], W=[kio + "g"])
        g.tt(iot, iot, ysb, ALU.mult, R=[kio + "g", kys], W=[kio + "g"])
        g.act(iot, iot, AF.Tanh, R=[kio + "g"], W=[kio + "g"], scale=math.sqrt(2.0 / math.pi))
        g.stt(iot, iot, 1.0, ysb, ALU.add, ALU.mult, R=[kio + "g", kys], W=[kio + "g"])
        g.act(zt, iot, AF.Copy, R=[kio + "g"], W=[kzt], scale=0.5)
        g.st(self.ZT[q * 128:(q + 1) * 128, :], zt, R=[kzt], W=[f"ZT{q}"])
        if q < 7:
            kb.op("pool", lambda e: e.iota(iot, pattern=[[1, T]], base=0, channel_multiplier=0, allow_small_or_imprecise_dtypes=True),
                  R=[kio + "g"], W=[kio, kio + "g"])
    if self.cfg.get("s5_stop") == 2:
        return
    A.reset()
    GW, kGW = A.alloc("GW", 8 * 2048, BF16)
    GWv = GW.rearrange("p (k n) -> p k n", n=2048)
    stg = [A.alloc(f"stgg{i}", 1024) for i in range(2)]
    for k in range(8):
        for hf in range(2):
            s, ks = stg[(2 * k + hf) % 2]
            g.ld(s, self.s5_glu_w[oi, k * 128:(k + 1) * 128, hf * 1024:(hf + 1) * 1024], R=[], W=[ks])
            g.cp(GWv[:, k, hf * 1024:(hf + 1) * 1024], s, R=[ks], W=[kGW], eng=ce[(2 * k + hf) % 3])
    gb, kgb = A.alloc("gb", 2048)
    g.load_bcast(self.s5_glu_b[oi], gb, kgb)
    zT = [A.alloc(f"zT{i}", 1024, BF16) for i in range(2)]
    asb, kasb = A.alloc("asb", 1024)
    gsb, kgsb = A.alloc("gsb", 1024)
    dt_, kdt = A.alloc("dtile", 1024)

    def d_fn(tt):
        z_, kz = zT[tt % 2]
        z3 = z_.rearrange("p (c t) -> p c t", t=128)
        g.ld(z3, ZTv[:, :, tt * 128:(tt + 1) * 128], R=["ZTall"], W=[kz])
        for ch in range(4):
            pp, kpp = (self.ps[0], "ps0") if ch < 2 else (self.ps[3], "ps3")
            for k in range(8):
                g.mm(pp[:, (ch % 2) * 512:(ch % 2 + 1) * 512], z3[:, k, :], GWv[:, k, ch * 512:(ch + 1) * 512], k == 0, k == 7,
                     R=[kz, kGW], W=[kpp])
        g.tt(asb, self.ps[0][:, :], gb[:, 0:1024], ALU.add, R=["ps0", kgb], W=[kasb])
        g.tt(gsb, self.ps[3][:, :], gb[:, 1024:2048], ALU.add, R=["ps3", kgb], W=[kgsb])
        g.act(gsb, gsb, AF.Sigmoid, R=[kgsb], W=[kgsb])
        g.tt(dt_, asb, gsb, ALU.mult, R=[kasb, kgsb], W=[kdt], eng="pool")
        return dt_, kdt
    self.post_stage(li, l, d_fn)


Prog.s5_mixer = s5_mixer


def shared_weights(inp, layers):
    ev = [l // 2 for l in layers if l % 2 == 0]
    od = [l // 2 for l in layers if l % 2 == 1]
    f = lambda a: np.ascontiguousarray(np.asarray(a, dtype=np.float32))
    w = {}
    w["mod_w"] = f(inp["mod_w"][layers])
    w["mod_b"] = f(inp["mod_b"][layers])
    w["norm_g"] = f(inp["norm_g"][layers])
    w["moe_router_w"] = f(inp["moe_router_w"][layers])
    w["moe_w1"] = f(inp["moe_w1"][layers])
    w["moe_w3"] = f(inp["moe_w3"][layers])
    w["moe_w2"] = f(inp["moe_w2"][layers])
    w["final_norm_g"] = f(inp["final_norm_g"])
    if ev:
        w["mix_in_w"] = f(inp["mix_in_w"][ev])
        w["mix_out_w"] = f(inp["mix_out_w"][ev])
        w["ret_log_rate"] = f(np.asarray(inp["ret_log_rate"])[ev].reshape(len(ev), 16))
        w["ret_gn_g"] = f(inp["ret_gn_g"][ev])
        w["qk_norm_g"] = f(np.asarray(inp["qk_norm_g"])[ev].reshape(len(ev), 128))
    else:
        w["mix_in_w"] = np.zeros((1, D, 2816), np.float32)
        w["mix_out_w"] = np.zeros((1, D, D), np.float32)
        w["ret_log_rate"] = np.zeros((1, 16), np.float32)
        w["ret_gn_g"] = np.zeros((1, 512), np.float32)
        w["qk_norm_g"] = np.zeros((1, 128), np.float32)
    if od:
        no = len(od)
        a_re = np.asarray(inp["s5_a_re"])[od]
        a_im = np.asarray(inp["s5_a_im"])[od]
        ldt = np.asarray(inp["s5_log_dt"])[od]
        pair = lambda a: a.reshape(no, 2, 32, 2, 64).transpose(0, 1, 3, 4, 2).reshape(no, 2, 128, 32)
        ldt_b = np.broadcast_to(ldt[..., None], (no, 2, 64, 64))
        w["s5p"] = f(np.stack([pair(a_re), pair(a_im), pair(ldt_b)], axis=2))
        w["s5_b_re"] = f(inp["s5_b_re"][od])
        w["s5_b_im"] = f(inp["s5_b_im"][od])
        w["s5_c_re"] = f(np.asarray(inp["s5_c_re"])[od].transpose(0, 1, 2, 4, 3))
        w["s5_c_im"] = f(np.asarray(inp["s5_c_im"])[od].transpose(0, 1, 2, 4, 3))
        w["s5_d"] = f(np.asarray(inp["s5_d"])[od].reshape(no, 8, 128).transpose(0, 2, 1))
        w["s5_glu_w"] = f(inp["s5_glu_w"][od])
        w["s5_glu_b"] = f(inp["s5_glu_b"][od])
    else:
        w["s5p"] = np.zeros((1, 2, 3, 128, 32), np.float32)
        w["s5_b_re"] = np.zeros((1, 64, 64, 16), np.float32)
        w["s5_b_im"] = np.zeros((1, 64, 64, 16), np.float32)
        w["s5_c_re"] = np.zeros((1, 2, 64, 64, 16), np.float32)
        w["s5_c_im"] = np.zeros((1, 2, 64, 64, 16), np.float32)
        w["s5_d"] = np.zeros((1, 128, 8), np.float32)
        w["s5_glu_w"] = np.zeros((1, D, 2 * D), np.float32)
        w["s5_glu_b"] = np.zeros((1, 2 * D), np.float32)
    return w


def core_inputs(inp, b, w):
    m = dict(w)
    x = np.asarray(inp["x"][b], dtype=np.float32)
    ctx = np.asarray(inp["ctx"][b], dtype=np.float32)
    m["xin"] = np.ascontiguousarray(np.concatenate([ctx, x], axis=0))
    c = np.asarray(inp["c"][b], dtype=np.float32).reshape(8, 128).T
    cc = np.asarray(inp["c_ctx"], dtype=np.float32).reshape(8, 128).T
    m["cin"] = np.ascontiguousarray(np.stack([c, cc], axis=2).reshape(128, 16))
    return m


def kernel(**inputs):
    layers = [0, 1, 2, 3]
    prog = Prog(dict(layers=layers))
    nc = prog.build()
    w = shared_weights(inputs, layers)
    in_maps = [core_inputs(inputs, b, w) for b in range(8)]
    res = run_bass_kernel_spmd(nc, in_maps, core_ids=list(range(8)))
    return np.stack([np.asarray(r["out"], dtype=np.float32) for r in res.results], axis=0)
```

```python
import contextlib
import math
import numpy as np
import concourse.bass as bass
import concourse.mybir as mybir
from concourse.bass_utils import run_bass_kernel_spmd

F32 = mybir.dt.float32
BF16 = mybir.dt.bfloat16
I32 = mybir.dt.int32
U32 = mybir.dt.uint32
ALU = mybir.AluOpType
AF = mybir.ActivationFunctionType
AX = mybir.AxisListType

SEM_LIMIT = 30000
NSLOT = 10

D = 1024
NCTX = 256
NLAT = 4096
T = NCTX + NLAT
NT = T // 128
EPS = 1e-6
TWO_PI = 2.0 * math.pi


class KB:
    ENG = ("pe", "act", "dve", "pool", "sp")

    def __init__(self, nc):
        self.nc = nc
        self.stack = contextlib.ExitStack()
        self.q = {e: [] for e in self.ENG}
        self.nsem = 0
        self.cur = {}
        for e in ("pe", "act", "dve", "pool"):
            self.cur[e] = [self._newsem(e), 0]
        self.slots = {}
        self.slot_i = {}
        for e in ("sp", "pool"):
            self.slots[e] = [[self._newsem("d" + e), 0] for _ in range(NSLOT)]
            self.slot_i[e] = 0
        self.known = {e: {} for e in self.ENG}
        self.last_w = {}
        self.reads = {}
        self.pending = {e: [] for e in self.ENG}
        self.ninst = 0

    def _newsem(self, tag):
        self.nsem += 1
        return self.stack.enter_context(self.nc.semaphore(f"s_{tag}_{self.nsem}"))

    def sbuf(self, name, shape, dtype):
        return self.stack.enter_context(self.nc.sbuf_tensor(name, list(shape), dtype))

    def psum(self, name, shape, dtype):
        return self.stack.enter_context(self.nc.psum_tensor(name, list(shape), dtype))

    def all_tokens(self):
        toks = []
        for e in ("pe", "act", "dve", "pool"):
            c = self.cur[e]
            if c[1] > 0:
                toks.append((c[0], c[1]))
        for e in self.slots:
            for s in self.slots[e]:
                if s[1] > 0:
                    toks.append((s[0], s[1]))
        return toks

    def barrier(self):
        toks = self.all_tokens()
        for e in self.ENG:
            self.pending[e] = list(toks)
        self.last_w = {}
        self.reads = {}

    def _deps(self, eng, R, W):
        need = {}

        def add(tok):
            if tok is None:
                return
            sem, val = tok
            k = id(sem)
            if k not in need or need[k][1] < val:
                need[k] = (sem, val)
        for tok in self.pending[eng]:
            add(tok)
        self.pending[eng] = []
        for r in R:
            add(self.last_w.get(r))
        for w in W:
            add(self.last_w.get(w))
            for t in self.reads.get(w, {}).values():
                add(t)
        out = []
        kn = self.known[eng]
        for k, (sem, val) in need.items():
            if kn.get(k, 0) >= val:
                continue
            kn[k] = val
            out.append((sem, val))
        return out

    def _commit(self, tok, R, W, tag):
        for w in W:
            self.last_w[w] = tok
            self.reads[w] = {}
        for r in R:
            if r in W:
                continue
            self.reads.setdefault(r, {})[tag] = tok

    def op(self, eng, fn, R=(), W=()):
        R = tuple(R)
        W = tuple(W)
        waits = self._deps(eng, R, W)
        c = self.cur[eng]
        if c[1] >= SEM_LIMIT:
            c[0] = self._newsem(eng)
            c[1] = 0
        c[1] += 1
        sem, val = c[0], c[1]
        if eng == "pe":
            self.known[eng][id(sem)] = val
        self.q[eng].append((waits, fn, sem, 1))
        self._commit((sem, val), R, W, eng)
        self.ninst += 1

    def dma(self, qeng, fn, R=(), W=()):
        R = tuple(R)
        W = tuple(W)
        waits = self._deps(qeng, R, W)
        i = self.slot_i[qeng]
        self.slot_i[qeng] = (i + 1) % NSLOT
        s = self.slots[qeng][i]
        kn = self.known[qeng]
        if s[1] > 0 and kn.get(id(s[0]), 0) < s[1]:
            waits.append((s[0], s[1]))
            kn[id(s[0])] = s[1]
        s[1] += 16
        self.q[qeng].append((waits, fn, s[0], 16))
        self._commit((s[0], s[1]), R, W, ("dma", qeng, i))
        self.ninst += 1

    def emit(self):
        nc = self.nc
        finals = self.all_tokens()
        with nc.Block() as block:
            def run(engname):
                def body(e):
                    for waits, fn, sem, inc in self.q[engname]:
                        for (ws, wv) in waits:
                            e.wait_ge(ws, wv)
                        fn(e).then_inc(sem, inc)
                    if engname == "sp":
                        for (ws, wv) in finals:
                            e.wait_ge(ws, wv)
                return body
            block.sync(run("sp"))
            block.tensor(run("pe"))
            block.scalar(run("act"))
            block.vector(run("dve"))
            block.gpsimd(run("pool"))


class Arena:
    def __init__(self, kb, words):
        self.kb = kb
        self.words = words
        self.t = kb.sbuf("arena", [128, words], F32)
        self.off = 0
        self.phase = 0

    def reset(self):
        self.kb.barrier()
        self.off = 0
        self.phase += 1

    def alloc(self, name, cols, dtype=F32, parts=128):
        w = cols if dtype in (F32, I32, U32) else (cols + 1) // 2
        w = (w + 7) // 8 * 8
        assert self.off + w <= self.words, f"arena overflow at {name}: {self.off}+{w}>{self.words}"
        ap = self.t[0:parts, self.off:self.off + w]
        self.off += w
        if dtype != F32:
            ap = ap.bitcast(dtype)
        ap = ap[:, 0:cols]
        return ap, f"p{self.phase}.{name}"


class Gen:
    def __init__(self, cfg):
        self.cfg = cfg
        self.nc = bass.Bass("TRN2", target_bir_lowering=False)
        self.kb = KB(self.nc)
        self.dbg = {}

    def mm(self, out, lhsT, rhs, start, stop, R, W):
        self.kb.op("pe", lambda e: e.matmul(out, lhsT=lhsT, rhs=rhs, start=start, stop=stop), R=R, W=W)

    def tr(self, out, in_, ident, R, W):
        self.kb.op("pe", lambda e: e.transpose(out=out, in_=in_, identity=ident), R=R, W=W)

    def act(self, out, in_, func, R, W, **kw):
        self.kb.op("act", lambda e: e.activation(out=out, in_=in_, func=func, **kw), R=R, W=W)

    def tt(self, out, a, b, op, R, W, eng="dve"):
        self.kb.op(eng, lambda e: e.tensor_tensor(out=out, in0=a, in1=b, op=op), R=R, W=W)

    def ts(self, out, in0, s1, s2, op0, op1, R, W, eng="dve", **kw):
        if s2 is None:
            self.kb.op(eng, lambda e: e.tensor_scalar(out=out, in0=in0, scalar1=s1, scalar2=None, op0=op0, **kw), R=R, W=W)
        else:
            self.kb.op(eng, lambda e: e.tensor_scalar(out=out, in0=in0, scalar1=s1, scalar2=s2, op0=op0, op1=op1, **kw), R=R, W=W)

    def stt(self, out, in0, scalar, in1, op0, op1, R, W):
        self.kb.op("dve", lambda e: e.scalar_tensor_tensor(out=out, in0=in0, scalar=scalar, in1=in1, op0=op0, op1=op1), R=R, W=W)

    def cp(self, out, in_, R, W, eng="dve"):
        if eng == "act":
            self.kb.op("act", lambda e: e.copy(out=out, in_=in_), R=R, W=W)
        else:
            self.kb.op(eng, lambda e: e.tensor_copy(out=out, in_=in_), R=R, W=W)

    def memset(self, out, val, W, eng="dve"):
        self.kb.op(eng, lambda e: e.memset(out, val), W=W)

    def ld(self, out, in_, R, W, q="sp"):
        self.kb.dma(q, lambda e: e.dma_start(out=out, in_=in_), R=R, W=W)

    def st(self, out, in_, R, W, q="pool"):
        self.kb.dma(q, lambda e: e.dma_start(out=out, in_=in_), R=R, W=W)

    def red(self, out, in_, op, R, W, axis=AX.X):
        self.kb.op("dve", lambda e: e.tensor_reduce(out=out, in_=in_, axis=axis, op=op), R=R, W=W)

    def recip(self, out, in_, R, W):
        self.kb.op("dve", lambda e: e.reciprocal(out=out, in_=in_), R=R, W=W)

    def rsqrt_small(self, out, in_, scale, tmpkey, R, W):
        self.ts(out, in_, scale, EPS, ALU.mult, ALU.add, R=R, W=W)
        self.act(out, out, AF.Sqrt, R=W, W=W)
        self.recip(out, out, R=W, W=W)

    def dram_in(self, name, shape, dtype=F32):
        return self.nc.dram_tensor(name, list(shape), dtype, kind="ExternalInput").ap()

    def dram_out(self, name, shape, dtype=F32):
        return self.nc.dram_tensor(name, list(shape), dtype, kind="ExternalOutput").ap()

    def dram_tmp(self, name, shape, dtype=F32):
        if self.cfg.get("dbg_" + name):
            ap = self.nc.dram_tensor(name, list(shape), dtype, kind="ExternalOutput").ap()
            self.dbg[name] = ap
            return ap
        return self.nc.dram_tensor(name, list(shape), dtype, kind="Internal").ap()


class Prog(Gen):
    def __init__(self, cfg):
        super().__init__(cfg)
        g = self
        layers = cfg["layers"]
        self.layers = layers
        nl = len(layers)
        ev = [l for l in layers if l % 2 == 0]
        od = [l for l in layers if l % 2 == 1]
        self.ev_idx = {l: i for i, l in enumerate(ev)}
        self.od_idx = {l: i for i, l in enumerate(od)}
        ne, no = max(len(ev), 1), max(len(od), 1)
        self.xin = g.dram_in("xin", [T, D])
        self.cin = g.dram_in("cin", [128, 16])
        self.mod_w = g.dram_in("mod_w", [nl, D, 6 * D])
        self.mod_b = g.dram_in("mod_b", [nl, 6 * D])
        self.norm_g = g.dram_in("norm_g", [nl, 2, D])
        self.mix_in_w = g.dram_in("mix_in_w", [ne, D, 2816])
        self.mix_out_w = g.dram_in("mix_out_w", [ne, D, D])
        self.ret_log_rate = g.dram_in("ret_log_rate", [ne, 16])
        self.ret_gn_g = g.dram_in("ret_gn_g", [ne, 512])
        self.qk_norm_g = g.dram_in("qk_norm_g", [ne, 128])
        self.s5p = g.dram_in("s5p", [no, 2, 3, 128, 32])
        self.s5_b_re = g.dram_in("s5_b_re", [no, 64, 64, 16])
        self.s5_b_im = g.dram_in("s5_b_im", [no, 64, 64, 16])
        self.s5_c_re = g.dram_in("s5_c_re", [no, 2, 64, 64, 16])
        self.s5_c_im = g.dram_in("s5_c_im", [no, 2, 64, 64, 16])
        self.s5_d = g.dram_in("s5_d", [no, 128, 8])
        self.s5_glu_w = g.dram_in("s5_glu_w", [no, D, 2 * D])
        self.s5_glu_b = g.dram_in("s5_glu_b", [no, 2 * D])
        self.moe_router_w = g.dram_in("moe_router_w", [nl, D, 16])
        self.moe_w1 = g.dram_in("moe_w1", [nl, 16, D, 2 * D])
        self.moe_w3 = g.dram_in("moe_w3", [nl, 16, D, 2 * D])
        self.moe_w2 = g.dram_in("moe_w2", [nl, 16, 2 * D, D])
        self.final_norm_g = g.dram_in("final_norm_g", [D])
        self.out = g.dram_out("out", [NLAT, D])
        self.X = g.dram_tmp("X", [T, D])
        self.Fb = g.dram_tmp("Fb", [T, D], BF16)
        self.MOD = g.dram_tmp("MOD", [nl, 2, 6 * D])
        self.AFF = g.dram_tmp("AFF", [16, T])
        kb = self.kb
        self.arena = Arena(kb, cfg.get("arena_words", 40448))
        self.ident_f = kb.sbuf("ident_f", [128, 128], F32)
        self.ident_b = kb.sbuf("ident_b", [128, 128], BF16)
        self.IDXI = kb.sbuf("IDXI", [128, 80], I32)
        self.GVT = kb.sbuf("GVT", [128, 80], F32)
        self.ps = [kb.psum(f"ps{i}", [128, 1024], F32) for i in range(4)]
        self.psk = [f"ps{i}" for i in range(4)]
        kb.op("pool", lambda e: e.iota(self.ident_f[:], pattern=[[1, 128]], base=0, channel_multiplier=-1,
                                       allow_small_or_imprecise_dtypes=True), W=["ident_f"])
        g.kb.op("dve", lambda e: e.tensor_single_scalar(out=self.ident_f[:], in_=self.ident_f[:], scalar=0.0, op=ALU.is_equal),
                R=["ident_f"], W=["ident_f"])
        g.cp(self.ident_b[:], self.ident_f[:], R=["ident_f"], W=["ident_b"])

    def phase0(self):
        g = self
        A = self.arena
        A.reset()
        for i in range(4):
            r0 = i * (T // 4)
            g.ld(self.X[r0:r0 + T // 4, :], self.xin[r0:r0 + T // 4, :], R=[], W=[f"Xinit{i}"])
        cs, kcs = A.alloc("cs", 16)
        g.ld(cs, self.cin, R=[], W=[kcs])
        g.act(cs, cs, AF.Silu, R=[kcs], W=[kcs])
        cs3 = cs.rearrange("p (k c) -> p k c", c=2)
        mb, kmb = A.alloc("mb", 6144, parts=2)
        m2, km2 = A.alloc("m2", 6144, parts=2)
        stg = [A.alloc(f"stg{i}", 4096) for i in range(2)]
        for li in range(len(self.layers)):
            g.ld(mb, self.mod_b[li].partition_broadcast(2), R=[], W=[kmb])
            for n in range(12):
                s, ks = stg[n % 2]
                s3 = s.rearrange("p (k n) -> p k n", n=512)
                g.ld(s3, self.mod_w[li][:, n * 512:(n + 1) * 512].rearrange("(k p) n -> p k n", p=128), R=[], W=[ks])
                for k in range(8):
                    g.mm(self.ps[0][0:2, 0:512], cs3[:, k, :], s3[:, k, :], k == 0, k == 7, R=[kcs, ks], W=["ps0"])
                g.tt(m2[:, n * 512:(n + 1) * 512], self.ps[0][0:2, 0:512], mb[:, n * 512:(n + 1) * 512], ALU.add,
                     R=["ps0", kmb], W=[km2])
            g.st(self.MOD[li], m2, R=[km2], W=[f"MOD{li}"])

    def load_mod(self, li, which, isctx, dst, kdst):
        self.ld(dst, self.MOD[li, isctx, which * D:(which + 1) * D].partition_broadcast(128), R=[], W=[kdst])

    def load_bcast(self, vec_ap, dst, kdst, n=128):
        self.ld(dst, vec_ap.partition_broadcast(n), R=[], W=[kdst])

    def norm_tile(self, x, kx, ssq, kss, junk, kjunk):
        g = self
        g.act(junk, x, AF.Square, R=[kx], W=[kjunk, kss], accum_out=ssq)
        g.rsqrt_small(ssq, ssq, 1.0 / D, None, R=[kss], W=[kss])

    def post_stage(self, li, l, d_fn, alloc_extra=None):
        g = self
        A = self.arena
        G1 = [A.alloc(f"G1_{c}", D) for c in range(2)]
        A2 = [A.alloc(f"A2_{c}", D) for c in range(2)]
        B2 = [A.alloc(f"B2_{c}", D) for c in range(2)]
        ng, kng = A.alloc("ng", D)
        g.load_bcast(self.norm_g[li, 1], ng, kng)
        for c in range(2):
            g.load_mod(li, 2, c, *G1[c])
            g.load_mod(li, 4, c, *A2[c])
            g.load_mod(li, 3, c, *B2[c])
            g.stt(A2[c][0], A2[c][0], 1.0, ng, ALU.add, ALU.mult, R=[A2[c][1], kng], W=[A2[c][1]])
        xt = [A.alloc(f"xt{i}", D) for i in range(2)]
        xn = [A.alloc(f"xn{i}", D) for i in range(2)]
        tmp, ktmp = A.alloc("tmp", D)
        ff = [A.alloc(f"ff{i}", D) for i in range(2)]
        fb = [A.alloc(f"fb{i}", D, BF16) for i in range(2)]
        fT, kfT = A.alloc("fT32", D)
        wr, kwr = A.alloc("wr", 128)
        st_, kst = A.alloc("stat", 8)
        ex, kex = A.alloc("ex", 16)
        aft, kaft = A.alloc("AFFT", T, parts=16)
        g.ld(wr.rearrange("p (k e) -> p k e", e=16), self.moe_router_w[li].rearrange("(k p) e -> p k e", p=128), R=[], W=[kwr])
        wr3 = wr.rearrange("p (k e) -> p k e", e=16)
        psd, pst, psl = self.ps[0], self.ps[1], self.ps[2]
        def part1(tt):
            c = 1 if tt < 2 else 0
            b = tt % 2
            x, kx = xt[b]
            g.ld(x, self.X[tt * 128:(tt + 1) * 128, :], R=[f"X{tt}"], W=[kx])
            d, kd = d_fn(tt)
            g.tt(tmp, d, G1[c][0], ALU.mult, R=[kd, G1[c][1]], W=[ktmp])
            xo, kxo = xn[b]
            g.tt(xo, x, tmp, ALU.add, R=[kx, ktmp], W=[kxo])
        part1(0)
        for tt in range(NT):
            c = 1 if tt < 2 else 0
            b = tt % 2
            xo, kxo = xn[b]
            if tt + 1 < NT:
                part1(tt + 1)
            g.st(self.X[tt * 128:(tt + 1) * 128, :], xo, R=[kxo], W=[f"X{tt}"])
            ssq = st_[:, 0:1]
            g.act(fT, xo, AF.Square, R=[kxo], W=[kfT, kst], accum_out=ssq)
            g.rsqrt_small(ssq, ssq, 1.0 / D, None, R=[kst], W=[kst])
            f, kf = ff[b]
            g.stt(f, xo, ssq, A2[c][0], ALU.mult, ALU.mult, R=[kxo, kst, A2[c][1]], W=[kf])
            g.tt(f, f, B2[c][0], ALU.add, R=[kf, B2[c][1]], W=[kf])
            fbt, kfb = fb[b]
            g.cp(fbt, f, R=[kf], W=[kfb], eng="act")
            g.st(self.Fb[tt * 128:(tt + 1) * 128, :], fbt, R=[kfb], W=[f"Fb{tt}"])
            for k in range(8):
                g.tr(pst[:, k * 128:(k + 1) * 128], f[:, k * 128:(k + 1) * 128], self.ident_f[:], R=[kf, "ident_f"], W=["ps1"])
            g.cp(fT, pst[:, :], R=["ps1"], W=[kfT], eng="act")
            for k in range(8):
                g.mm(psl[:, 0:16], fT[:, k * 128:(k + 1) * 128], wr3[:, k, :], k == 0, k == 7, R=[kfT, kwr], W=["ps2"])
            mx = st_[:, 1:2]
            sm = st_[:, 2:3]
            g.red(mx, psl[:, 0:16], ALU.max, R=["ps2"], W=[kst])
            g.ts(mx, mx, -1.0, None, ALU.mult, None, R=[kst], W=[kst])
            g.act(ex, psl[:, 0:16], AF.Exp, R=["ps2", kst], W=[kex, kst], bias=mx, accum_out=sm)
            g.recip(sm, sm, R=[kst], W=[kst])
            g.ts(ex, ex, sm, None, ALU.mult, None, R=[kex, kst], W=[kex])
            g.tr(psl[0:16, 512:640], ex, self.ident_f[:], R=[kex, "ident_f"], W=["ps2"])
            g.cp(aft[:, tt * 128:(tt + 1) * 128], psl[0:16, 512:640], R=["ps2"], W=[kaft], eng="act")
        g.st(self.AFF, aft, R=[kaft], W=["AFF"])

    def moe_topk(self):
        g = self
        A = self.arena
        A.reset()
        af, kaf = A.alloc("af", T, parts=16)
        wk = [A.alloc(f"wk{i}", NLAT, parts=16) for i in range(2)]
        mxv, kmx = A.alloc("mxv", 544, parts=16)
        ixv, kix = A.alloc("ixv", 544, U32, parts=16)
        idf, kidf = A.alloc("idf", 544, parts=16)
        tf, ktf = A.alloc("tf", 80)
        g.ld(af, self.AFF, R=["AFF"], W=[kaf])

        def rounds(src0, ksrc0, n, col0, nr):
            src, ksrc = src0, ksrc0
            for r in range(nr):
                c0 = col0 + 8 * r
                g.kb.op("dve", lambda e, c0=c0, src=src: e.max(out=mxv[:, c0:c0 + 8], in_=src), R=[ksrc], W=[kmx])
                g.kb.op("dve", lambda e, c0=c0, src=src: e.max_index(out=ixv[:, c0:c0 + 8], in_max=mxv[:, c0:c0 + 8], in_values=src),
                        R=[ksrc, kmx], W=[kix])
                if r < nr - 1:
                    dst, kdst = wk[r % 2]
                    dstv = dst[:, 0:n]
                    g.kb.op("dve", lambda e, c0=c0, src=src, dstv=dstv: e.match_replace(out=dstv, in_to_replace=mxv[:, c0:c0 + 8],
                                                                                         in_values=src, imm_value=-1.0),
                            R=[ksrc, kmx], W=[kdst])
                    src, ksrc = dstv, kdst
        rounds(af[:, NCTX:T], kaf, NLAT, 0, 64)
        rounds(af[:, 0:NCTX], kaf, NCTX, 512, 4)
        g.cp(idf, ixv, R=[kix], W=[kidf])
        g.ts(idf[:, 0:512], idf[:, 0:512], float(NCTX), None, ALU.add, None, R=[kidf], W=[kidf])
        psl = self.ps[2]
        for sc in range(5):
            n = 128 if sc < 4 else 32
            g.tr(psl[0:n, 0:16], idf[:, sc * 128:sc * 128 + n], self.ident_f[0:16, 0:16], R=[kidf, "ident_f"], W=["ps2"])
            g.cp(self.IDXI[0:n, sc * 16:(sc + 1) * 16], psl[0:n, 0:16], R=["ps2"], W=["IDXI"])
            g.tr(psl[0:n, 16:32], mxv[:, sc * 128:sc * 128 + n], self.ident_f[0:16, 0:16], R=[kmx, "ident_f"], W=["ps2"])
            g.cp(self.GVT[0:n, sc * 16:(sc + 1) * 16], psl[0:n, 16:32], R=["ps2"], W=["GVT"])

    def moe_experts(self, li):
        g = self
        A = self.arena
        A.reset()
        W1b, _ = A.alloc("W1b", 8 * 2048, BF16)
        W3b, _ = A.alloc("W3b", 8 * 2048, BF16)
        W2b, _ = A.alloc("W2b", 16 * 1024, BF16)
        W1v = W1b.rearrange("p (k f) -> p k f", f=2048)
        W3v = W3b.rearrange("p (k f) -> p k f", f=2048)
        W2v = W2b.rearrange("p (k f) -> p k f", f=1024)
        XSt, kXS = A.alloc("XS", 5 * 1024, BF16)
        XS = XSt.rearrange("p (s d) -> p s d", d=1024)
        HIDt, kHID = A.alloc("HID", 16 * 544, BF16)
        HID = HIDt.rearrange("p (f s) -> p f s", s=544)
        XST, kXST = A.alloc("XST", 8 * 544, BF16)
        XSTv = XST.rearrange("p (k s) -> p k s", s=544)
        SIL = [A.alloc(f"sil{i}", 544) for i in range(2)]
        YSb = [A.alloc(f"YS{i}", 1024) for i in range(2)]
        G2 = [A.alloc(f"G2_{c}", D) for c in range(2)]
        for c in range(2):
            g.load_mod(li, 5, c, *G2[c])

        def load13(e):
            for (wsrc, wv, nm) in ((self.moe_w1, W1v, "W1"), (self.moe_w3, W3v, "W3")):
                for k in range(8):
                    g.ld(wv[:, k, :], wsrc[li, e, k * 128:(k + 1) * 128, :], R=[], W=[f"{nm}.{k}"], q="pool")

        def load2(e):
            for fc in range(0, 16, 2):
                g.ld(W2v[:, fc:fc + 2, :], self.moe_w2[li, e, fc * 128:(fc + 2) * 128, :].rearrange("(a p) n -> p a n", p=128),
                     R=[], W=[f"W2.{fc}", f"W2.{fc + 1}"], q="pool")

        def gather(e):
            for sc in range(5):
                n = 128 if sc < 4 else 32
                col = sc * 16 + e
                g.kb.dma("pool", lambda en, n=n, sc=sc, col=col: en.indirect_dma_start(
                    out=XS[0:n, sc, :], out_offset=None, in_=self.Fb,
                    in_offset=bass.IndirectOffsetOnAxis(ap=self.IDXI[0:n, col:col + 1], axis=0)),
                    R=["IDXI", "Fball"], W=[kXS])

        def transposes(e):
            pT = self.ps[3].bitcast(BF16)
            for sc in range(5):
                n = 128 if sc < 4 else 32
                for k in range(8):
                    g.tr(pT[:, k * 128:k * 128 + n], XS[0:n, sc, k * 128:(k + 1) * 128], self.ident_b[0:n, 0:n],
                         R=[kXS, "ident_b"], W=["ps3"])
                g.cp(XSTv[:, :, sc * 128:sc * 128 + n], pT[:, 0:1024].rearrange("p (k s) -> p k s", s=128)[:, :, 0:n],
                     R=["ps3"], W=[kXST], eng="act" if sc % 2 == 0 else "dve")

        def hidden(e):
            for fc in range(16):
                p1, k1 = (self.ps[0], "ps0") if fc % 2 == 0 else (self.ps[2], "ps2")
                p3, k3 = (self.ps[1], "ps1") if fc % 2 == 0 else (self.ps[3], "ps3")
                for (wv, nm, pp, kp) in ((W1v, "W1", p1, k1), (W3v, "W3", p3, k3)):
                    for (n0, n1) in ((0, 512), (512, 544)):
                        for k in range(8):
                            g.mm(pp[:, n0:n1], wv[:, k, fc * 128:(fc + 1) * 128], XSTv[:, k, n0:n1], k == 0, k == 7,
                                 R=[f"{nm}.{k}", kXST], W=[kp])
                sil, ksil = SIL[fc % 2]
                g.act(sil, p1[:, 0:544], AF.Silu, R=[k1], W=[ksil])
                g.tt(HID[:, fc, :], sil, p3[:, 0:544], ALU.mult, R=[ksil, k3], W=[kHID])

        ysi = [0]

        def outscatter(e):
            for sc in range(5):
                n = 128 if sc < 4 else 32
                c = 0 if sc < 4 else 1
                col = sc * 16 + e
                py, ky = (self.ps[0], "ps0") if sc % 2 == 0 else (self.ps[1], "ps1")
                for hf in range(2):
                    for fc in range(16):
                        g.mm(py[0:n, hf * 512:(hf + 1) * 512], HID[:, fc, sc * 128:sc * 128 + n], W2v[:, fc, hf * 512:(hf + 1) * 512],
                             fc == 0, fc == 15, R=[kHID, f"W2.{fc}"], W=[ky])
                YS, kYS = YSb[ysi[0] % 2]
                ysi[0] += 1
                g.stt(YS[0:n, :], py[0:n, :], self.GVT[0:n, col:col + 1], G2[c][0][0:n, :], ALU.mult, ALU.mult,
                      R=[ky, "GVT", G2[c][1]], W=[kYS])
                g.kb.dma("pool", lambda en, n=n, col=col, YS=YS: en.indirect_dma_start(
                    out=self.X, out_offset=bass.IndirectOffsetOnAxis(ap=self.IDXI[0:n, col:col + 1], axis=0),
                    in_=YS[0:n, :], in_offset=None, compute_op=ALU.add),
                    R=["IDXI", kYS, "Xsc"], W=["Xsc"])

        gather(0)
        load13(0)
        load2(0)
        transposes(0)
        for e in range(16):
            if e + 1 < 16:
                gather(e + 1)
            hidden(e)
            if e + 1 < 16:
                load13(e + 1)
                transposes(e + 1)
            outscatter(e)
            if e + 1 < 16:
                load2(e + 1)

    def final_norm(self):
        g = self
        A = self.arena
        A.reset()
        fg, kfg = A.alloc("fg", D)
        g.load_bcast(self.final_norm_g, fg, kfg)
        xt = [A.alloc(f"xt{i}", D) for i in range(2)]
        ot = [A.alloc(f"ot{i}", D) for i in range(2)]
        junk, kj = A.alloc("junk", D)
        st_, kst = A.alloc("stat", 8)
        for tt in range(2, NT):
            b = tt % 2
            x, kx = xt[b]
            o, ko = ot[b]
            g.ld(x, self.X[tt * 128:(tt + 1) * 128, :], R=[f"X{tt}"], W=[kx])
            g.norm_tile(x, kx, st_[:, 0:1], kst, junk, kj)
            g.stt(o, x, st_[:, 0:1], fg, ALU.mult, ALU.mult, R=[kx, kst, kfg], W=[ko])
            g.st(self.out[(tt - 2) * 128:(tt - 1) * 128, :], o, R=[ko], W=[f"out{tt}"])

    def build(self):
        g = self
        cfg = self.cfg
        self.phase0()
        if cfg.get("mixer", True) and any(l % 2 == 0 for l in self.layers):
            self.setup_rope()
        for li, l in enumerate(self.layers):
            if cfg.get("mixer", True):
                if l % 2 == 0:
                    self.even_mixer(li, l)
                else:
                    self.s5_mixer(li, l)
            else:
                A = self.arena
                A.reset()
                z, kz = A.alloc("zero", D)
                g.memset(z, 0.0, W=[kz])
                self.post_stage(li, l, lambda tt: (z, kz))
            if cfg.get("moe", True):
                self.moe_topk()
                self.moe_experts(li)
        self.final_norm()
        self.kb.emit()
        return self.nc


def bc(ap, axis, n):
    a = ap.unsqueeze(axis)
    shp = list(a.shape)
    shp[axis] = n
    return a.broadcast_to(shp)


def setup_rope(self):
    g = self
    kb = self.kb
    self.cosT = kb.sbuf("cosT", [128, 1024], F32)
    self.sinT = kb.sbuf("sinT", [128, 1024], F32)
    A = self.arena
    A.reset()
    fi, kfi = A.alloc("fi", 16)
    inv, kinv = A.alloc("inv", 16)
    pidx, kp = A.alloc("pidx", 1)
    ph, kph = A.alloc("ph", 1)
    colv, kcol = A.alloc("colv", 1)
    rowv, krow = A.alloc("rowv", 32)
    ang, kang = A.alloc("ang", 1024)
    ri, kri = A.alloc("ri", 1024, I32)
    ab, kab = A.alloc("ab", 1024)
    kb.op("pool", lambda e: e.iota(fi, pattern=[[1, 16]], base=0, channel_multiplier=0, allow_small_or_imprecise_dtypes=True), W=[kfi])
    g.act(inv, fi, AF.Exp, R=[kfi], W=[kinv], scale=-(2.0 / 32.0) * math.log(10000.0))
    kb.op("pool", lambda e: e.iota(pidx, pattern=[[0, 1]], base=0, channel_multiplier=1, allow_small_or_imprecise_dtypes=True), W=[kp])
    g.ts(ph, pidx, 64.0, None, ALU.is_ge, None, R=[kp], W=[kph])
    g.stt(colv, ph, -64.0, pidx, ALU.mult, ALU.add, R=[kph, kp], W=[kcol])
    kb.op("pool", lambda e: e.iota(rowv, pattern=[[2, 32]], base=0, channel_multiplier=0, allow_small_or_imprecise_dtypes=True), W=[krow])
    g.ts(rowv, rowv, ph, None, ALU.add, None, R=[krow, kph], W=[krow])
    a4 = ang.rearrange("p (t a i) -> p t a i", a=2, i=16)
    g.tt(a4[:, :, 0, :], bc(rowv, 2, 16), bc(inv, 1, 32), ALU.mult, R=[krow, kinv], W=[kang])
    g.ts(a4[:, :, 1, :], bc(inv, 1, 32), colv, None, ALU.mult, None, R=[kinv, kcol, kang], W=[kang])
    g.ts(ang, ang, 1.0 / TWO_PI, None, ALU.mult, None, R=[kang], W=[kang])
    g.cp(ri, ang, R=[kang], W=[kri])
    g.tt(ang, ang, ri, ALU.subtract, R=[kang, kri], W=[kang])
    g.act(self.sinT[:], ang, AF.Sin, R=[kang], W=["sinT"], scale=TWO_PI)
    g.act(ab, ang, AF.Abs, R=[kang], W=[kab])
    hp, khp = A.alloc("halfpi", 1)
    g.memset(hp, math.pi / 2.0, W=[khp])
    g.act(self.cosT[:], ab, AF.Sin, R=[kab, khp], W=["cosT"], scale=-TWO_PI, bias=hp)


def rope(self, src, ksrc, dst, kdst, H, tt, tmps):
    g = self
    t = tt - 2
    sv = src.rearrange("p (h a s i) -> p h a s i", a=2, s=2, i=16)
    dv = dst.rearrange("p (h a s i) -> p h a s i", a=2, s=2, i=16)
    x1, x2 = sv[:, :, :, 0, :], sv[:, :, :, 1, :]
    cos = bc(self.cosT[:, t * 32:(t + 1) * 32].rearrange("p (a i) -> p a i", i=16), 1, H)
    sin = bc(self.sinT[:, t * 32:(t + 1) * 32].rearrange("p (a i) -> p a i", i=16), 1, H)
    (t1, k1), (t2, k2), (t3, k3), (t4, k4) = tmps
    v = lambda a: a[:, 0:H * 32].rearrange("p (h a i) -> p h a i", a=2, i=16)
    g.tt(v(t1), x1, cos, ALU.mult, R=[ksrc, "cosT"], W=[k1])
    g.tt(v(t2), x2, sin, ALU.mult, R=[ksrc, "sinT"], W=[k2])
    g.tt(dv[:, :, :, 0, :], v(t1), v(t2), ALU.subtract, R=[k1, k2], W=[kdst])
    g.tt(v(t3), x1, sin, ALU.mult, R=[ksrc, "sinT"], W=[k3], eng="pool")
    g.tt(v(t4), x2, cos, ALU.mult, R=[ksrc, "cosT"], W=[k4], eng="pool")
    g.tt(dv[:, :, :, 1, :], v(t3), v(t4), ALU.add, R=[k3, k4], W=[kdst], eng="pool")


def even_mixer(self, li, l):
    g = self
    kb = self.kb
    A = self.arena
    ei = self.ev_idx[l]
    if not hasattr(self, "QT"):
        self.QT = g.dram_tmp("QT", [1664, T], BF16)
        self.TMd = g.dram_tmp("TMd", [T, 2176], BF16)
        self.OF = g.dram_tmp("OF", [T, 512])
        self.MIXT = g.dram_tmp("MIXT", [1024, T], BF16)
    QTv = self.QT.rearrange("(c p) t -> p c t", p=128)
    MIXTv = self.MIXT.rearrange("(c p) t -> p c t", p=128)
    A.reset()
    Wb, _ = A.alloc("Wb", 8 * 2816, BF16)
    Wv = Wb.rearrange("p (k n) -> p k n", n=2816)
    stg = [A.alloc(f"stg{i}", 1408) for i in range(2)]
    ce = ["pool", "act", "dve"]
    for k in range(8):
        for hf in range(2):
            s, ks = stg[(2 * k + hf) % 2]
            g.ld(s, self.mix_in_w[ei, k * 128:(k + 1) * 128, hf * 1408:(hf + 1) * 1408], R=[], W=[ks])
            g.cp(Wv[:, k, hf * 1408:(hf + 1) * 1408], s, R=[ks], W=["Wb"], eng=ce[(2 * k + hf) % 3])
    A1 = [A.alloc(f"A1_{c}", D) for c in range(2)]
    B1 = [A.alloc(f"B1_{c}", D) for c in range(2)]
    ng, kng = A.alloc("ng", D)
    g.load_bcast(self.norm_g[li, 0], ng, kng)
    for c in range(2):
        g.load_mod(li, 1, c, *A1[c])
        g.load_mod(li, 0, c, *B1[c])
        g.stt(A1[c][0], A1[c][0], 1.0, ng, ALU.add, ALU.mult, R=[A1[c][1], kng], W=[A1[c][1]])
    gq, kgq = A.alloc("gq", 64)
    gk, kgk = A.alloc("gk", 64)
    g.load_bcast(self.qk_norm_g[ei, 0:64], gq, kgq)
    g.load_bcast(self.qk_norm_g[ei, 64:128], gk, kgk)
    lgt, klg = A.alloc("lgt", 16)
    g.load_bcast(self.ret_log_rate[ei], lgt, klg)
    g.act(lgt, lgt, AF.Exp, R=[klg], W=[klg])
    g.ts(lgt, lgt, -1.0, None, ALU.mult, None, R=[klg], W=[klg])
    pidx, kp = A.alloc("pidx", 1)
    pr_, kpr = A.alloc("prev", 1)
    kb.op("pool", lambda e: e.iota(pidx, pattern=[[0, 1]], base=0, channel_multiplier=1, allow_small_or_imprecise_dtypes=True), W=[kp])
    g.ts(pr_, pidx, -1.0, 127.0, ALU.mult, ALU.add, R=[kp], W=[kpr])
    DK, kDK = A.alloc("DK", 16)
    g.ts(DK[:, 0:8], lgt[:, 0:8], pr_, None, ALU.mult, None, R=[klg, kpr], W=[kDK])
    g.ts(DK[:, 8:16], lgt[:, 8:16], pidx, None, ALU.mult, None, R=[klg, kp, kDK], W=[kDK])
    g.act(DK, DK, AF.Exp, R=[kDK], W=[kDK])
    xt = [A.alloc(f"xt{i}", D) for i in range(2)]
    tmp, ktmp = A.alloc("tmp", D)
    hb, khb = A.alloc("hb", D, BF16)
    hT, khT = A.alloc("hT", D, BF16)
    P, kP = A.alloc("P", 2816)
    sqt, ksq = A.alloc("sqt", 640)
    st_, kst = A.alloc("stat", 16)
    QKb = [A.alloc(f"QKb{i}", 1664, BF16) for i in range(2)]
    TM = [A.alloc(f"TM{i}", 2176, BF16) for i in range(2)]
    QTs = [A.alloc(f"QTs{i}", 1664, BF16) for i in range(2)]
    tmps = [A.alloc(f"rt{i}", 512) for i in range(4)]
    psT = self.ps[3].bitcast(BF16)
    for tt in range(NT):
        c = 1 if tt < 2 else 0
        b = tt % 2
        x, kx = xt[b]
        g.ld(x, self.X[tt * 128:(tt + 1) * 128, :], R=[f"X{tt}"], W=[kx])
        ssq = st_[:, 0:1]
        g.act(tmp, x, AF.Square, R=[kx], W=[ktmp, kst], accum_out=ssq)
        g.rsqrt_small(ssq, ssq, 1.0 / D, None, R=[kst], W=[kst])
        g.stt(tmp, x, ssq, A1[c][0], ALU.mult, ALU.mult, R=[kx, kst, A1[c][1]], W=[ktmp])
        g.tt(hb, tmp, B1[c][0], ALU.add, R=[ktmp, B1[c][1]], W=[khb])
        for k in range(8):
            g.tr(psT[:, k * 128:(k + 1) * 128], hb[:, k * 128:(k + 1) * 128], self.ident_b[:], R=[khb, "ident_b"], W=["ps3"])
        g.cp(hT, psT[:, 0:1024], R=["ps3"], W=[khT], eng="act")
        for (n0, w, pp, kp_, off) in ((0, 512, 0, "ps0", 0), (512, 512, 0, "ps0", 512), (1024, 512, 1, "ps1", 0),
                                      (1536, 512, 1, "ps1", 512), (2048, 512, 2, "ps2", 0), (2560, 256, 2, "ps2", 512)):
            for k in range(8):
                g.mm(self.ps[pp][:, off:off + w], hT[:, k * 128:(k + 1) * 128], Wv[:, k, n0:n0 + w], k == 0, k == 7,
                     R=[khT, "Wb"], W=[kp_])
        g.cp(P[:, 0:1024], self.ps[0][:, :], R=["ps0"], W=[kP], eng="act")
        g.cp(P[:, 1024:2048], self.ps[1][:, :], R=["ps1"], W=[kP], eng="dve")
        g.cp(P[:, 2048:2816], self.ps[2][:, 0:768], R=["ps2"], W=[kP], eng="act")
        qa = P[:, 2048:2688]
        qa3 = qa.rearrange("p (h d) -> p h d", d=64)
        g.act(sqt, qa, AF.Square, R=[kP], W=[ksq])
        ss10 = st_[:, 4:14]
        g.red(ss10, sqt.rearrange("p (h d) -> p h d", d=64), ALU.add, R=[ksq], W=[kst])
        g.rsqrt_small(ss10, ss10, 1.0 / 64.0, None, R=[kst], W=[kst])
        g.tt(qa3, qa3, bc(ss10, 2, 64), ALU.mult, R=[kP, kst], W=[kP])
        g.tt(qa3[:, 0:8, :], qa3[:, 0:8, :], bc(gq, 1, 8), ALU.mult, R=[kP, kgq], W=[kP])
        g.tt(qa3[:, 8:10, :], qa3[:, 8:10, :], bc(gk, 1, 2), ALU.mult, R=[kP, kgk], W=[kP])
        g.ts(P[:, 512:1024], P[:, 512:1024], 0.125, None, ALU.mult, None, R=[kP], W=[kP], eng="pool")
        qk, kqk = QKb[b]
        if c == 0:
            rope(self, P[:, 0:1024], kP, qk[:, 0:1024], kqk, 16, tt, tmps)
            rope(self, P[:, 2048:2688], kP, qk[:, 1024:1664], kqk, 10, tt, tmps)
        else:
            g.cp(qk[:, 0:1024], P[:, 0:1024], R=[kP], W=[kqk], eng="dve")
            g.cp(qk[:, 1024:1664], P[:, 2048:2688], R=[kP], W=[kqk], eng="pool")
        tm, ktm = TM[b]
        rk3 = qk[:, 512:1024].rearrange("p (h d) -> p h d", d=64)
        g.tt(tm[:, 0:512].rearrange("p (h d) -> p h d", d=64), rk3, bc(DK[:, 0:8], 2, 64), ALU.mult, R=[kqk, kDK], W=[ktm])
        g.tt(tm[:, 512:1024].rearrange("p (h d) -> p h d", d=64), rk3, bc(DK[:, 8:16], 2, 64), ALU.mult, R=[kqk, kDK], W=[ktm], eng="pool")
        g.cp(tm[:, 1024:1536], P[:, 1024:1536], R=[kP], W=[ktm], eng="pool")
        g.act(tm[:, 1536:2048], P[:, 1536:2048], AF.Silu, R=[kP], W=[ktm])
        g.cp(tm[:, 2048:2176], P[:, 2688:2816], R=[kP], W=[ktm], eng="act")
        g.st(self.TMd[tt * 128:(tt + 1) * 128, :], tm, R=[ktm], W=[f"TMd{tt}"])
        for ch in range(13):
            g.tr(psT[:, ch * 128:(ch + 1) * 128], qk[:, ch * 128:(ch + 1) * 128], self.ident_b[:], R=[kqk, "ident_b"], W=["ps3"])
        qs, kqs = QTs[b]
        g.cp(qs, psT[:, 0:1664], R=["ps3"], W=[kqs], eng="act")
        g.st(QTv[:, :, tt * 128:(tt + 1) * 128], qs.rearrange("p (c t) -> p c t", t=128), R=[kqs], W=[f"QT{tt}"])

    if self.cfg.get('even_stop') == 1:
        return
    A.reset()
    lgt, klg = A.alloc("lgt", 16)
    g.load_bcast(self.ret_log_rate[ei], lgt, klg)
    g.act(lgt, lgt, AF.Exp, R=[klg], W=[klg])
    g.ts(lgt, lgt, -1.0, None, ALU.mult, None, R=[klg], W=[klg])
    diff, kdf = A.alloc("diff", 128)
    ndiff, kndf = A.alloc("ndiff", 128)
    mk, kmk = A.alloc("mk", 128)
    i1, ki1 = A.alloc("i1", 128)
    i2, ki2 = A.alloc("i2", 128)
    kb.op("pool", lambda e: e.iota(diff, pattern=[[1, 128]], base=0, channel_multiplier=-1, allow_small_or_imprecise_dtypes=True), W=[kdf])
    kb.op("pool", lambda e: e.iota(ndiff, pattern=[[-1, 128]], base=0, channel_multiplier=1, allow_small_or_imprecise_dtypes=True), W=[kndf])
    kb.op("pool", lambda e: e.iota(i1, pattern=[[1, 128]], base=1, channel_multiplier=0, allow_small_or_imprecise_dtypes=True), W=[ki1])
    kb.op("pool", lambda e: e.iota(i2, pattern=[[-1, 128]], base=128, channel_multiplier=0, allow_small_or_imprecise_dtypes=True), W=[ki2])
    DT = [A.alloc(f"DT{d}", 1024) for d in range(2)]
    DQ = [A.alloc(f"DQ{d}", 512) for d in range(2)]
    dc = [A.alloc(f"dc{d}", 4) for d in range(2)]
    lgh = [A.alloc(f"lgh{d}", 4) for d in range(2)]
    for d in range(2):
        src_d, ksd = (diff, kdf) if d == 0 else (ndiff, kndf)
        g.ts(mk, diff, 0.0, None, ALU.is_ge if d == 0 else ALU.is_lt, None, R=[kdf], W=[kmk])
        dt_, kdt = DT[d]
        for h in range(8):
            sl = (h % 2) * 4 + h // 2
            g.act(dt_[:, sl * 128:(sl + 1) * 128], src_d, AF.Exp, R=[ksd, klg], W=[kdt], scale=lgt[:, d * 8 + h:d * 8 + h + 1])
        g.tt(dt_.rearrange("p (h i) -> p h i", i=128), dt_.rearrange("p (h i) -> p h i", i=128), bc(mk, 1, 8), ALU.mult,
             R=[kdt, kmk], W=[kdt])
        lh, klh = lgh[d]
        lsel = lgt[:, d * 8:(d + 1) * 8].rearrange("p (q two) -> p q two", two=2)
        g.cp(lh[0:64, :], lsel[0:64, :, 0], R=[klg], W=[klh])
        g.cp(lh[64:128, :], lsel[64:128, :, 1], R=[klg], W=[klh])
        dq, kdq = DQ[d]
        isrc, kis = (i1, ki1) if d == 0 else (i2, ki2)
        for q in range(4):
            g.act(dq[:, q * 128:(q + 1) * 128], isrc, AF.Exp, R=[kis, klh], W=[kdq], scale=lh[:, q:q + 1])
        g.act(dc[d][0], lh, AF.Exp, R=[klh], W=[dc[d][1]], scale=128.0)
    gng, kgng = A.alloc("gng", 512)
    g.load_bcast(self.ret_gn_g[ei], gng, kgng)
    QKT = [A.alloc(f"QKT{i}", 1024, BF16) for i in range(2)]
    TMt = [A.alloc(f"TMt{i}", 2176, BF16) for i in range(2)]
    PT, kPT = A.alloc("PT", 1024, BF16)
    qtl, kqtl = A.alloc("qtl", 512, BF16)
    S, kS = A.alloc("S", 256)
    Sb, kSb = A.alloc("Sb", 256, BF16)
    o32 = [A.alloc(f"o32_{i}", 512) for i in range(2)]
    oft = [A.alloc(f"of{i}", 512) for i in range(2)]
    sq2, ksq2 = A.alloc("sq2", 512)
    gs, kgs = A.alloc("gs", 32)
    mr = [A.alloc(f"mr{i}", 512, BF16) for i in range(2)]
    mrT = [A.alloc(f"mrT{i}", 512, BF16) for i in range(2)]
    psS, psO = self.ps[0], self.ps[1]
    S3 = S.rearrange("p (q e) -> p q e", e=64)
    if self.cfg.get('even_stop') == 21:
        return
    for d in range(2):
        if d == 1 and self.cfg.get('even_stop') == 22:
            return
        order = list(range(NT)) if d == 0 else [1, 0] + list(range(NT - 1, 1, -1))
        g.memset(S, 0.0, W=[kS])
        g.memset(Sb, 0.0, W=[kSb])
        koff = 0 if d == 0 else 512
        for n_i, tt in enumerate(order):
            b = n_i % 2
            qkt, kq = QKT[b]
            tm, ktm = TMt[b]
            qk3 = qkt.rearrange("p (c t) -> p c t", t=128)
            g.ld(qk3, QTv[:, 0:8, tt * 128:(tt + 1) * 128], R=[f"QT{tt}"], W=[kq])
            g.ld(tm, self.TMd[tt * 128:(tt + 1) * 128, :], R=[f"TMd{tt}"], W=[ktm])
            g.tt(qtl, qkt[:, 0:512], DQ[d][0], ALU.mult, R=[kq, DQ[d][1]], W=[kqtl])
            for h in range(8):
                par, pr = h % 2, h // 2
                sl = par * 4 + pr
                g.mm(psS[:, sl * 128:(sl + 1) * 128], qk3[par * 64:(par + 1) * 64, 4 + pr, :], qk3[par * 64:(par + 1) * 64, pr, :],
                     True, True, R=[kq], W=["ps0"])
            g.tt(PT, psS[:, :], DT[d][0], ALU.mult, R=["ps0", DT[d][1]], W=[kPT])
            for h in range(8):
                par, pr = h % 2, h // 2
                sl = par * 4 + pr
                g.mm(psO[:, h * 64:(h + 1) * 64], PT[:, sl * 128:(sl + 1) * 128], tm[:, 1024 + h * 64:1024 + (h + 1) * 64],
                     True, bool(self.cfg.get('no_acc')), R=[kPT, ktm], W=["ps1"])
                if self.cfg.get('no_acc'):
                    continue
                g.mm(psO[:, h * 64:(h + 1) * 64], qtl[par * 64:(par + 1) * 64, pr * 128:(pr + 1) * 128],
                     Sb[par * 64:(par + 1) * 64, pr * 64:(pr + 1) * 64], False, True, R=[kqtl, kSb], W=["ps1"])
            for pr in range(4):
                g.mm(psO[:, 512 + pr * 128:512 + (pr + 1) * 128], tm[:, koff + pr * 128:koff + (pr + 1) * 128],
                     tm[:, 1024 + pr * 128:1024 + (pr + 1) * 128], True, True, R=[ktm], W=["ps1u"])
            g.tt(S3, S3, bc(dc[d][0], 2, 64), ALU.mult, R=[kS, dc[d][1]], W=[kS])
            U3 = psO[:, 512:1024].rearrange("p (q e) -> p q e", e=128)
            g.tt(S3[0:64], S3[0:64], U3[0:64, :, 0:64], ALU.add, R=[kS, "ps1u"], W=[kS])
            g.tt(S3[64:128], S3[64:128], U3[64:128, :, 64:128], ALU.add, R=[kS, "ps1u"], W=[kS])
            g.cp(Sb, S, R=[kS], W=[kSb], eng="act")
            o, ko = o32[b]
            if d == 0:
                g.cp(o, psO[:, 0:512], R=["ps1"], W=[ko], eng="act")
                g.st(self.OF[tt * 128:(tt + 1) * 128, :], o, R=[ko], W=[f"OF{tt}"])
            else:
                of, kof = oft[b]
                g.ld(of, self.OF[tt * 128:(tt + 1) * 128, :], R=[f"OF{tt}"], W=[kof])
                g.tt(o, psO[:, 0:512], of, ALU.add, R=["ps1", kof], W=[ko])
                o3 = o.rearrange("p (h e) -> p h e", e=64)
                s1, s2, mean, msq, var = gs[:, 0:8], gs[:, 8:16], gs[:, 16:24], gs[:, 24:32], gs[:, 8:16]
                g.red(s1, o3, ALU.add, R=[ko], W=[kgs])
                g.act(sq2, o, AF.Square, R=[ko], W=[ksq2])
                g.red(s2, sq2.rearrange("p (h e) -> p h e", e=64), ALU.add, R=[ksq2], W=[kgs])
                g.ts(mean, s1, 1.0 / 64.0, None, ALU.mult, None, R=[kgs], W=[kgs])
                g.tt(msq, mean, mean, ALU.mult, R=[kgs], W=[kgs])
                g.stt(var, s2, 1.0 / 64.0, msq, ALU.mult, ALU.subtract, R=[kgs], W=[kgs])
                g.rsqrt_small(var, var, 1.0, None, R=[kgs], W=[kgs])
                g.tt(o3, o3, bc(mean, 2, 64), ALU.subtract, R=[ko, kgs], W=[ko])
                g.tt(o3, o3, bc(var, 2, 64), ALU.mult, R=[ko, kgs], W=[ko])
                g.tt(o, o, gng, ALU.mult, R=[ko, kgng], W=[ko], eng="pool")
                m_, km = mr[b]
                g.tt(m_, o, tm[:, 1536:2048], ALU.mult, R=[ko, ktm], W=[km], eng="pool")
                pT2 = self.ps[3].bitcast(BF16)
                for ch in range(4):
                    g.tr(pT2[:, ch * 128:(ch + 1) * 128], m_[:, ch * 128:(ch + 1) * 128], self.ident_b[:], R=[km, "ident_b"], W=["ps3"])
                mt, kmt = mrT[b]
                g.cp(mt, pT2[:, 0:512], R=["ps3"], W=[kmt], eng="act")
                g.st(MIXTv[:, 0:4, tt * 128:(tt + 1) * 128], mt.rearrange("p (c t) -> p c t", t=128), R=[kmt], W=[f"MIXT{tt}"])

    if self.cfg.get('even_stop') == 2:
        return
    A.reset()
    Kstd, kKs = A.alloc("Kstd", T, BF16)
    Kswp, kKw = A.alloc("Kswp", T, BF16)
    g.ld(Kstd, self.QT[1536:1664, :], R=["QTall"], W=[kKs])
    g.ld(Kswp[0:64, :], self.QT[1600:1664, :], R=["QTall"], W=[kKw])
    g.ld(Kswp[64:128, :], self.QT[1536:1600, :], R=["QTall"], W=[kKw])
    VA, kVA = A.alloc("VA", NT * 130, BF16)
    VA4 = VA.rearrange("p (t kv d) -> p t kv d", kv=2, d=65)
    g.memset(VA4[:, :, :, 64:65], 1.0, W=[kVA])
    for t in range(NT):
        g.ld(VA4[:, t, :, 0:64], self.TMd[t * 128:(t + 1) * 128, 2048:2176].rearrange("p (kv d) -> p kv d", kv=2), R=["TMdall"], W=[kVA])
    onesf, kon = A.alloc("onesf", 64)
    g.memset(onesf, 1.0, W=[kon])
    Qb = [A.alloc(f"Qb{i}", 2048, BF16) for i in range(2)]
    PTa = [A.alloc(f"PTa{i}", 512, BF16) for i in range(2)]
    rd, krd = A.alloc("rd", 512)
    rdb, krdb = A.alloc("rdb", 512)
    aT = [A.alloc(f"aT{i}", 512, BF16) for i in range(2)]
    blocks = [(0, 256, [0, 1])] + [(NCTX + qb * 512, 512, list(range(NT))) for qb in range(8)]
    cnt = [0]
    for bi, (q0, nq, ktiles) in enumerate(blocks):
        qb_, kqb = Qb[bi % 2]
        qb3 = qb_.rearrange("p (c t) -> p c t", t=512)
        g.ld(qb3[:, :, 0:nq], QTv[:, 8:12, q0:q0 + nq], R=["QTall"], W=[kqb])
        items = []
        for h in range(8):
            for ki, kt in enumerate(ktiles):
                items.append(dict(h=h, ki=ki, kt=kt, last=(ki == len(ktiles) - 1), idx=cnt[0]))
                cnt[0] += 1

        def emitS(it):
            h = it["h"]
            par, kv, pr = h % 2, h // 4, h // 2
            K_, kK = (Kstd, kKs) if par == kv else (Kswp, kKw)
            psSt, kps = (self.ps[0], "ps0") if it["idx"] % 2 == 0 else (self.ps[1], "ps1")
            g.mm(psSt[:, 0:nq], K_[par * 64:(par + 1) * 64, it["kt"] * 128:(it["kt"] + 1) * 128], qb3[par * 64:(par + 1) * 64, pr, 0:nq],
                 True, True, R=[kK, kqb], W=[kps])

        def emitEP(it):
            h = it["h"]
            kv = h // 4
            psSt, kps = (self.ps[0], "ps0") if it["idx"] % 2 == 0 else (self.ps[1], "ps1")
            psOt, kpo = (self.ps[2], "ps2") if h % 2 == 0 else (self.ps[3], "ps3")
            pa, kpa = PTa[it["idx"] % 2]
            g.act(pa[:, 0:nq], psSt[:, 0:nq], AF.Exp, R=[kps], W=[kpa], scale=0.125)
            g.mm(psOt[0:65, 0:nq], VA4[:, it["kt"], kv, :], pa[:, 0:nq], it["ki"] == 0, it["last"], R=[kVA, kpa], W=[kpo])
            if it["last"]:
                g.recip(rd[64:65, 0:nq], psOt[64:65, 0:nq], R=[kpo], W=[krd])
                g.mm(psOt[0:64, 512:512 + nq], onesf[64:65, 0:64], rd[64:65, 0:nq], True, True, R=[kon, krd], W=[kpo + "b"])
                g.cp(rdb[0:64, 0:nq], psOt[0:64, 512:512 + nq], R=[kpo + "b"], W=[krdb], eng="act")
                at, kat = aT[h % 2]
                g.tt(at[0:64, 0:nq], psOt[0:64, 0:nq], rdb[0:64, 0:nq], ALU.mult, R=[kpo, krdb], W=[kat])
                g.st(self.MIXT[512 + h * 64:512 + (h + 1) * 64, q0:q0 + nq], at[0:64, 0:nq], R=[kat], W=[f"MIXTa{bi}_{h}"])
        emitS(items[0])
        for i_ in range(len(items)):
            if i_ + 1 < len(items):
                emitS(items[i_ + 1])
            emitEP(items[i_])

    if self.cfg.get('even_stop') == 3:
        return
    A.reset()
    Wo, kWo = A.alloc("Wo", 8 * 1024, BF16)
    Wov = Wo.rearrange("p (k n) -> p k n", n=1024)
    stg2 = [A.alloc(f"stgo{i}", 1024) for i in range(2)]
    for k in range(8):
        s, ks = stg2[k % 2]
        g.ld(s, self.mix_out_w[ei, k * 128:(k + 1) * 128, :], R=[], W=[ks])
        g.cp(Wov[:, k, :], s, R=[ks], W=[kWo], eng=ce[k % 3])
    mixT = [A.alloc(f"mixT{i}", 1024, BF16) for i in range(2)]

    def d_fn(tt):
        m_, km = mixT[tt % 2]
        m3 = m_.rearrange("p (c t) -> p c t", t=128)
        g.ld(m3, MIXTv[:, :, tt * 128:(tt + 1) * 128], R=["MIXTall"], W=[km])
        for hf in range(2):
            for k in range(8):
                g.mm(self.ps[0][:, hf * 512:(hf + 1) * 512], m3[:, k, :], Wov[:, k, hf * 512:(hf + 1) * 512], k == 0, k == 7,
                     R=[km, kWo], W=["ps0"])
        return self.ps[0][:, :], "ps0"
    self.post_stage(li, l, d_fn)


Prog.setup_rope = setup_rope
Prog.even_mixer = even_mixer


def rev(ap, lo, hi):
    return ap[:, lo:hi][:, ::-1]


def s5_mixer(self, li, l):
    g = self
    kb = self.kb
    A = self.arena
    oi = self.od_idx[l]
    if not hasattr(self, "UT"):
        self.UT = g.dram_tmp("UT", [D, T], BF16)
        self.ZT = g.dram_tmp("ZT", [D, T], BF16)
    UTv = self.UT.rearrange("(c p) t -> p c t", p=128)
    ZTv = self.ZT.rearrange("(c p) t -> p c t", p=128)
    ce = ["pool", "act", "dve"]
    A.reset()
    A1 = [A.alloc(f"A1_{c}", D) for c in range(2)]
    B1 = [A.alloc(f"B1_{c}", D) for c in range(2)]
    ng, kng = A.alloc("ng", D)
    g.load_bcast(self.norm_g[li, 0], ng, kng)
    for c in range(2):
        g.load_mod(li, 1, c, *A1[c])
        g.load_mod(li, 0, c, *B1[c])
        g.stt(A1[c][0], A1[c][0], 1.0, ng, ALU.add, ALU.mult, R=[A1[c][1], kng], W=[A1[c][1]])
    xt = [A.alloc(f"xt{i}", D) for i in range(2)]
    tmp, ktmp = A.alloc("tmp", D)
    hb, khb = A.alloc("hb", D, BF16)
    hT = [A.alloc(f"hT{i}", D, BF16) for i in range(2)]
    st_, kst = A.alloc("stat", 8)
    psT = self.ps[3].bitcast(BF16)
    for tt in range(NT):
        c = 1 if tt < 2 else 0
        b = tt % 2
        x, kx = xt[b]
        g.ld(x, self.X[tt * 128:(tt + 1) * 128, :], R=[f"X{tt}"], W=[kx])
        ssq = st_[:, 0:1]
        g.act(tmp, x, AF.Square, R=[kx], W=[ktmp, kst], accum_out=ssq)
        g.rsqrt_small(ssq, ssq, 1.0 / D, None, R=[kst], W=[kst])
        g.stt(tmp, x, ssq, A1[c][0], ALU.mult, ALU.mult, R=[kx, kst, A1[c][1]], W=[ktmp])
        g.tt(hb, tmp, B1[c][0], ALU.add, R=[ktmp, B1[c][1]], W=[khb])
        for k in range(8):
            g.tr(psT[:, k * 128:(k + 1) * 128], hb[:, k * 128:(k + 1) * 128], self.ident_b[:], R=[khb, "ident_b"], W=["ps3"])
        h_, kh = hT[b]
        g.cp(h_, psT[:, 0:1024], R=["ps3"], W=[kh], eng="act")
        g.st(UTv[:, :, tt * 128:(tt + 1) * 128], h_.rearrange("p (c t) -> p c t", t=128), R=[kh], W=[f"UT{tt}"])

    A.reset()
    BT, kBT = A.alloc("BT", 64 * 128, BF16)
    BTv = BT.rearrange("p (a s) -> p a s", s=128)
    CX, kCX = A.alloc("CX", 6 * 32 * 64, BF16)
    CXv = CX.rearrange("p (a g c) -> p a g c", g=32, c=64)
    rho = [A.alloc(f"rho{d}", 32) for d in range(2)]
    rph = [A.alloc(f"rph{d}", 32) for d in range(2)]
    CN = [[A.alloc(f"cn{d}_{n}", 32) for n in range(2)] for d in range(2)]
    SN = [[A.alloc(f"sn{d}_{n}", 32) for n in range(2)] for d in range(2)]
    NSN = [[A.alloc(f"nsn{d}_{n}", 32) for n in range(2)] for d in range(2)]
    dcol, kdcol = A.alloc("dcol", 8)
    g.ld(dcol, self.s5_d[oi], R=[], W=[kdcol])
    hp, khp = A.alloc("halfpi", 1)
    g.memset(hp, math.pi / 2.0, W=[khp])
    mark = A.off
    g.memset(CX, 0.0, W=[kCX])
    brt, kbr = A.alloc("brt", 512)
    bit, kbi = A.alloc("bit", 512)
    g.ld(brt.rearrange("p (g j) -> p g j", j=16), self.s5_b_re[oi].rearrange("(gp two) p j -> (two p) gp j", two=2), R=[], W=[kbr])
    g.ld(bit.rearrange("p (g j) -> p g j", j=16), self.s5_b_im[oi].rearrange("(gp two) p j -> (two p) gp j", two=2), R=[], W=[kbi])
    br3 = brt.rearrange("p (g j) -> p g j", j=16)
    bi3 = bit.rearrange("p (g j) -> p g j", j=16)
    pt = {}
    for nm in ("are", "aim", "ldt", "lre", "dt", "mag", "ang", "r", "r2", "sn", "cs", "bre", "bim", "den", "nr", "t1", "t2", "kre", "kim"):
        pt[nm] = A.alloc("pp_" + nm, 32)
    ri, kri = A.alloc("pp_ri", 32, I32)
    bbr, kbbr = A.alloc("bbr", 512)
    bbi, kbbi = A.alloc("bbi", 512)
    tb1, ktb1 = A.alloc("tb1", 512)
    tb2, ktb2 = A.alloc("tb2", 512)
    MX, kMX = A.alloc("MX", 32 * 32, BF16)
    crt, kcr = A.alloc("crt", 512)
    cit, kci = A.alloc("cit", 512)
    psT = self.ps[3].bitcast(BF16)
    P_ = lambda n: pt[n][0]
    Kk = lambda n: pt[n][1]

    def T2(out, a, b, op):
        g.tt(P_(out), P_(a), P_(b), op, R=[Kk(a), Kk(b)], W=[Kk(out)])
    for d in range(2):
        for j, nm in enumerate(("are", "aim", "ldt")):
            g.ld(P_(nm), self.s5p[oi, d, j], R=[], W=[Kk(nm)])
        g.ts(P_("lre"), P_("are"), -1e-4, None, ALU.min, None, R=[Kk("are")], W=[Kk("lre")])
        g.act(P_("dt"), P_("ldt"), AF.Exp, R=[Kk("ldt")], W=[Kk("dt")])
        T2("mag", "lre", "dt", ALU.mult)
        g.act(P_("mag"), P_("mag"), AF.Exp, R=[Kk("mag")], W=[Kk("mag")])
        T2("ang", "aim", "dt", ALU.mult)
        g.ts(P_("r"), P_("ang"), 1.0 / TWO_PI, None, ALU.mult, None, R=[Kk("ang")], W=[Kk("r")])
        g.cp(ri, P_("r"), R=[Kk("r")], W=[kri])
        g.tt(P_("r"), P_("r"), ri, ALU.subtract, R=[Kk("r"), kri], W=[Kk("r")])
        g.act(P_("sn"), P_("r"), AF.Sin, R=[Kk("r")], W=[Kk("sn")], scale=TWO_PI)
        g.act(P_("r2"), P_("r"), AF.Abs, R=[Kk("r")], W=[Kk("r2")])
        g.act(P_("cs"), P_("r2"), AF.Sin, R=[Kk("r2"), khp], W=[Kk("cs")], scale=-TWO_PI, bias=hp)
        T2("bre", "mag", "cs", ALU.mult)
        T2("bim", "mag", "sn", ALU.mult)
        T2("den", "lre", "lre", ALU.mult)
        T2("t1", "aim", "aim", ALU.mult)
        T2("den", "den", "t1", ALU.add)
        g.recip(P_("den"), P_("den"), R=[Kk("den")], W=[Kk("den")])
        g.ts(P_("nr"), P_("bre"), -1.0, None, ALU.add, None, R=[Kk("bre")], W=[Kk("nr")])
        T2("t1", "nr", "lre", ALU.mult)
        T2("t2", "bim", "aim", ALU.mult)
        T2("kre", "t1", "t2", ALU.add)
        T2("kre", "kre", "den", ALU.mult)
        T2("t1", "bim", "lre", ALU.mult)
        T2("t2", "nr", "aim", ALU.mult)
        T2("kim", "t1", "t2", ALU.subtract)
        T2("kim", "kim", "den", ALU.mult)
        g.cp(rho[d][0], P_("mag"), R=[Kk("mag")], W=[rho[d][1]])
        g.cp(rph[d][0], P_("r"), R=[Kk("r")], W=[rph[d][1]])
        for ni, nn in enumerate((256, 512)):
            g.ts(P_("t1"), P_("r"), float(nn), None, ALU.mult, None, R=[Kk("r")], W=[Kk("t1")])
            g.cp(ri, P_("t1"), R=[Kk("t1")], W=[kri])
            g.tt(P_("t1"), P_("t1"), ri, ALU.subtract, R=[Kk("t1"), kri], W=[Kk("t1")])
            g.act(SN[d][ni][0], P_("t1"), AF.Sin, R=[Kk("t1")], W=[SN[d][ni][1]], scale=TWO_PI)
            g.act(P_("t2"), P_("t1"), AF.Abs, R=[Kk("t1")], W=[Kk("t2")])
            g.act(CN[d][ni][0], P_("t2"), AF.Sin, R=[Kk("t2"), khp], W=[CN[d][ni][1]], scale=-TWO_PI, bias=hp)
            g.ts(NSN[d][ni][0], SN[d][ni][0], -1.0, None, ALU.mult, None, R=[SN[d][ni][1]], W=[NSN[d][ni][1]])
        bb3r = bbr.rearrange("p (g j) -> p g j", j=16)
        bb3i = bbi.rearrange("p (g j) -> p g j", j=16)
        t13 = tb1.rearrange("p (g j) -> p g j", j=16)
        t23 = tb2.rearrange("p (g j) -> p g j", j=16)
        g.tt(t13, br3, bc(P_("kre"), 2, 16), ALU.mult, R=[kbr, Kk("kre")], W=[ktb1])
        g.tt(t23, bi3, bc(P_("kim"), 2, 16), ALU.mult, R=[kbi, Kk("kim")], W=[ktb2])
        g.tt(bbr, tb1, tb2, ALU.subtract, R=[ktb1, ktb2], W=[kbbr])
        g.tt(t13, bi3, bc(P_("kre"), 2, 16), ALU.mult, R=[kbi, Kk("kre")], W=[ktb1])
        g.tt(t23, br3, bc(P_("kim"), 2, 16), ALU.mult, R=[kbr, Kk("kim")], W=[ktb2])
        g.tt(bbi, tb1, tb2, ALU.add, R=[ktb1, ktb2], W=[kbbi])
        for part, (bsrc, kbs) in enumerate(((bbr, kbbr), (bbi, kbbi))):
            b4 = bsrc.rearrange("p (g two j) -> p g two j", two=2, j=16)
            for par in range(2):
                g.memset(MX, 0.0, W=[kMX])
                MX4 = MX.rearrange("p (g two c) -> p g two c", two=2, c=32)
                g.cp(MX4[0:64, :, par, 0:16], b4[0:64, :, par, :], R=[kbs], W=[kMX])
                g.cp(MX4[64:128, :, par, 16:32], b4[64:128, :, par, :], R=[kbs], W=[kMX])
                for q in range(8):
                    g.tr(psT[:, q * 128:(q + 1) * 128], MX[:, q * 128:(q + 1) * 128], self.ident_b[:], R=[kMX, "ident_b"], W=["ps3"])
                a0 = ((d * 2 + part) * 2 + par) * 8
                g.cp(BT[:, a0 * 128:(a0 + 8) * 128], psT[:, 0:1024], R=["ps3"], W=[kBT], eng="act")
        g.ld(crt.rearrange("p (g k) -> p g k", k=16), self.s5_c_re[oi, d].rearrange("(gp two) p k -> (two p) gp k", two=2), R=[], W=[kcr])
        g.ld(cit.rearrange("p (g k) -> p g k", k=16), self.s5_c_im[oi, d].rearrange("(gp two) p k -> (two p) gp k", two=2), R=[], W=[kci])
        for part, (csrc, kcs, sgn) in enumerate(((crt, kcr, 1.0), (cit, kci, -1.0), (crt, kcr, -1.0))):
            c4 = csrc.rearrange("p (g two k) -> p g two k", two=2, k=16)
            Cv = CXv[:, d * 3 + part].rearrange("p (g two) c -> p g two c", two=2)
            for gpar in range(2):
                g.ts(Cv[0:64, :, gpar, gpar * 32:gpar * 32 + 16], c4[0:64, :, gpar, :], sgn, None, ALU.mult, None, R=[kcs], W=[kCX])
                g.ts(Cv[64:128, :, gpar, gpar * 32 + 16:gpar * 32 + 32], c4[64:128, :, gpar, :], sgn, None, ALU.mult, None, R=[kcs], W=[kCX])
    if self.cfg.get("s5_stop") == 1:
        return
    kb.barrier()
    A.off = mark
    iot, kio = A.alloc("iota", T)
    kb.op("pool", lambda e: e.iota(iot, pattern=[[1, T]], base=0, channel_multiplier=0, allow_small_or_imprecise_dtypes=True), W=[kio])
    uT = [A.alloc("uT0", T, BF16)] * 2
    ysb, kys = A.alloc("ysb", T)
    NB = 512
    TI, kti = A.alloc("TI", NB, I32)
    TFt = [A.alloc(f"TF{i}", NB) for i in range(2)]
    ST = [A.alloc(f"ST{i}", NB) for i in range(2)]
    CT = [A.alloc(f"CT{i}", NB) for i in range(2)]
    W1 = [A.alloc(f"w1_{i}", NB, BF16) for i in range(2)]
    W2 = [A.alloc(f"w2_{i}", NB, BF16) for i in range(2)]
    W3 = [A.alloc(f"w3_{i}", NB, BF16) for i in range(2)]
    W4 = [A.alloc(f"w4_{i}", NB, BF16) for i in range(2)]
    BTR = [A.alloc(f"btr{i}", NB, BF16) for i in range(2)]
    BTI = [A.alloc(f"bti{i}", NB, BF16) for i in range(2)]
    WR = [A.alloc(f"wr{i}", NB) for i in range(2)]
    WI = [A.alloc(f"wi{i}", NB) for i in range(2)]
    P1 = [A.alloc(f"p1_{i}", NB, BF16) for i in range(2)]
    P2 = [A.alloc(f"p2_{i}", NB, BF16) for i in range(2)]
    P3 = [A.alloc(f"p3_{i}", NB, BF16) for i in range(2)]
    P4 = [A.alloc(f"p4_{i}", NB, BF16) for i in range(2)]
    YE = [A.alloc(f"ye{i}", NB) for i in range(2)]
    CAR = [A.alloc(f"carry{i}", 4) for i in range(2)]
    zt, kzt = uT[0]
    lat = [(NCTX + i * NB, NCTX + (i + 1) * NB) for i in range(NLAT // NB)]
    fblocks = [(0, NCTX, False)] + [(lo, hi, False) for (lo, hi) in lat]
    bblocks = [(0, NCTX, True)] + [(lo, hi, True) for (lo, hi) in reversed(lat)]
    gcount = [0]
    for q in range(8):
        u_, ku = uT[q % 2]
        g.ld(u_, self.UT[q * 128:(q + 1) * 128, :], R=["UTall"], W=[ku])
        g.ts(ysb, u_, dcol[:, q:q + 1], None, ALU.mult, None, R=[ku, kdcol], W=[kys])
        blist = []
        for gl in range(4):
            for d in range(2):
                gi = gcount[0]
                gcount[0] += 1
                for bidx, (lo, hi, rv) in enumerate(fblocks if d == 0 else bblocks):
                    blist.append(dict(gl=gl, d=d, gi=gi, bidx=bidx, lo=lo, hi=hi, rv=rv, i=len(blist)))
        for i_, bl in enumerate(blist):
            bl["prev"] = blist[i_ - 1] if bl["bidx"] > 0 else None

        def tokf(bl):
            lo, hi = bl["lo"], bl["hi"]
            return (lambda ap: rev(ap, lo, hi)) if bl["rv"] else (lambda ap: ap[:, lo:hi])

        def stageA(bl):
            gl, d, gi, b = bl["gl"], bl["d"], bl["gi"], bl["i"] % 2
            gp = 4 * q + gl
            half, par = gl // 2, gl % 2
            rows = slice(64 * half, 64 * half + 64)
            n = bl["hi"] - bl["lo"]
            tb = gi % 2
            st, kst2 = ST[tb]
            ct, kct = CT[tb]
            if bl["bidx"] == 0:
                tf, ktf = TFt[tb]
                rcol = rph[d][0][:, gp:gp + 1]
                g.ts(TI, iot[:, 0:NB], rcol, None, ALU.mult, None, R=[kio, rph[d][1]], W=[kti])
                g.stt(tf, iot[:, 0:NB], rcol, TI, ALU.mult, ALU.subtract, R=[kio, rph[d][1], kti], W=[ktf])
                g.act(st, tf, AF.Sin, R=[ktf], W=[kst2], scale=TWO_PI)
                g.act(tf, tf, AF.Abs, R=[ktf], W=[ktf])
                g.act(ct, tf, AF.Sin, R=[ktf, khp], W=[kct], scale=-TWO_PI, bias=hp)
            psBr = self.ps[0][:, b * 512:b * 512 + 512]
            psBi = self.ps[1][:, b * 512:b * 512 + 512]
            k0, k1 = f"ps0.{b}", f"ps1.{b}"
            tok = tokf(bl)
            for part, (pp, kpp) in enumerate(((psBr, k0), (psBi, k1))):
                a0 = ((d * 2 + part) * 2 + par) * 8 + q
                g.mm(pp[:, 0:n], BTv[rows, a0, :], tok(u_[rows, :]), True, True, R=[kBT, ku], W=[kpp])
            brs, kbrs = psBr, k0
            bis, kbis = psBi, k1
            w1, kw1 = W1[b]
            w2, kw2 = W2[b]
            w3, kw3 = W3[b]
            w4, kw4 = W4[b]
            btr, kbtr = BTR[b]
            bti, kbti = BTI[b]
            g.tt(w1[:, 0:n], brs[:, 0:n], ct[:, 0:n], ALU.mult, R=[kbrs, kct], W=[kw1])
            g.tt(w2[:, 0:n], bis[:, 0:n], st[:, 0:n], ALU.mult, R=[kbis, kst2], W=[kw2])
            g.tt(btr[:, 0:n], w1[:, 0:n], w2[:, 0:n], ALU.add, R=[kw1, kw2], W=[kbtr], eng="pool")
            g.tt(w3[:, 0:n], bis[:, 0:n], ct[:, 0:n], ALU.mult, R=[kbis, kct], W=[kw3])
            g.tt(w4[:, 0:n], brs[:, 0:n], st[:, 0:n], ALU.mult, R=[kbrs, kst2], W=[kw4])
            g.tt(bti[:, 0:n], w3[:, 0:n], w4[:, 0:n], ALU.subtract, R=[kw3, kw4], W=[kbti], eng="pool")

        def stageB(bl):
            gl, d, gi, b = bl["gl"], bl["d"], bl["gi"], bl["i"] % 2
            gp = 4 * q + gl
            half = gl // 2
            rows = slice(64 * half, 64 * half + 64)
            n = bl["hi"] - bl["lo"]
            tb = gi % 2
            st, kst2 = ST[tb]
            ct, kct = CT[tb]
            btr, kbtr = BTR[b]
            bti, kbti = BTI[b]
            wr, kwr = WR[b]
            wi, kwi = WI[b]
            car, kcar = CAR[b]
            pv = bl["prev"]
            if pv is None:
                g.cp(self.ps[3][:, 0:512], st, R=[kst2], W=["ps3.S"], eng="act")
                g.cp(self.ps[3][:, 512:1024], ct, R=[kct], W=["ps3.C"], eng="act")
                ini_r, ini_i, Rc = 0.0, 0.0, []
            else:
                pb = pv["i"] % 2
                npv = pv["hi"] - pv["lo"]
                ni = 0 if npv == 256 else 1
                wrl = WR[pb][0][:, npv - 1:npv]
                wil = WI[pb][0][:, npv - 1:npv]
                cn = CN[d][ni][0][:, gp:gp + 1]
                sn = SN[d][ni][0][:, gp:gp + 1]
                nsn = NSN[d][ni][0][:, gp:gp + 1]
                Rk = [WR[pb][1], WI[pb][1], CN[d][ni][1], SN[d][ni][1], NSN[d][ni][1]]
                g.act(car[:, 2:3], wil, AF.Copy, R=Rk, W=[kcar], scale=nsn)
                g.act(car[:, 3:4], wil, AF.Copy, R=Rk + [kcar], W=[kcar], scale=cn)
                g.act(car[:, 0:1], wrl, AF.Identity, R=Rk + [kcar], W=[kcar], scale=cn, bias=car[:, 2:3])
                g.act(car[:, 1:2], wrl, AF.Identity, R=Rk + [kcar], W=[kcar], scale=sn, bias=car[:, 3:4])
                ini_r, ini_i, Rc = car[:, 0:1], car[:, 1:2], [kcar]
            rb = rho[d][0][:, gp:gp + 1].to_broadcast([128, n])
            kb.op("dve", lambda e: e.tensor_tensor_scan(out=wr[:, 0:n], data0=rb, data1=btr[:, 0:n], initial=ini_r,
                                                        op0=ALU.mult, op1=ALU.add), R=[rho[d][1], kbtr] + Rc, W=[kwr])
            kb.op("dve", lambda e: e.tensor_tensor_scan(out=wi[:, 0:n], data0=rb, data1=bti[:, 0:n], initial=ini_i,
                                                        op0=ALU.mult, op1=ALU.add), R=[rho[d][1], kbti] + Rc, W=[kwi])
            p1, kp1 = P1[b]
            p2, kp2 = P2[b]
            p3, kp3 = P3[b]
            p4, kp4 = P4[b]
            pS, pC = self.ps[3][:, 0:512], self.ps[3][:, 512:1024]
            g.tt(p1[:, 0:n], wr[:, 0:n], pC[:, 0:n], ALU.mult, R=[kwr, "ps3.C"], W=[kp1])
            g.tt(p2[:, 0:n], wi[:, 0:n], pS[:, 0:n], ALU.mult, R=[kwi, "ps3.S"], W=[kp2])
            g.tt(p3[:, 0:n], wr[:, 0:n], st[:, 0:n], ALU.mult, R=[kwr, kst2], W=[kp3], eng="pool")
            g.tt(p4[:, 0:n], wi[:, 0:n], ct[:, 0:n], ALU.mult, R=[kwi, kct], W=[kp4], eng="pool")
            psY = self.ps[2][:, b * 512:b * 512 + 512]
            k2 = f"ps2.{b}"
            g.mm(psY[rows, 0:n], CXv[:, d * 3 + 0, gp, :], p1[:, 0:n], True, False, R=[kCX, kp1], W=[k2])
            g.mm(psY[rows, 0:n], CXv[:, d * 3 + 2, gp, :], p2[:, 0:n], False, False, R=[kCX, kp2], W=[k2])
            g.mm(psY[rows, 0:n], CXv[:, d * 3 + 1, gp, :], p3[:, 0:n], False, False, R=[kCX, kp3], W=[k2])
            g.mm(psY[rows, 0:n], CXv[:, d * 3 + 1, gp, :], p4[:, 0:n], False, True, R=[kCX, kp4], W=[k2])

        def stageC(bl):
            gl, b = bl["gl"], bl["i"] % 2
            half = gl // 2
            rows = slice(64 * half, 64 * half + 64)
            n = bl["hi"] - bl["lo"]
            psY = self.ps[2][:, b * 512:b * 512 + 512]
            k2 = f"ps2.{b}"
            tok = tokf(bl)
            g.tt(tok(ysb[rows, :]), tok(ysb[rows, :]), psY[rows, 0:n], ALU.add, R=[kys, k2], W=[kys])

        stageA(blist[0])
        for i_ in range(len(blist)):
            if i_ + 1 < len(blist):
                stageA(blist[i_ + 1])
            stageB(blist[i_])
            if i_ >= 1:
                stageC(blist[i_ - 1])
        stageC(blist[-1])
        g.act(iot, ysb, AF.Square, R=[kys], W=[kio + "g"])
        g.ts(iot, iot, 0.044715, 1.0, ALU.mult, ALU.add, R=[kio + "g"], W=[kio + "g"])
        g.tt(iot, iot, ysb, ALU.mult, R=[kio + "g", kys], W=[kio + "g"])
        g.act(iot, iot, AF.Tanh, R=[kio + "g"], W=[kio + "g"], scale=math.sqrt(2.0 / math.pi))
        g.stt(iot, iot, 1.0, ysb, ALU.add, ALU.mult, R=[kio + "g", kys], W=[kio + "g"])
        g.act(zt, iot, AF.Copy, R=[kio + "g"], W=[kzt], scale=0.5)
        g.st(self.ZT[q * 128:(q + 1) * 128, :], zt, R=[kzt], W=[f"ZT{q}"])
        if q < 7:
            kb.op("pool", lambda e: e.iota(iot, pattern=[[1, T]], base=0, channel_multiplier=0, allow_small_or_imprecise_dtypes=True),
                  R=[kio + "g"], W=[kio, kio + "g"])
    if self.cfg.get("s5_stop") == 2:
        return
    A.reset()
    GW, kGW = A.alloc("GW", 8 * 2048, BF16)
    GWv = GW.rearrange("p (k n) -> p k n", n=2048)
    stg = [A.alloc(f"stgg{i}", 1024) for i in range(2)]
    for k in range(8):
        for hf in range(2):
            s, ks = stg[(2 * k + hf) % 2]
            g.ld(s, self.s5_glu_w[oi, k * 128:(k + 1) * 128, hf * 1024:(hf + 1) * 1024], R=[], W=[ks])
            g.cp(GWv[:, k, hf * 1024:(hf + 1) * 1024], s, R=[ks], W=[kGW], eng=ce[(2 * k + hf) % 3])
    gb, kgb = A.alloc("gb", 2048)
    g.load_bcast(self.s5_glu_b[oi], gb, kgb)
    zT = [A.alloc(f"zT{i}", 1024, BF16) for i in range(2)]
    asb, kasb = A.alloc("asb", 1024)
    gsb, kgsb = A.alloc("gsb", 1024)
    dt_, kdt = A.alloc("dtile", 1024)

    def d_fn(tt):
        z_, kz = zT[tt % 2]
        z3 = z_.rearrange("p (c t) -> p c t", t=128)
        g.ld(z3, ZTv[:, :, tt * 128:(tt + 1) * 128], R=["ZTall"], W=[kz])
        for ch in range(4):
            pp, kpp = (self.ps[0], "ps0") if ch < 2 else (self.ps[3], "ps3")
            for k in range(8):
                g.mm(pp[:, (ch % 2) * 512:(ch % 2 + 1) * 512], z3[:, k, :], GWv[:, k, ch * 512:(ch + 1) * 512], k == 0, k == 7,
                     R=[kz, kGW], W=[kpp])
        g.tt(asb, self.ps[0][:, :], gb[:, 0:1024], ALU.add, R=["ps0", kgb], W=[kasb])
        g.tt(gsb, self.ps[3][:, :], gb[:, 1024:2048], ALU.add, R=["ps3", kgb], W=[kgsb])
        g.act(gsb, gsb, AF.Sigmoid, R=[kgsb], W=[kgsb])
        g.tt(dt_, asb, gsb, ALU.mult, R=[kasb, kgsb], W=[kdt], eng="pool")
        return dt_, kdt
    self.post_stage(li, l, d_fn)


Prog.s5_mixer = s5_mixer


def shared_weights(inp, layers):
    ev = [l // 2 for l in layers if l % 2 == 0]
    od = [l // 2 for l in layers if l % 2 == 1]
    f = lambda a: np.ascontiguousarray(np.asarray(a, dtype=np.float32))
    w = {}
    w["mod_w"] = f(inp["mod_w"][layers])
    w["mod_b"] = f(inp["mod_b"][layers])
    w["norm_g"] = f(inp["norm_g"][layers])
    w["moe_router_w"] = f(inp["moe_router_w"][layers])
    w["moe_w1"] = f(inp["moe_w1"][layers])
    w["moe_w3"] = f(inp["moe_w3"][layers])
    w["moe_w2"] = f(inp["moe_w2"][layers])
    w["final_norm_g"] = f(inp["final_norm_g"])
    if ev:
        w["mix_in_w"] = f(inp["mix_in_w"][ev])
        w["mix_out_w"] = f(inp["mix_out_w"][ev])
        w["ret_log_rate"] = f(np.asarray(inp["ret_log_rate"])[ev].reshape(len(ev), 16))
        w["ret_gn_g"] = f(inp["ret_gn_g"][ev])
        w["qk_norm_g"] = f(np.asarray(inp["qk_norm_g"])[ev].reshape(len(ev), 128))
    else:
        w["mix_in_w"] = np.zeros((1, D, 2816), np.float32)
        w["mix_out_w"] = np.zeros((1, D, D), np.float32)
        w["ret_log_rate"] = np.zeros((1, 16), np.float32)
        w["ret_gn_g"] = np.zeros((1, 512), np.float32)
        w["qk_norm_g"] = np.zeros((1, 128), np.float32)
    if od:
        no = len(od)
        a_re = np.asarray(inp["s5_a_re"])[od]
        a_im = np.asarray(inp["s5_a_im"])[od]
        ldt = np.asarray(inp["s5_log_dt"])[od]
        pair = lambda a: a.reshape(no, 2, 32, 2, 64).transpose(0, 1, 3, 4, 2).reshape(no, 2, 128, 32)
        ldt_b = np.broadcast_to(ldt[..., None], (no, 2, 64, 64))
        w["s5p"] = f(np.stack([pair(a_re), pair(a_im), pair(ldt_b)], axis=2))
        w["s5_b_re"] = f(inp["s5_b_re"][od])
        w["s5_b_im"] = f(inp["s5_b_im"][od])
        w["s5_c_re"] = f(np.asarray(inp["s5_c_re"])[od].transpose(0, 1, 2, 4, 3))
        w["s5_c_im"] = f(np.asarray(inp["s5_c_im"])[od].transpose(0, 1, 2, 4, 3))
        w["s5_d"] = f(np.asarray(inp["s5_d"])[od].reshape(no, 8, 128).transpose(0, 2, 1))
        w["s5_glu_w"] = f(inp["s5_glu_w"][od])
        w["s5_glu_b"] = f(inp["s5_glu_b"][od])
    else:
        w["s5p"] = np.zeros((1, 2, 3, 128, 32), np.float32)
        w["s5_b_re"] = np.zeros((1, 64, 64, 16), np.float32)
        w["s5_b_im"] = np.zeros((1, 64, 64, 16), np.float32)
        w["s5_c_re"] = np.zeros((1, 2, 64, 64, 16), np.float32)
        w["s5_c_im"] = np.zeros((1, 2, 64, 64, 16), np.float32)
        w["s5_d"] = np.zeros((1, 128, 8), np.float32)
        w["s5_glu_w"] = np.zeros((1, D, 2 * D), np.float32)
        w["s5_glu_b"] = np.zeros((1, 2 * D), np.float32)
    return w


def core_inputs(inp, b, w):
    m = dict(w)
    x = np.asarray(inp["x"][b], dtype=np.float32)
    ctx = np.asarray(inp["ctx"][b], dtype=np.float32)
    m["xin"] = np.ascontiguousarray(np.concatenate([ctx, x], axis=0))
    c = np.asarray(inp["c"][b], dtype=np.float32).reshape(8, 128).T
    cc = np.asarray(inp["c_ctx"], dtype=np.float32).reshape(8, 128).T
    m["cin"] = np.ascontiguousarray(np.stack([c, cc], axis=2).reshape(128, 16))
    return m


def kernel(**inputs):
    layers = [0, 1, 2, 3]
    prog = Prog(dict(layers=layers))
    nc = prog.build()
    w = shared_weights(inputs, layers)
    in_maps = [core_inputs(inputs, b, w) for b in range(8)]
    res = run_bass_kernel_spmd(nc, in_maps, core_ids=list(range(8)))
    return np.stack([np.asarray(r["out"], dtype=np.float32) for r in res.results], axis=0)
```

```python
import contextlib
import math
import numpy as np
import concourse.bass as bass
import concourse.mybir as mybir
from concourse.bass_utils import run_bass_kernel_spmd

F32 = mybir.dt.float32
BF16 = mybir.dt.bfloat16
I32 = mybir.dt.int32
U32 = mybir.dt.uint32
ALU = mybir.AluOpType
AF = mybir.ActivationFunctionType
AX = mybir.AxisListType

SEM_LIMIT = 30000
NSLOT = 10

D = 1024
NCTX = 256
NLAT = 4096
T = NCTX + NLAT
NT = T // 128
EPS = 1e-6
TWO_PI = 2.0 * math.pi


class KB:
    ENG = ("pe", "act", "dve", "pool", "sp")

    def __init__(self, nc):
        self.nc = nc
        self.stack = contextlib.ExitStack()
        self.q = {e: [] for e in self.ENG}
        self.nsem = 0
        self.cur = {}
        for e in ("pe", "act", "dve", "pool"):
            self.cur[e] = [self._newsem(e), 0]
        self.slots = {}
        self.slot_i = {}
        for e in ("sp", "pool"):
            self.slots[e] = [[self._newsem("d" + e), 0] for _ in range(NSLOT)]
            self.slot_i[e] = 0
        self.known = {e: {} for e in self.ENG}
        self.last_w = {}
        self.reads = {}
        self.pending = {e: [] for e in self.ENG}
        self.ninst = 0

    def _newsem(self, tag):
        self.nsem += 1
        return self.stack.enter_context(self.nc.semaphore(f"s_{tag}_{self.nsem}"))

    def sbuf(self, name, shape, dtype):
        return self.stack.enter_context(self.nc.sbuf_tensor(name, list(shape), dtype))

    def psum(self, name, shape, dtype):
        return self.stack.enter_context(self.nc.psum_tensor(name, list(shape), dtype))

    def all_tokens(self):
        toks = []
        for e in ("pe", "act", "dve", "pool"):
            c = self.cur[e]
            if c[1] > 0:
                toks.append((c[0], c[1]))
        for e in self.slots:
            for s in self.slots[e]:
                if s[1] > 0:
                    toks.append((s[0], s[1]))
        return toks

    def barrier(self):
        toks = self.all_tokens()
        for e in self.ENG:
            self.pending[e] = list(toks)
        self.last_w = {}
        self.reads = {}

    def _deps(self, eng, R, W):
        need = {}

        def add(tok):
            if tok is None:
                return
            sem, val = tok
            k = id(sem)
            if k not in need or need[k][1] < val:
                need[k] = (sem, val)
        for tok in self.pending[eng]:
            add(tok)
        self.pending[eng] = []
        for r in R:
            add(self.last_w.get(r))
        for w in W:
            add(self.last_w.get(w))
            for t in self.reads.get(w, {}).values():
                add(t)
        out = []
        kn = self.known[eng]
        for k, (sem, val) in need.items():
            if kn.get(k, 0) >= val:
                continue
            kn[k] = val
            out.append((sem, val))
        return out

    def _commit(self, tok, R, W, tag):
        for w in W:
            self.last_w[w] = tok
            self.reads[w] = {}
        for r in R:
            if r in W:
                continue
            self.reads.setdefault(r, {})[tag] = tok

    def op(self, eng, fn, R=(), W=()):
        R = tuple(R)
        W = tuple(W)
        waits = self._deps(eng, R, W)
        c = self.cur[eng]
        if c[1] >= SEM_LIMIT:
            c[0] = self._newsem(eng)
            c[1] = 0
        c[1] += 1
        sem, val = c[0], c[1]
        if eng == "pe":
            self.known[eng][id(sem)] = val
        self.q[eng].append((waits, fn, sem, 1))
        self._commit((sem, val), R, W, eng)
        self.ninst += 1

    def dma(self, qeng, fn, R=(), W=()):
        R = tuple(R)
        W = tuple(W)
        waits = self._deps(qeng, R, W)
        i = self.slot_i[qeng]
        self.slot_i[qeng] = (i + 1) % NSLOT
        s = self.slots[qeng][i]
        kn = self.known[qeng]
        if s[1] > 0 and kn.get(id(s[0]), 0) < s[1]:
            waits.append((s[0], s[1]))
            kn[id(s[0])] = s[1]
        s[1] += 16
        self.q[qeng].append((waits, fn, s[0], 16))
        self._commit((s[0], s[1]), R, W, ("dma", qeng, i))
        self.ninst += 1

    def emit(self):
        nc = self.nc
        finals = self.all_tokens()
        with nc.Block() as block:
            def run(engname):
                def body(e):
                    for waits, fn, sem, inc in self.q[engname]:
                        for (ws, wv) in waits:
                            e.wait_ge(ws, wv)
                        fn(e).then_inc(sem, inc)
                    if engname == "sp":
                        for (ws, wv) in finals:
                            e.wait_ge(ws, wv)
                return body
            block.sync(run("sp"))
            block.tensor(run("pe"))
            block.scalar(run("act"))
            block.vector(run("dve"))
            block.gpsimd(run("pool"))


class Arena:
    def __init__(self, kb, words):
        self.kb = kb
        self.words = words
        self.t = kb.sbuf("arena", [128, words], F32)
        self.off = 0
        self.phase = 0

    def reset(self):
        self.kb.barrier()
        self.off = 0
        self.phase += 1

    def alloc(self, name, cols, dtype=F32, parts=128):
        w = cols if dtype in (F32, I32, U32) else (cols + 1) // 2
        w = (w + 7) // 8 * 8
        assert self.off + w <= self.words, f"arena overflow at {name}: {self.off}+{w}>{self.words}"
        ap = self.t[0:parts, self.off:self.off + w]
        self.off += w
        if dtype != F32:
            ap = ap.bitcast(dtype)
        ap = ap[:, 0:cols]
        return ap, f"p{self.phase}.{name}"


class Gen:
    def __init__(self, cfg):
        self.cfg = cfg
        self.nc = bass.Bass("TRN2", target_bir_lowering=False)
        self.kb = KB(self.nc)
        self.dbg = {}

    def mm(self, out, lhsT, rhs, start, stop, R, W):
        self.kb.op("pe", lambda e: e.matmul(out, lhsT=lhsT, rhs=rhs, start=start, stop=stop), R=R, W=W)

    def tr(self, out, in_, ident, R, W):
        self.kb.op("pe", lambda e: e.transpose(out=out, in_=in_, identity=ident), R=R, W=W)

    def act(self, out, in_, func, R, W, **kw):
        self.kb.op("act", lambda e: e.activation(out=out, in_=in_, func=func, **kw), R=R, W=W)

    def tt(self, out, a, b, op, R, W, eng="dve"):
        self.kb.op(eng, lambda e: e.tensor_tensor(out=out, in0=a, in1=b, op=op), R=R, W=W)

    def ts(self, out, in0, s1, s2, op0, op1, R, W, eng="dve", **kw):
        if s2 is None:
            self.kb.op(eng, lambda e: e.tensor_scalar(out=out, in0=in0, scalar1=s1, scalar2=None, op0=op0, **kw), R=R, W=W)
        else:
            self.kb.op(eng, lambda e: e.tensor_scalar(out=out, in0=in0, scalar1=s1, scalar2=s2, op0=op0, op1=op1, **kw), R=R, W=W)

    def stt(self, out, in0, scalar, in1, op0, op1, R, W):
        self.kb.op("dve", lambda e: e.scalar_tensor_tensor(out=out, in0=in0, scalar=scalar, in1=in1, op0=op0, op1=op1), R=R, W=W)

    def cp(self, out, in_, R, W, eng="dve"):
        if eng == "act":
            self.kb.op("act", lambda e: e.copy(out=out, in_=in_), R=R, W=W)
        else:
            self.kb.op(eng, lambda e: e.tensor_copy(out=out, in_=in_), R=R, W=W)

    def memset(self, out, val, W, eng="dve"):
        self.kb.op(eng, lambda e: e.memset(out, val), W=W)

    def ld(self, out, in_, R, W, q="sp"):
        self.kb.dma(q, lambda e: e.dma_start(out=out, in_=in_), R=R, W=W)

    def st(self, out, in_, R, W, q="pool"):
        self.kb.dma(q, lambda e: e.dma_start(out=out, in_=in_), R=R, W=W)

    def red(self, out, in_, op, R, W, axis=AX.X):
        self.kb.op("dve", lambda e: e.tensor_reduce(out=out, in_=in_, axis=axis, op=op), R=R, W=W)

    def recip(self, out, in_, R, W):
        self.kb.op("dve", lambda e: e.reciprocal(out=out, in_=in_), R=R, W=W)

    def rsqrt_small(self, out, in_, scale, tmpkey, R, W):
        self.ts(out, in_, scale, EPS, ALU.mult, ALU.add, R=R, W=W)
        self.act(out, out, AF.Sqrt, R=W, W=W)
        self.recip(out, out, R=W, W=W)

    def dram_in(self, name, shape, dtype=F32):
        return self.nc.dram_tensor(name, list(shape), dtype, kind="ExternalInput").ap()

    def dram_out(self, name, shape, dtype=F32):
        return self.nc.dram_tensor(name, list(shape), dtype, kind="ExternalOutput").ap()

    def dram_tmp(self, name, shape, dtype=F32):
        if self.cfg.get("dbg_" + name):
            ap = self.nc.dram_tensor(name, list(shape), dtype, kind="ExternalOutput").ap()
            self.dbg[name] = ap
            return ap
        return self.nc.dram_tensor(name, list(shape), dtype, kind="Internal").ap()


class Prog(Gen):
    def __init__(self, cfg):
        super().__init__(cfg)
        g = self
        layers = cfg["layers"]
        self.layers = layers
        nl = len(layers)
        ev = [l for l in layers if l % 2 == 0]
        od = [l for l in layers if l % 2 == 1]
        self.ev_idx = {l: i for i, l in enumerate(ev)}
        self.od_idx = {l: i for i, l in enumerate(od)}
        ne, no = max(len(ev), 1), max(len(od), 1)
        self.xin = g.dram_in("xin", [T, D])
        self.cin = g.dram_in("cin", [128, 16])
        self.mod_w = g.dram_in("mod_w", [nl, D, 6 * D])
        self.mod_b = g.dram_in("mod_b", [nl, 6 * D])
        self.norm_g = g.dram_in("norm_g", [nl, 2, D])
        self.mix_in_w = g.dram_in("mix_in_w", [ne, D, 2816])
        self.mix_out_w = g.dram_in("mix_out_w", [ne, D, D])
        self.ret_log_rate = g.dram_in("ret_log_rate", [ne, 16])
        self.ret_gn_g = g.dram_in("ret_gn_g", [ne, 512])
        self.qk_norm_g = g.dram_in("qk_norm_g", [ne, 128])
        self.s5p = g.dram_in("s5p", [no, 2, 3, 128, 32])
        self.s5_b_re = g.dram_in("s5_b_re", [no, 64, 64, 16])
        self.s5_b_im = g.dram_in("s5_b_im", [no, 64, 64, 16])
        self.s5_c_re = g.dram_in("s5_c_re", [no, 2, 64, 64, 16])
        self.s5_c_im = g.dram_in("s5_c_im", [no, 2, 64, 64, 16])
        self.s5_d = g.dram_in("s5_d", [no, 128, 8])
        self.s5_glu_w = g.dram_in("s5_glu_w", [no, D, 2 * D])
        self.s5_glu_b = g.dram_in("s5_glu_b", [no, 2 * D])
        self.moe_router_w = g.dram_in("moe_router_w", [nl, D, 16])
        self.moe_w1 = g.dram_in("moe_w1", [nl, 16, D, 2 * D])
        self.moe_w3 = g.dram_in("moe_w3", [nl, 16, D, 2 * D])
        self.moe_w2 = g.dram_in("moe_w2", [nl, 16, 2 * D, D])
        self.final_norm_g = g.dram_in("final_norm_g", [D])
        self.out = g.dram_out("out", [NLAT, D])
        self.X = g.dram_tmp("X", [T, D])
        self.Fb = g.dram_tmp("Fb", [T, D], BF16)
        self.MOD = g.dram_tmp("MOD", [nl, 2, 6 * D])
        self.AFF = g.dram_tmp("AFF", [16, T])
        kb = self.kb
        self.arena = Arena(kb, cfg.get("arena_words", 40448))
        self.ident_f = kb.sbuf("ident_f", [128, 128], F32)
        self.ident_b = kb.sbuf("ident_b", [128, 128], BF16)
        self.IDXI = kb.sbuf("IDXI", [128, 80], I32)
        self.GVT = kb.sbuf("GVT", [128, 80], F32)
        self.ps = [kb.psum(f"ps{i}", [128, 1024], F32) for i in range(4)]
        self.psk = [f"ps{i}" for i in range(4)]
        kb.op("pool", lambda e: e.iota(self.ident_f[:], pattern=[[1, 128]], base=0, channel_multiplier=-1,
                                       allow_small_or_imprecise_dtypes=True), W=["ident_f"])
        g.kb.op("dve", lambda e: e.tensor_single_scalar(out=self.ident_f[:], in_=self.ident_f[:], scalar=0.0, op=ALU.is_equal),
                R=["ident_f"], W=["ident_f"])
        g.cp(self.ident_b[:], self.ident_f[:], R=["ident_f"], W=["ident_b"])

    def phase0(self):
        g = self
        A = self.arena
        A.reset()
        for i in range(4):
            r0 = i * (T // 4)
            g.ld(self.X[r0:r0 + T // 4, :], self.xin[r0:r0 + T // 4, :], R=[], W=[f"Xinit{i}"])
        cs, kcs = A.alloc("cs", 16)
        g.ld(cs, self.cin, R=[], W=[kcs])
        g.act(cs, cs, AF.Silu, R=[kcs], W=[kcs])
        cs3 = cs.rearrange("p (k c) -> p k c", c=2)
        mb, kmb = A.alloc("mb", 6144, parts=2)
        m2, km2 = A.alloc("m2", 6144, parts=2)
        stg = [A.alloc(f"stg{i}", 4096) for i in range(2)]
        for li in range(len(self.layers)):
            g.ld(mb, self.mod_b[li].partition_broadcast(2), R=[], W=[kmb])
            for n in range(12):
                s, ks = stg[n % 2]
                s3 = s.rearrange("p (k n) -> p k n", n=512)
                g.ld(s3, self.mod_w[li][:, n * 512:(n + 1) * 512].rearrange("(k p) n -> p k n", p=128), R=[], W=[ks])
                for k in range(8):
                    g.mm(self.ps[0][0:2, 0:512], cs3[:, k, :], s3[:, k, :], k == 0, k == 7, R=[kcs, ks], W=["ps0"])
                g.tt(m2[:, n * 512:(n + 1) * 512], self.ps[0][0:2, 0:512], mb[:, n * 512:(n + 1) * 512], ALU.add,
                     R=["ps0", kmb], W=[km2])
            g.st(self.MOD[li], m2, R=[km2], W=[f"MOD{li}"])

    def load_mod(self, li, which, isctx, dst, kdst):
        self.ld(dst, self.MOD[li, isctx, which * D:(which + 1) * D].partition_broadcast(128), R=[], W=[kdst])

    def load_bcast(self, vec_ap, dst, kdst, n=128):
        self.ld(dst, vec_ap.partition_broadcast(n), R=[], W=[kdst])

    def norm_tile(self, x, kx, ssq, kss, junk, kjunk):
        g = self
        g.act(junk, x, AF.Square, R=[kx], W=[kjunk, kss], accum_out=ssq)
        g.rsqrt_small(ssq, ssq, 1.0 / D, None, R=[kss], W=[kss])

    def post_stage(self, li, l, d_fn, alloc_extra=None):
        g = self
        A = self.arena
        G1 = [A.alloc(f"G1_{c}", D) for c in range(2)]
        A2 = [A.alloc(f"A2_{c}", D) for c in range(2)]
        B2 = [A.alloc(f"B2_{c}", D) for c in range(2)]
        ng, kng = A.alloc("ng", D)
        g.load_bcast(self.norm_g[li, 1], ng, kng)
        for c in range(2):
            g.load_mod(li, 2, c, *G1[c])
            g.load_mod(li, 4, c, *A2[c])
            g.load_mod(li, 3, c, *B2[c])
            g.stt(A2[c][0], A2[c][0], 1.0, ng, ALU.add, ALU.mult, R=[A2[c][1], kng], W=[A2[c][1]])
        xt = [A.alloc(f"xt{i}", D) for i in range(2)]
        xn = [A.alloc(f"xn{i}", D) for i in range(2)]
        tmp, ktmp = A.alloc("tmp", D)
        ff = [A.alloc(f"ff{i}", D) for i in range(2)]
        fb = [A.alloc(f"fb{i}", D, BF16) for i in range(2)]
        fT, kfT = A.alloc("fT32", D)
        wr, kwr = A.alloc("wr", 128)
        st_, kst = A.alloc("stat", 8)
        ex, kex = A.alloc("ex", 16)
        aft, kaft = A.alloc("AFFT", T, parts=16)
        g.ld(wr.rearrange("p (k e) -> p k e", e=16), self.moe_router_w[li].rearrange("(k p) e -> p k e", p=128), R=[], W=[kwr])
        wr3 = wr.rearrange("p (k e) -> p k e", e=16)
        psd, pst, psl = self.ps[0], self.ps[1], self.ps[2]
        def part1(tt):
            c = 1 if tt < 2 else 0
            b = tt % 2
            x, kx = xt[b]
            g.ld(x, self.X[tt * 128:(tt + 1) * 128, :], R=[f"X{tt}"], W=[kx])
            d, kd = d_fn(tt)
            g.tt(tmp, d, G1[c][0], ALU.mult, R=[kd, G1[c][1]], W=[ktmp])
            xo, kxo = xn[b]
            g.tt(xo, x, tmp, ALU.add, R=[kx, ktmp], W=[kxo])
        part1(0)
        for tt in range(NT):
            c = 1 if tt < 2 else 0
            b = tt % 2
            xo, kxo = xn[b]
            if tt + 1 < NT:
                part1(tt + 1)
            g.st(self.X[tt * 128:(tt + 1) * 128, :], xo, R=[kxo], W=[f"X{tt}"])
            ssq = st_[:, 0:1]
            g.act(fT, xo, AF.Square, R=[kxo], W=[kfT, kst], accum_out=ssq)
            g.rsqrt_small(ssq, ssq, 1.0 / D, None, R=[kst], W=[kst])
            f, kf = ff[b]
            g.stt(f, xo, ssq, A2[c][0], ALU.mult, ALU.mult, R=[kxo, kst, A2[c][1]], W=[kf])
            g.tt(f, f, B2[c][0], ALU.add, R=[kf, B2[c][1]], W=[kf])
            fbt, kfb = fb[b]
            g.cp(fbt, f, R=[kf], W=[kfb], eng="act")
            g.st(self.Fb[tt * 128:(tt + 1) * 128, :], fbt, R=[kfb], W=[f"Fb{tt}"])
            for k in range(8):
                g.tr(pst[:, k * 128:(k + 1) * 128], f[:, k * 128:(k + 1) * 128], self.ident_f[:], R=[kf, "ident_f"], W=["ps1"])
            g.cp(fT, pst[:, :], R=["ps1"], W=[kfT], eng="act")
            for k in range(8):
                g.mm(psl[:, 0:16], fT[:, k * 128:(k + 1) * 128], wr3[:, k, :], k == 0, k == 7, R=[kfT, kwr], W=["ps2"])
            mx = st_[:, 1:2]
            sm = st_[:, 2:3]
            g.red(mx, psl[:, 0:16], ALU.max, R=["ps2"], W=[kst])
            g.ts(mx, mx, -1.0, None, ALU.mult, None, R=[kst], W=[kst])
            g.act(ex, psl[:, 0:16], AF.Exp, R=["ps2", kst], W=[kex, kst], bias=mx, accum_out=sm)
            g.recip(sm, sm, R=[kst], W=[kst])
            g.ts(ex, ex, sm, None, ALU.mult, None, R=[kex, kst], W=[kex])
            g.tr(psl[0:16, 512:640], ex, self.ident_f[:], R=[kex, "ident_f"], W=["ps2"])
            g.cp(aft[:, tt * 128:(tt + 1) * 128], psl[0:16, 512:640], R=["ps2"], W=[kaft], eng="act")
        g.st(self.AFF, aft, R=[kaft], W=["AFF"])

    def moe_topk(self):
        g = self
        A = self.arena
        A.reset()
        af, kaf = A.alloc("af", T, parts=16)
        wk = [A.alloc(f"wk{i}", NLAT, parts=16) for i in range(2)]
        mxv, kmx = A.alloc("mxv", 544, parts=16)
        ixv, kix = A.alloc("ixv", 544, U32, parts=16)
        idf, kidf = A.alloc("idf", 544, parts=16)
        tf, ktf = A.alloc("tf", 80)
        g.ld(af, self.AFF, R=["AFF"], W=[kaf])

        def rounds(src0, ksrc0, n, col0, nr):
            src, ksrc = src0, ksrc0
            for r in range(nr):
                c0 = col0 + 8 * r
                g.kb.op("dve", lambda e, c0=c0, src=src: e.max(out=mxv[:, c0:c0 + 8], in_=src), R=[ksrc], W=[kmx])
                g.kb.op("dve", lambda e, c0=c0, src=src: e.max_index(out=ixv[:, c0:c0 + 8], in_max=mxv[:, c0:c0 + 8], in_values=src),
                        R=[ksrc, kmx], W=[kix])
                if r < nr - 1:
                    dst, kdst = wk[r % 2]
                    dstv = dst[:, 0:n]
                    g.kb.op("dve", lambda e, c0=c0, src=src, dstv=dstv: e.match_replace(out=dstv, in_to_replace=mxv[:, c0:c0 + 8],
                                                                                         in_values=src, imm_value=-1.0),
                            R=[ksrc, kmx], W=[kdst])
                    src, ksrc = dstv, kdst
        rounds(af[:, NCTX:T], kaf, NLAT, 0, 64)
        rounds(af[:, 0:NCTX], kaf, NCTX, 512, 4)
        g.cp(idf, ixv, R=[kix], W=[kidf])
        g.ts(idf[:, 0:512], idf[:, 0:512], float(NCTX), None, ALU.add, None, R=[kidf], W=[kidf])
        psl = self.ps[2]
        for sc in range(5):
            n = 128 if sc < 4 else 32
            g.tr(psl[0:n, 0:16], idf[:, sc * 128:sc * 128 + n], self.ident_f[0:16, 0:16], R=[kidf, "ident_f"], W=["ps2"])
            g.cp(self.IDXI[0:n, sc * 16:(sc + 1) * 16], psl[0:n, 0:16], R=["ps2"], W=["IDXI"])
            g.tr(psl[0:n, 16:32], mxv[:, sc * 128:sc * 128 + n], self.ident_f[0:16, 0:16], R=[kmx, "ident_f"], W=["ps2"])
            g.cp(self.GVT[0:n, sc * 16:(sc + 1) * 16], psl[0:n, 16:32], R=["ps2"], W=["GVT"])

    def moe_experts(self, li):
        g = self
        A = self.arena
        A.reset()
        W1b, _ = A.alloc("W1b", 8 * 2048, BF16)
        W3b, _ = A.alloc("W3b", 8 * 2048, BF16)
        W2b, _ = A.alloc("W2b", 16 * 1024, BF16)
        W1v = W1b.rearrange("p (k f) -> p k f", f=2048)
        W3v = W3b.rearrange("p (k f) -> p k f", f=2048)
        W2v = W2b.rearrange("p (k f) -> p k f", f=1024)
        XSt, kXS = A.alloc("XS", 5 * 1024, BF16)
        XS = XSt.rearrange("p (s d) -> p s d", d=1024)
        HIDt, kHID = A.alloc("HID", 16 * 544, BF16)
        HID = HIDt.rearrange("p (f s) -> p f s", s=544)
        XST, kXST = A.alloc("XST", 8 * 544, BF16)
        XSTv = XST.rearrange("p (k s) -> p k s", s=544)
        SIL = [A.alloc(f"sil{i}", 544) for i in range(2)]
        YSb = [A.alloc(f"YS{i}", 1024) for i in range(2)]
        G2 = [A.alloc(f"G2_{c}", D) for c in range(2)]
        for c in range(2):
            g.load_mod(li, 5, c, *G2[c])

        def load13(e):
            for (wsrc, wv, nm) in ((self.moe_w1, W1v, "W1"), (self.moe_w3, W3v, "W3")):
                for k in range(8):
                    g.ld(wv[:, k, :], wsrc[li, e, k * 128:(k + 1) * 128, :], R=[], W=[f"{nm}.{k}"], q="pool")

        def load2(e):
            for fc in range(0, 16, 2):
                g.ld(W2v[:, fc:fc + 2, :], self.moe_w2[li, e, fc * 128:(fc + 2) * 128, :].rearrange("(a p) n -> p a n", p=128),
                     R=[], W=[f"W2.{fc}", f"W2.{fc + 1}"], q="pool")

        def gather(e):
            for sc in range(5):
                n = 128 if sc < 4 else 32
                col = sc * 16 + e
                g.kb.dma("pool", lambda en, n=n, sc=sc, col=col: en.indirect_dma_start(
                    out=XS[0:n, sc, :], out_offset=None, in_=self.Fb,
                    in_offset=bass.IndirectOffsetOnAxis(ap=self.IDXI[0:n, col:col + 1], axis=0)),
                    R=["IDXI", "Fball"], W=[kXS])

        def transposes(e):
            pT = self.ps[3].bitcast(BF16)
            for sc in range(5):
                n = 128 if sc < 4 else 32
                for k in range(8):
                    g.tr(pT[:, k * 128:k * 128 + n], XS[0:n, sc, k * 128:(k + 1) * 128], self.ident_b[0:n, 0:n],
                         R=[kXS, "ident_b"], W=["ps3"])
                g.cp(XSTv[:, :, sc * 128:sc * 128 + n], pT[:, 0:1024].rearrange("p (k s) -> p k s", s=128)[:, :, 0:n],
                     R=["ps3"], W=[kXST], eng="act" if sc % 2 == 0 else "dve")

        def hidden(e):
            for fc in range(16):
                p1, k1 = (self.ps[0], "ps0") if fc % 2 == 0 else (self.ps[2], "ps2")
                p3, k3 = (self.ps[1], "ps1") if fc % 2 == 0 else (self.ps[3], "ps3")
                for (wv, nm, pp, kp) in ((W1v, "W1", p1, k1), (W3v, "W3", p3, k3)):
                    for (n0, n1) in ((0, 512), (512, 544)):
                        for k in range(8):
                            g.mm(pp[:, n0:n1], wv[:, k, fc * 128:(fc + 1) * 128], XSTv[:, k, n0:n1], k == 0, k == 7,
                                 R=[f"{nm}.{k}", kXST], W=[kp])
                sil, ksil = SIL[fc % 2]
                g.act(sil, p1[:, 0:544], AF.Silu, R=[k1], W=[ksil])
                g.tt(HID[:, fc, :], sil, p3[:, 0:544], ALU.mult, R=[ksil, k3], W=[kHID])

        ysi = [0]

        def outscatter(e):
            for sc in range(5):
                n = 128 if sc < 4 else 32
                c = 0 if sc < 4 else 1
                col = sc * 16 + e
                py, ky = (self.ps[0], "ps0") if sc % 2 == 0 else (self.ps[1], "ps1")
                for hf in range(2):
                    for fc in range(16):
                        g.mm(py[0:n, hf * 512:(hf + 1) * 512], HID[:, fc, sc * 128:sc * 128 + n], W2v[:, fc, hf * 512:(hf + 1) * 512],
                             fc == 0, fc == 15, R=[kHID, f"W2.{fc}"], W=[ky])
                YS, kYS = YSb[ysi[0] % 2]
                ysi[0] += 1
                g.stt(YS[0:n, :], py[0:n, :], self.GVT[0:n, col:col + 1], G2[c][0][0:n, :], ALU.mult, ALU.mult,
                      R=[ky, "GVT", G2[c][1]], W=[kYS])
                g.kb.dma("pool", lambda en, n=n, col=col, YS=YS: en.indirect_dma_start(
                    out=self.X, out_offset=bass.IndirectOffsetOnAxis(ap=self.IDXI[0:n, col:col + 1], axis=0),
                    in_=YS[0:n, :], in_offset=None, compute_op=ALU.add),
                    R=["IDXI", kYS, "Xsc"], W=["Xsc"])

        gather(0)
        load13(0)
        load2(0)
        transposes(0)
        for e in range(16):
            if e + 1 < 16:
                gather(e + 1)
            hidden(e)
            if e + 1 < 16:
                load13(e + 1)
                transposes(e + 1)
            outscatter(e)
            if e + 1 < 16:
                load2(e + 1)

    def final_norm(self):
        g = self
        A = self.arena
        A.reset()
        fg, kfg = A.alloc("fg", D)
        g.load_bcast(self.final_norm_g, fg, kfg)
        xt = [A.alloc(f"xt{i}", D) for i in range(2)]
        ot = [A.alloc(f"ot{i}", D) for i in range(2)]
        junk, kj = A.alloc("junk", D)
        st_, kst = A.alloc("stat", 8)
        for tt in range(2, NT):
            b = tt % 2
            x, kx = xt[b]
            o, ko = ot[b]
            g.ld(x, self.X[tt * 128:(tt + 1) * 128, :], R=[f"X{tt}"], W=[kx])
            g.norm_tile(x, kx, st_[:, 0:1], kst, junk, kj)
            g.stt(o, x, st_[:, 0:1], fg, ALU.mult, ALU.mult, R=[kx, kst, kfg], W=[ko])
            g.st(self.out[(tt - 2) * 128:(tt - 1) * 128, :], o, R=[ko], W=[f"out{tt}"])

    def build(self):
        g = self
        cfg = self.cfg
        self.phase0()
        if cfg.get("mixer", True) and any(l % 2 == 0 for l in self.layers):
            self.setup_rope()
        for li, l in enumerate(self.layers):
            if cfg.get("mixer", True):
                if l % 2 == 0:
                    self.even_mixer(li, l)
                else:
                    self.s5_mixer(li, l)
            else:
                A = self.arena
                A.reset()
                z, kz = A.alloc("zero", D)
                g.memset(z, 0.0, W=[kz])
                self.post_stage(li, l, lambda tt: (z, kz))
            if cfg.get("moe", True):
                self.moe_topk()
                self.moe_experts(li)
        self.final_norm()
        self.kb.emit()
        return self.nc


def bc(ap, axis, n):
    a = ap.unsqueeze(axis)
    shp = list(a.shape)
    shp[axis] = n
    return a.broadcast_to(shp)


def setup_rope(self):
    g = self
    kb = self.kb
    self.cosT = kb.sbuf("cosT", [128, 1024], F32)
    self.sinT = kb.sbuf("sinT", [128, 1024], F32)
    A = self.arena
    A.reset()
    fi, kfi = A.alloc("fi", 16)
    inv, kinv = A.alloc("inv", 16)
    pidx, kp = A.alloc("pidx", 1)
    ph, kph = A.alloc("ph", 1)
    colv, kcol = A.alloc("colv", 1)
    rowv, krow = A.alloc("rowv", 32)
    ang, kang = A.alloc("ang", 1024)
    ri, kri = A.alloc("ri", 1024, I32)
    ab, kab = A.alloc("ab", 1024)
    kb.op("pool", lambda e: e.iota(fi, pattern=[[1, 16]], base=0, channel_multiplier=0, allow_small_or_imprecise_dtypes=True), W=[kfi])
    g.act(inv, fi, AF.Exp, R=[kfi], W=[kinv], scale=-(2.0 / 32.0) * math.log(10000.0))
    kb.op("pool", lambda e: e.iota(pidx, pattern=[[0, 1]], base=0, channel_multiplier=1, allow_small_or_imprecise_dtypes=True), W=[kp])
    g.ts(ph, pidx, 64.0, None, ALU.is_ge, None, R=[kp], W=[kph])
    g.stt(colv, ph, -64.0, pidx, ALU.mult, ALU.add, R=[kph, kp], W=[kcol])
    kb.op("pool", lambda e: e.iota(rowv, pattern=[[2, 32]], base=0, channel_multiplier=0, allow_small_or_imprecise_dtypes=True), W=[krow])
    g.ts(rowv, rowv, ph, None, ALU.add, None, R=[krow, kph], W=[krow])
    a4 = ang.rearrange("p (t a i) -> p t a i", a=2, i=16)
    g.tt(a4[:, :, 0, :], bc(rowv, 2, 16), bc(inv, 1, 32), ALU.mult, R=[krow, kinv], W=[kang])
    g.ts(a4[:, :, 1, :], bc(inv, 1, 32), colv, None, ALU.mult, None, R=[kinv, kcol, kang], W=[kang])
    g.ts(ang, ang, 1.0 / TWO_PI, None, ALU.mult, None, R=[kang], W=[kang])
    g.cp(ri, ang, R=[kang], W=[kri])
    g.tt(ang, ang, ri, ALU.subtract, R=[kang, kri], W=[kang])
    g.act(self.sinT[:], ang, AF.Sin, R=[kang], W=["sinT"], scale=TWO_PI)
    g.act(ab, ang, AF.Abs, R=[kang], W=[kab])
    hp, khp = A.alloc("halfpi", 1)
    g.memset(hp, math.pi / 2.0, W=[khp])
    g.act(self.cosT[:], ab, AF.Sin, R=[kab, khp], W=["cosT"], scale=-TWO_PI, bias=hp)


def rope(self, src, ksrc, dst, kdst, H, tt, tmps):
    g = self
    t = tt - 2
    sv = src.rearrange("p (h a s i) -> p h a s i", a=2, s=2, i=16)
    dv = dst.rearrange("p (h a s i) -> p h a s i", a=2, s=2, i=16)
    x1, x2 = sv[:, :, :, 0, :], sv[:, :, :, 1, :]
    cos = bc(self.cosT[:, t * 32:(t + 1) * 32].rearrange("p (a i) -> p a i", i=16), 1, H)
    sin = bc(self.sinT[:, t * 32:(t + 1) * 32].rearrange("p (a i) -> p a i", i=16), 1, H)
    (t1, k1), (t2, k2), (t3, k3), (t4, k4) = tmps
    v = lambda a: a[:, 0:H * 32].rearrange("p (h a i) -> p h a i", a=2, i=16)
    g.tt(v(t1), x1, cos, ALU.mult, R=[ksrc, "cosT"], W=[k1])
    g.tt(v(t2), x2, sin, ALU.mult, R=[ksrc, "sinT"], W=[k2])
    g.tt(dv[:, :, :, 0, :], v(t1), v(t2), ALU.subtract, R=[k1, k2], W=[kdst])
    g.tt(v(t3), x1, sin, ALU.mult, R=[ksrc, "sinT"], W=[k3], eng="pool")
    g.tt(v(t4), x2, cos, ALU.mult, R=[ksrc, "cosT"], W=[k4], eng="pool")
    g.tt(dv[:, :, :, 1, :], v(t3), v(t4), ALU.add, R=[k3, k4], W=[kdst], eng="pool")


def even_mixer(self, li, l):
    g = self
    kb = self.kb
    A = self.arena
    ei = self.ev_idx[l]
    if not hasattr(self, "QT"):
        self.QT = g.dram_tmp("QT", [1664, T], BF16)
        self.TMd = g.dram_tmp("TMd", [T, 2176], BF16)
        self.OF = g.dram_tmp("OF", [T, 512])
        self.MIXT = g.dram_tmp("MIXT", [1024, T], BF16)
    QTv = self.QT.rearrange("(c p) t -> p c t", p=128)
    MIXTv = self.MIXT.rearrange("(c p) t -> p c t", p=128)
    A.reset()
    Wb, _ = A.alloc("Wb", 8 * 2816, BF16)
    Wv = Wb.rearrange("p (k n) -> p k n", n=2816)
    stg = [A.alloc(f"stg{i}", 1408) for i in range(2)]
    ce = ["pool", "act", "dve"]
    for k in range(8):
        g.ld(Wv[:, k, :], self.mix_in_w[ei, k * 128:(k + 1) * 128, :], R=[], W=["Wb"], q="pool")
    A1 = [A.alloc(f"A1_{c}", D) for c in range(2)]
    B1 = [A.alloc(f"B1_{c}", D) for c in range(2)]
    ng, kng = A.alloc("ng", D)
    g.load_bcast(self.norm_g[li, 0], ng, kng)
    for c in range(2):
        g.load_mod(li, 1, c, *A1[c])
        g.load_mod(li, 0, c, *B1[c])
        g.stt(A1[c][0], A1[c][0], 1.0, ng, ALU.add, ALU.mult, R=[A1[c][1], kng], W=[A1[c][1]])
    gq, kgq = A.alloc("gq", 64)
    gk, kgk = A.alloc("gk", 64)
    g.load_bcast(self.qk_norm_g[ei, 0:64], gq, kgq)
    g.load_bcast(self.qk_norm_g[ei, 64:128], gk, kgk)
    lgt, klg = A.alloc("lgt", 16)
    g.load_bcast(self.ret_log_rate[ei], lgt, klg)
    g.act(lgt, lgt, AF.Exp, R=[klg], W=[klg])
    g.ts(lgt, lgt, -1.0, None, ALU.mult, None, R=[klg], W=[klg])
    pidx, kp = A.alloc("pidx", 1)
    pr_, kpr = A.alloc("prev", 1)
    kb.op("pool", lambda e: e.iota(pidx, pattern=[[0, 1]], base=0, channel_multiplier=1, allow_small_or_imprecise_dtypes=True), W=[kp])
    g.ts(pr_, pidx, -1.0, 127.0, ALU.mult, ALU.add, R=[kp], W=[kpr])
    DK, kDK = A.alloc("DK", 16)
    g.ts(DK[:, 0:8], lgt[:, 0:8], pr_, None, ALU.mult, None, R=[klg, kpr], W=[kDK])
    g.ts(DK[:, 8:16], lgt[:, 8:16], pidx, None, ALU.mult, None, R=[klg, kp, kDK], W=[kDK])
    g.act(DK, DK, AF.Exp, R=[kDK], W=[kDK])
    xt = [A.alloc(f"xt{i}", D) for i in range(2)]
    tmp, ktmp = A.alloc("tmp", D)
    hb, khb = A.alloc("hb", D, BF16)
    hT, khT = A.alloc("hT", D, BF16)
    P, kP = A.alloc("P", 2816)
    sqt, ksq = A.alloc("sqt", 640)
    st_, kst = A.alloc("stat", 16)
    QKb = [A.alloc(f"QKb{i}", 1664, BF16) for i in range(2)]
    TM = [A.alloc(f"TM{i}", 2176, BF16) for i in range(2)]
    QTs = [A.alloc(f"QTs{i}", 1664, BF16) for i in range(2)]
    tmps = [A.alloc(f"rt{i}", 512) for i in range(4)]
    psT = self.ps[3].bitcast(BF16)
    def e1_a(tt):
        c = 1 if tt < 2 else 0
        b = tt % 2
        x, kx = xt[b]
        g.ld(x, self.X[tt * 128:(tt + 1) * 128, :], R=[f"X{tt}"], W=[kx])
        ssq = st_[:, 0:1]
        g.act(tmp, x, AF.Square, R=[kx], W=[ktmp, kst], accum_out=ssq)
        g.rsqrt_small(ssq, ssq, 1.0 / D, None, R=[kst], W=[kst])
        g.stt(tmp, x, ssq, A1[c][0], ALU.mult, ALU.mult, R=[kx, kst, A1[c][1]], W=[ktmp])
        g.tt(hb, tmp, B1[c][0], ALU.add, R=[ktmp, B1[c][1]], W=[khb])
        for k in range(8):
            g.tr(psT[:, k * 128:(k + 1) * 128], hb[:, k * 128:(k + 1) * 128], self.ident_b[:], R=[khb, "ident_b"], W=["ps3"])
        g.cp(hT, psT[:, 0:1024], R=["ps3"], W=[khT], eng="act")
        for (n0, w, pp, kp_, off) in ((0, 512, 0, "ps0", 0), (512, 512, 0, "ps0", 512), (1024, 512, 1, "ps1", 0),
                                      (1536, 512, 1, "ps1", 512), (2048, 512, 2, "ps2", 0), (2560, 256, 2, "ps2", 512)):
            for k in range(8):
                g.mm(self.ps[pp][:, off:off + w], hT[:, k * 128:(k + 1) * 128], Wv[:, k, n0:n0 + w], k == 0, k == 7,
                     R=[khT, "Wb"], W=[kp_])
        g.cp(P[:, 0:1024], self.ps[0][:, :], R=["ps0"], W=[kP], eng="act")
        g.cp(P[:, 1024:2048], self.ps[1][:, :], R=["ps1"], W=[kP], eng="dve")
        g.cp(P[:, 2048:2816], self.ps[2][:, 0:768], R=["ps2"], W=[kP], eng="act")
    def e1_b(tt):
        c = 1 if tt < 2 else 0
        b = tt % 2
        qa = P[:, 2048:2688]
        qa3 = qa.rearrange("p (h d) -> p h d", d=64)
        g.act(sqt, qa, AF.Square, R=[kP], W=[ksq])
        ss10 = st_[:, 4:14]
        g.red(ss10, sqt.rearrange("p (h d) -> p h d", d=64), ALU.add, R=[ksq], W=[kst])
        g.rsqrt_small(ss10, ss10, 1.0 / 64.0, None, R=[kst], W=[kst])
        g.tt(qa3, qa3, bc(ss10, 2, 64), ALU.mult, R=[kP, kst], W=[kP])
        g.tt(qa3[:, 0:8, :], qa3[:, 0:8, :], bc(gq, 1, 8), ALU.mult, R=[kP, kgq], W=[kP])
        g.tt(qa3[:, 8:10, :], qa3[:, 8:10, :], bc(gk, 1, 2), ALU.mult, R=[kP, kgk], W=[kP])
        g.ts(P[:, 512:1024], P[:, 512:1024], 0.125, None, ALU.mult, None, R=[kP], W=[kP], eng="pool")
        qk, kqk = QKb[b]
        if c == 0:
            rope(self, P[:, 0:1024], kP, qk[:, 0:1024], kqk, 16, tt, tmps)
            rope(self, P[:, 2048:2688], kP, qk[:, 1024:1664], kqk, 10, tt, tmps)
        else:
            g.cp(qk[:, 0:1024], P[:, 0:1024], R=[kP], W=[kqk], eng="dve")
            g.cp(qk[:, 1024:1664], P[:, 2048:2688], R=[kP], W=[kqk], eng="pool")
        tm, ktm = TM[b]
        rk3 = qk[:, 512:1024].rearrange("p (h d) -> p h d", d=64)
        g.tt(tm[:, 0:512].rearrange("p (h d) -> p h d", d=64), rk3, bc(DK[:, 0:8], 2, 64), ALU.mult, R=[kqk, kDK], W=[ktm])
        g.tt(tm[:, 512:1024].rearrange("p (h d) -> p h d", d=64), rk3, bc(DK[:, 8:16], 2, 64), ALU.mult, R=[kqk, kDK], W=[ktm], eng="pool")
        g.cp(tm[:, 1024:1536], P[:, 1024:1536], R=[kP], W=[ktm], eng="pool")
        g.act(tm[:, 1536:2048], P[:, 1536:2048], AF.Silu, R=[kP], W=[ktm])
        g.cp(tm[:, 2048:2176], P[:, 2688:2816], R=[kP], W=[ktm], eng="act")
        g.st(self.TMd[tt * 128:(tt + 1) * 128, :], tm, R=[ktm], W=[f"TMd{tt}"])
    def e1_c(tt):
        c = 1 if tt < 2 else 0
        b = tt % 2
        qk, kqk = QKb[b]
        for ch in range(13):
            g.tr(psT[:, ch * 128:(ch + 1) * 128], qk[:, ch * 128:(ch + 1) * 128], self.ident_b[:], R=[kqk, "ident_b"], W=["ps3"])
        qs, kqs = QTs[b]
        g.cp(qs, psT[:, 0:1664], R=["ps3"], W=[kqs], eng="act")
        g.st(QTv[:, :, tt * 128:(tt + 1) * 128], qs.rearrange("p (c t) -> p c t", t=128), R=[kqs], W=[f"QT{tt}"])

    e1_a(0)
    for tt in range(NT):
        e1_b(tt)
        if tt + 1 < NT:
            e1_a(tt + 1)
        e1_c(tt)
    if self.cfg.get('even_stop') == 1:
        return
    A.reset()
    lgt, klg = A.alloc("lgt", 16)
    g.load_bcast(self.ret_log_rate[ei], lgt, klg)
    g.act(lgt, lgt, AF.Exp, R=[klg], W=[klg])
    g.ts(lgt, lgt, -1.0, None, ALU.mult, None, R=[klg], W=[klg])
    diff, kdf = A.alloc("diff", 128)
    ndiff, kndf = A.alloc("ndiff", 128)
    mk, kmk = A.alloc("mk", 128)
    i1, ki1 = A.alloc("i1", 128)
    i2, ki2 = A.alloc("i2", 128)
    kb.op("pool", lambda e: e.iota(diff, pattern=[[1, 128]], base=0, channel_multiplier=-1, allow_small_or_imprecise_dtypes=True), W=[kdf])
    kb.op("pool", lambda e: e.iota(ndiff, pattern=[[-1, 128]], base=0, channel_multiplier=1, allow_small_or_imprecise_dtypes=True), W=[kndf])
    kb.op("pool", lambda e: e.iota(i1, pattern=[[1, 128]], base=1, channel_multiplier=0, allow_small_or_imprecise_dtypes=True), W=[ki1])
    kb.op("pool", lambda e: e.iota(i2, pattern=[[-1, 128]], base=128, channel_multiplier=0, allow_small_or_imprecise_dtypes=True), W=[ki2])
    DT = [A.alloc(f"DT{d}", 1024) for d in range(2)]
    DQ = [A.alloc(f"DQ{d}", 512) for d in range(2)]
    dc = [A.alloc(f"dc{d}", 4) for d in range(2)]
    lgh = [A.alloc(f"lgh{d}", 4) for d in range(2)]
    for d in range(2):
        src_d, ksd = (diff, kdf) if d == 0 else (ndiff, kndf)
        g.ts(mk, diff, 0.0, None, ALU.is_ge if d == 0 else ALU.is_lt, None, R=[kdf], W=[kmk])
        dt_, kdt = DT[d]
        for h in range(8):
            sl = (h % 2) * 4 + h // 2
            g.act(dt_[:, sl * 128:(sl + 1) * 128], src_d, AF.Exp, R=[ksd, klg], W=[kdt], scale=lgt[:, d * 8 + h:d * 8 + h + 1])
        g.tt(dt_.rearrange("p (h i) -> p h i", i=128), dt_.rearrange("p (h i) -> p h i", i=128), bc(mk, 1, 8), ALU.mult,
             R=[kdt, kmk], W=[kdt])
        lh, klh = lgh[d]
        lsel = lgt[:, d * 8:(d + 1) * 8].rearrange("p (q two) -> p q two", two=2)
        g.cp(lh[0:64, :], lsel[0:64, :, 0], R=[klg], W=[klh])
        g.cp(lh[64:128, :], lsel[64:128, :, 1], R=[klg], W=[klh])
        dq, kdq = DQ[d]
        isrc, kis = (i1, ki1) if d == 0 else (i2, ki2)
        for q in range(4):
            g.act(dq[:, q * 128:(q + 1) * 128], isrc, AF.Exp, R=[kis, klh], W=[kdq], scale=lh[:, q:q + 1])
        g.act(dc[d][0], lh, AF.Exp, R=[klh], W=[dc[d][1]], scale=128.0)
    gng, kgng = A.alloc("gng", 512)
    g.load_bcast(self.ret_gn_g[ei], gng, kgng)
    QKT = [A.alloc(f"QKT{i}", 1024, BF16) for i in range(2)]
    TMt = [A.alloc(f"TMt{i}", 2176, BF16) for i in range(2)]
    PT, kPT = A.alloc("PT", 1024, BF16)
    qtl, kqtl = A.alloc("qtl", 512, BF16)
    S, kS = A.alloc("S", 256)
    Sb, kSb = A.alloc("Sb", 256, BF16)
    o32 = [A.alloc(f"o32_{i}", 512) for i in range(2)]
    oft = [A.alloc(f"of{i}", 512) for i in range(2)]
    sq2, ksq2 = A.alloc("sq2", 512)
    gs, kgs = A.alloc("gs", 32)
    mr = [A.alloc(f"mr{i}", 512, BF16) for i in range(2)]
    mrT = [A.alloc(f"mrT{i}", 512, BF16) for i in range(2)]
    psS, psO = self.ps[0], self.ps[1]
    S3 = S.rearrange("p (q e) -> p q e", e=64)
    if self.cfg.get('even_stop') == 21:
        return
    for d in range(2):
        if d == 1 and self.cfg.get('even_stop') == 22:
            return
        order = list(range(NT)) if d == 0 else [1, 0] + list(range(NT - 1, 1, -1))
        g.memset(S, 0.0, W=[kS])
        g.memset(Sb, 0.0, W=[kSb])
        koff = 0 if d == 0 else 512
        for n_i, tt in enumerate(order):
            b = n_i % 2
            qkt, kq = QKT[b]
            tm, ktm = TMt[b]
            qk3 = qkt.rearrange("p (c t) -> p c t", t=128)
            g.ld(qk3, QTv[:, 0:8, tt * 128:(tt + 1) * 128], R=[f"QT{tt}"], W=[kq])
            g.ld(tm, self.TMd[tt * 128:(tt + 1) * 128, :], R=[f"TMd{tt}"], W=[ktm])
            g.tt(qtl, qkt[:, 0:512], DQ[d][0], ALU.mult, R=[kq, DQ[d][1]], W=[kqtl])
            for h in range(8):
                par, pr = h % 2, h // 2
                sl = par * 4 + pr
                g.mm(psS[:, sl * 128:(sl + 1) * 128], qk3[par * 64:(par + 1) * 64, 4 + pr, :], qk3[par * 64:(par + 1) * 64, pr, :],
                     True, True, R=[kq], W=["ps0"])
            g.tt(PT, psS[:, :], DT[d][0], ALU.mult, R=["ps0", DT[d][1]], W=[kPT])
            for h in range(8):
                par, pr = h % 2, h // 2
                sl = par * 4 + pr
                g.mm(psO[:, h * 64:(h + 1) * 64], PT[:, sl * 128:(sl + 1) * 128], tm[:, 1024 + h * 64:1024 + (h + 1) * 64],
                     True, bool(self.cfg.get('no_acc')), R=[kPT, ktm], W=["ps1"])
                if self.cfg.get('no_acc'):
                    continue
                g.mm(psO[:, h * 64:(h + 1) * 64], qtl[par * 64:(par + 1) * 64, pr * 128:(pr + 1) * 128],
                     Sb[par * 64:(par + 1) * 64, pr * 64:(pr + 1) * 64], False, True, R=[kqtl, kSb], W=["ps1"])
            for pr in range(4):
                g.mm(psO[:, 512 + pr * 128:512 + (pr + 1) * 128], tm[:, koff + pr * 128:koff + (pr + 1) * 128],
                     tm[:, 1024 + pr * 128:1024 + (pr + 1) * 128], True, True, R=[ktm], W=["ps1u"])
            g.tt(S3, S3, bc(dc[d][0], 2, 64), ALU.mult, R=[kS, dc[d][1]], W=[kS])
            U3 = psO[:, 512:1024].rearrange("p (q e) -> p q e", e=128)
            g.tt(S3[0:64], S3[0:64], U3[0:64, :, 0:64], ALU.add, R=[kS, "ps1u"], W=[kS])
            g.tt(S3[64:128], S3[64:128], U3[64:128, :, 64:128], ALU.add, R=[kS, "ps1u"], W=[kS])
            g.cp(Sb, S, R=[kS], W=[kSb], eng="act")
            o, ko = o32[b]
            if d == 0:
                g.cp(o, psO[:, 0:512], R=["ps1"], W=[ko], eng="act")
                g.st(self.OF[tt * 128:(tt + 1) * 128, :], o, R=[ko], W=[f"OF{tt}"])
            else:
                of, kof = oft[b]
                g.ld(of, self.OF[tt * 128:(tt + 1) * 128, :], R=[f"OF{tt}"], W=[kof])
                g.tt(o, psO[:, 0:512], of, ALU.add, R=["ps1", kof], W=[ko])
                o3 = o.rearrange("p (h e) -> p h e", e=64)
                s1, s2, mean, msq, var = gs[:, 0:8], gs[:, 8:16], gs[:, 16:24], gs[:, 24:32], gs[:, 8:16]
                g.red(s1, o3, ALU.add, R=[ko], W=[kgs])
                g.act(sq2, o, AF.Square, R=[ko], W=[ksq2])
                g.red(s2, sq2.rearrange("p (h e) -> p h e", e=64), ALU.add, R=[ksq2], W=[kgs])
                g.ts(mean, s1, 1.0 / 64.0, None, ALU.mult, None, R=[kgs], W=[kgs])
                g.tt(msq, mean, mean, ALU.mult, R=[kgs], W=[kgs])
                g.stt(var, s2, 1.0 / 64.0, msq, ALU.mult, ALU.subtract, R=[kgs], W=[kgs])
                g.rsqrt_small(var, var, 1.0, None, R=[kgs], W=[kgs])
                g.tt(o3, o3, bc(mean, 2, 64), ALU.subtract, R=[ko, kgs], W=[ko])
                g.tt(o3, o3, bc(var, 2, 64), ALU.mult, R=[ko, kgs], W=[ko])
                g.tt(o, o, gng, ALU.mult, R=[ko, kgng], W=[ko], eng="pool")
                m_, km = mr[b]
                g.tt(m_, o, tm[:, 1536:2048], ALU.mult, R=[ko, ktm], W=[km], eng="pool")
                pT2 = self.ps[3].bitcast(BF16)
                for ch in range(4):
                    g.tr(pT2[:, ch * 128:(ch + 1) * 128], m_[:, ch * 128:(ch + 1) * 128], self.ident_b[:], R=[km, "ident_b"], W=["ps3"])
                mt, kmt = mrT[b]
                g.cp(mt, pT2[:, 0:512], R=["ps3"], W=[kmt], eng="act")
                g.st(MIXTv[:, 0:4, tt * 128:(tt + 1) * 128], mt.rearrange("p (c t) -> p c t", t=128), R=[kmt], W=[f"MIXT{tt}"])

    if self.cfg.get('even_stop') == 2:
        return
    A.reset()
    Kstd, kKs = A.alloc("Kstd", T, BF16)
    Kswp, kKw = A.alloc("Kswp", T, BF16)
    g.ld(Kstd, self.QT[1536:1664, :], R=["QTall"], W=[kKs])
    g.ld(Kswp[0:64, :], self.QT[1600:1664, :], R=["QTall"], W=[kKw])
    g.ld(Kswp[64:128, :], self.QT[1536:1600, :], R=["QTall"], W=[kKw])
    VA, kVA = A.alloc("VA", NT * 130, BF16)
    VA4 = VA.rearrange("p (t kv d) -> p t kv d", kv=2, d=65)
    g.memset(VA4[:, :, :, 64:65], 1.0, W=[kVA])
    for t in range(NT):
        g.ld(VA4[:, t, :, 0:64], self.TMd[t * 128:(t + 1) * 128, 2048:2176].rearrange("p (kv d) -> p kv d", kv=2), R=["TMdall"], W=[kVA])
    onesf, kon = A.alloc("onesf", 64)
    g.memset(onesf, 1.0, W=[kon])
    Qb = [A.alloc(f"Qb{i}", 2048, BF16) for i in range(2)]
    PTa = [A.alloc(f"PTa{i}", 512, BF16) for i in range(2)]
    rd, krd = A.alloc("rd", 512)
    rdb, krdb = A.alloc("rdb", 512)
    aT = [A.alloc(f"aT{i}", 512, BF16) for i in range(2)]
    blocks = [(0, 256, [0, 1])] + [(NCTX + qb * 512, 512, list(range(NT))) for qb in range(8)]
    PTa4 = PTa + [A.alloc(f"PTa{i}", 512, BF16) for i in range(2, 4)]
    cnt = [0]
    for bi, (q0, nq, ktiles) in enumerate(blocks):
        qb_, kqb = Qb[bi % 2]
        qb3 = qb_.rearrange("p (c t) -> p c t", t=512)
        g.ld(qb3[:, :, 0:nq], QTv[:, 8:12, q0:q0 + nq], R=["QTall"], W=[kqb])
        units = []
        for hp in range(4):
            for ki, kt in enumerate(ktiles):
                u = []
                for h in (2 * hp, 2 * hp + 1):
                    u.append(dict(h=h, ki=ki, kt=kt, last=(ki == len(ktiles) - 1), idx=cnt[0]))
                    cnt[0] += 1
                units.append(u)

        def sbuf_of(it):
            j = it["idx"] % 4
            t = self.ps[j % 2]
            off = (j // 2) * 512
            return t[:, off:off + 512], f"ps{j % 2}.{j // 2}"

        def emitS(it):
            h = it["h"]
            par, kv, pr = h % 2, h // 4, h // 2
            K_, kK = (Kstd, kKs) if par == kv else (Kswp, kKw)
            psSt, kps = sbuf_of(it)
            g.mm(psSt[:, 0:nq], K_[par * 64:(par + 1) * 64, it["kt"] * 128:(it["kt"] + 1) * 128], qb3[par * 64:(par + 1) * 64, pr, 0:nq],
                 True, True, R=[kK, kqb], W=[kps])

        def emitEP(it):
            h = it["h"]
            kv = h // 4
            psSt, kps = sbuf_of(it)
            psOt, kpo = (self.ps[2], "ps2") if h % 2 == 0 else (self.ps[3], "ps3")
            pa, kpa = PTa4[it["idx"] % 4]
            g.act(pa[:, 0:nq], psSt[:, 0:nq], AF.Exp, R=[kps], W=[kpa], scale=0.125)
            g.mm(psOt[0:65, 0:nq], VA4[:, it["kt"], kv, :], pa[:, 0:nq], it["ki"] == 0, it["last"], R=[kVA, kpa], W=[kpo])
            if it["last"]:
                g.recip(rd[64:65, 0:nq], psOt[64:65, 0:nq], R=[kpo], W=[krd])
                g.mm(psOt[0:64, 512:512 + nq], onesf[64:65, 0:64], rd[64:65, 0:nq], True, True, R=[kon, krd], W=[kpo + "b"])
                g.cp(rdb[0:64, 0:nq], psOt[0:64, 512:512 + nq], R=[kpo + "b"], W=[krdb], eng="act")
                at, kat = aT[h % 2]
                g.tt(at[0:64, 0:nq], psOt[0:64, 0:nq], rdb[0:64, 0:nq], ALU.mult, R=[kpo, krdb], W=[kat])
                g.st(self.MIXT[512 + h * 64:512 + (h + 1) * 64, q0:q0 + nq], at[0:64, 0:nq], R=[kat], W=[f"MIXTa{bi}_{h}"])
        for it in units[0]:
            emitS(it)
        for u_ in range(len(units)):
            if u_ + 1 < len(units):
                for it in units[u_ + 1]:
                    emitS(it)
            for it in units[u_]:
                emitEP(it)

    if self.cfg.get('even_stop') == 3:
        return
    A.reset()
    Wo, kWo = A.alloc("Wo", 8 * 1024, BF16)
    Wov = Wo.rearrange("p (k n) -> p k n", n=1024)
    stg2 = [A.alloc(f"stgo{i}", 1024) for i in range(2)]
    for k in range(0, 8, 2):
        g.ld(Wov[:, k:k + 2, :], self.mix_out_w[ei, k * 128:(k + 2) * 128, :].rearrange("(a p) n -> p a n", p=128), R=[], W=[kWo], q="pool")
    mixT = [A.alloc(f"mixT{i}", 1024, BF16) for i in range(2)]

    def d_fn(tt):
        m_, km = mixT[tt % 2]
        m3 = m_.rearrange("p (c t) -> p c t", t=128)
        g.ld(m3, MIXTv[:, :, tt * 128:(tt + 1) * 128], R=["MIXTall"], W=[km])
        for hf in range(2):
            for k in range(8):
                g.mm(self.ps[0][:, hf * 512:(hf + 1) * 512], m3[:, k, :], Wov[:, k, hf * 512:(hf + 1) * 512], k == 0, k == 7,
                     R=[km, kWo], W=["ps0"])
        return self.ps[0][:, :], "ps0"
    self.post_stage(li, l, d_fn)


Prog.setup_rope = setup_rope
Prog.even_mixer = even_mixer


def rev(ap, lo, hi):
    return ap[:, lo:hi][:, ::-1]


def s5_mixer(self, li, l):
    g = self
    kb = self.kb
    A = self.arena
    oi = self.od_idx[l]
    if not hasattr(self, "UT"):
        self.UT = g.dram_tmp("UT", [D, T], BF16)
        self.ZT = g.dram_tmp("ZT", [D, T], BF16)
    UTv = self.UT.rearrange("(c p) t -> p c t", p=128)
    ZTv = self.ZT.rearrange("(c p) t -> p c t", p=128)
    ce = ["pool", "act", "dve"]
    A.reset()
    A1 = [A.alloc(f"A1_{c}", D) for c in range(2)]
    B1 = [A.alloc(f"B1_{c}", D) for c in range(2)]
    ng, kng = A.alloc("ng", D)
    g.load_bcast(self.norm_g[li, 0], ng, kng)
    for c in range(2):
        g.load_mod(li, 1, c, *A1[c])
        g.load_mod(li, 0, c, *B1[c])
        g.stt(A1[c][0], A1[c][0], 1.0, ng, ALU.add, ALU.mult, R=[A1[c][1], kng], W=[A1[c][1]])
    xt = [A.alloc(f"xt{i}", D) for i in range(2)]
    tmp, ktmp = A.alloc("tmp", D)
    hb, khb = A.alloc("hb", D, BF16)
    hT = [A.alloc(f"hT{i}", D, BF16) for i in range(2)]
    st_, kst = A.alloc("stat", 8)
    psT = self.ps[3].bitcast(BF16)
    for tt in range(NT):
        c = 1 if tt < 2 else 0
        b = tt % 2
        x, kx = xt[b]
        g.ld(x, self.X[tt * 128:(tt + 1) * 128, :], R=[f"X{tt}"], W=[kx])
        ssq = st_[:, 0:1]
        g.act(tmp, x, AF.Square, R=[kx], W=[ktmp, kst], accum_out=ssq)
        g.rsqrt_small(ssq, ssq, 1.0 / D, None, R=[kst], W=[kst])
        g.stt(tmp, x, ssq, A1[c][0], ALU.mult, ALU.mult, R=[kx, kst, A1[c][1]], W=[ktmp])
        g.tt(hb, tmp, B1[c][0], ALU.add, R=[ktmp, B1[c][1]], W=[khb])
        for k in range(8):
            g.tr(psT[:, k * 128:(k + 1) * 128], hb[:, k * 128:(k + 1) * 128], self.ident_b[:], R=[khb, "ident_b"], W=["ps3"])
        h_, kh = hT[b]
        g.cp(h_, psT[:, 0:1024], R=["ps3"], W=[kh], eng="act")
        g.st(UTv[:, :, tt * 128:(tt + 1) * 128], h_.rearrange("p (c t) -> p c t", t=128), R=[kh], W=[f"UT{tt}"])

    A.reset()
    BT, kBT = A.alloc("BT", 64 * 128, BF16)
    BTv = BT.rearrange("p (a s) -> p a s", s=128)
    CX, kCX = A.alloc("CX", 6 * 32 * 64, BF16)
    CXv = CX.rearrange("p (a g c) -> p a g c", g=32, c=64)
    rho = [A.alloc(f"rho{d}", 32) for d in range(2)]
    rph = [A.alloc(f"rph{d}", 32) for d in range(2)]
    CN = [[A.alloc(f"cn{d}_{n}", 32) for n in range(2)] for d in range(2)]
    SN = [[A.alloc(f"sn{d}_{n}", 32) for n in range(2)] for d in range(2)]
    NSN = [[A.alloc(f"nsn{d}_{n}", 32) for n in range(2)] for d in range(2)]
    dcol, kdcol = A.alloc("dcol", 8)
    g.ld(dcol, self.s5_d[oi], R=[], W=[kdcol])
    hp, khp = A.alloc("halfpi", 1)
    g.memset(hp, math.pi / 2.0, W=[khp])
    mark = A.off
    g.memset(CX, 0.0, W=[kCX])
    brt, kbr = A.alloc("brt", 512)
    bit, kbi = A.alloc("bit", 512)
    g.ld(brt.rearrange("p (g j) -> p g j", j=16), self.s5_b_re[oi].rearrange("(gp two) p j -> (two p) gp j", two=2), R=[], W=[kbr])
    g.ld(bit.rearrange("p (g j) -> p g j", j=16), self.s5_b_im[oi].rearrange("(gp two) p j -> (two p) gp j", two=2), R=[], W=[kbi])
    br3 = brt.rearrange("p (g j) -> p g j", j=16)
    bi3 = bit.rearrange("p (g j) -> p g j", j=16)
    pt = {}
    for nm in ("are", "aim", "ldt", "lre", "dt", "mag", "ang", "r", "r2", "sn", "cs", "bre", "bim", "den", "nr", "t1", "t2", "kre", "kim"):
        pt[nm] = A.alloc("pp_" + nm, 32)
    ri, kri = A.alloc("pp_ri", 32, I32)
    bbr, kbbr = A.alloc("bbr", 512)
    bbi, kbbi = A.alloc("bbi", 512)
    tb1, ktb1 = A.alloc("tb1", 512)
    tb2, ktb2 = A.alloc("tb2", 512)
    MX, kMX = A.alloc("MX", 32 * 32, BF16)
    crt, kcr = A.alloc("crt", 512)
    cit, kci = A.alloc("cit", 512)
    psT = self.ps[3].bitcast(BF16)
    P_ = lambda n: pt[n][0]
    Kk = lambda n: pt[n][1]

    def T2(out, a, b, op):
        g.tt(P_(out), P_(a), P_(b), op, R=[Kk(a), Kk(b)], W=[Kk(out)])
    for d in range(2):
        for j, nm in enumerate(("are", "aim", "ldt")):
            g.ld(P_(nm), self.s5p[oi, d, j], R=[], W=[Kk(nm)])
        g.ts(P_("lre"), P_("are"), -1e-4, None, ALU.min, None, R=[Kk("are")], W=[Kk("lre")])
        g.act(P_("dt"), P_("ldt"), AF.Exp, R=[Kk("ldt")], W=[Kk("dt")])
        T2("mag", "lre", "dt", ALU.mult)
        g.act(P_("mag"), P_("mag"), AF.Exp, R=[Kk("mag")], W=[Kk("mag")])
        T2("ang", "aim", "dt", ALU.mult)
        g.ts(P_("r"), P_("ang"), 1.0 / TWO_PI, None, ALU.mult, None, R=[Kk("ang")], W=[Kk("r")])
        g.cp(ri, P_("r"), R=[Kk("r")], W=[kri])
        g.tt(P_("r"), P_("r"), ri, ALU.subtract, R=[Kk("r"), kri], W=[Kk("r")])
        g.act(P_("sn"), P_("r"), AF.Sin, R=[Kk("r")], W=[Kk("sn")], scale=TWO_PI)
        g.act(P_("r2"), P_("r"), AF.Abs, R=[Kk("r")], W=[Kk("r2")])
        g.act(P_("cs"), P_("r2"), AF.Sin, R=[Kk("r2"), khp], W=[Kk("cs")], scale=-TWO_PI, bias=hp)
        T2("bre", "mag", "cs", ALU.mult)
        T2("bim", "mag", "sn", ALU.mult)
        T2("den", "lre", "lre", ALU.mult)
        T2("t1", "aim", "aim", ALU.mult)
        T2("den", "den", "t1", ALU.add)
        g.recip(P_("den"), P_("den"), R=[Kk("den")], W=[Kk("den")])
        g.ts(P_("nr"), P_("bre"), -1.0, None, ALU.add, None, R=[Kk("bre")], W=[Kk("nr")])
        T2("t1", "nr", "lre", ALU.mult)
        T2("t2", "bim", "aim", ALU.mult)
        T2("kre", "t1", "t2", ALU.add)
        T2("kre", "kre", "den", ALU.mult)
        T2("t1", "bim", "lre", ALU.mult)
        T2("t2", "nr", "aim", ALU.mult)
        T2("kim", "t1", "t2", ALU.subtract)
        T2("kim", "kim", "den", ALU.mult)
        g.cp(rho[d][0], P_("mag"), R=[Kk("mag")], W=[rho[d][1]])
        g.cp(rph[d][0], P_("r"), R=[Kk("r")], W=[rph[d][1]])
        for ni, nn in enumerate((256, 512)):
            g.ts(P_("t1"), P_("r"), float(nn), None, ALU.mult, None, R=[Kk("r")], W=[Kk("t1")])
            g.cp(ri, P_("t1"), R=[Kk("t1")], W=[kri])
            g.tt(P_("t1"), P_("t1"), ri, ALU.subtract, R=[Kk("t1"), kri], W=[Kk("t1")])
            g.act(SN[d][ni][0], P_("t1"), AF.Sin, R=[Kk("t1")], W=[SN[d][ni][1]], scale=TWO_PI)
            g.act(P_("t2"), P_("t1"), AF.Abs, R=[Kk("t1")], W=[Kk("t2")])
            g.act(CN[d][ni][0], P_("t2"), AF.Sin, R=[Kk("t2"), khp], W=[CN[d][ni][1]], scale=-TWO_PI, bias=hp)
            g.ts(NSN[d][ni][0], SN[d][ni][0], -1.0, None, ALU.mult, None, R=[SN[d][ni][1]], W=[NSN[d][ni][1]])
        bb3r = bbr.rearrange("p (g j) -> p g j", j=16)
        bb3i = bbi.rearrange("p (g j) -> p g j", j=16)
        t13 = tb1.rearrange("p (g j) -> p g j", j=16)
        t23 = tb2.rearrange("p (g j) -> p g j", j=16)
        g.tt(t13, br3, bc(P_("kre"), 2, 16), ALU.mult, R=[kbr, Kk("kre")], W=[ktb1])
        g.tt(t23, bi3, bc(P_("kim"), 2, 16), ALU.mult, R=[kbi, Kk("kim")], W=[ktb2])
        g.tt(bbr, tb1, tb2, ALU.subtract, R=[ktb1, ktb2], W=[kbbr])
        g.tt(t13, bi3, bc(P_("kre"), 2, 16), ALU.mult, R=[kbi, Kk("kre")], W=[ktb1])
        g.tt(t23, br3, bc(P_("kim"), 2, 16), ALU.mult, R=[kbr, Kk("kim")], W=[ktb2])
        g.tt(bbi, tb1, tb2, ALU.add, R=[ktb1, ktb2], W=[kbbi])
        for part, (bsrc, kbs) in enumerate(((bbr, kbbr), (bbi, kbbi))):
            b4 = bsrc.rearrange("p (g two j) -> p g two j", two=2, j=16)
            for par in range(2):
                g.memset(MX, 0.0, W=[kMX])
                MX4 = MX.rearrange("p (g two c) -> p g two c", two=2, c=32)
                g.cp(MX4[0:64, :, par, 0:16], b4[0:64, :, par, :], R=[kbs], W=[kMX])
                g.cp(MX4[64:128, :, par, 16:32], b4[64:128, :, par, :], R=[kbs], W=[kMX])
                for q in range(8):
                    g.tr(psT[:, q * 128:(q + 1) * 128], MX[:, q * 128:(q + 1) * 128], self.ident_b[:], R=[kMX, "ident_b"], W=["ps3"])
                a0 = ((d * 2 + part) * 2 + par) * 8
                g.cp(BT[:, a0 * 128:(a0 + 8) * 128], psT[:, 0:1024], R=["ps3"], W=[kBT], eng="act")
        g.ld(crt.rearrange("p (g k) -> p g k", k=16), self.s5_c_re[oi, d].rearrange("(gp two) p k -> (two p) gp k", two=2), R=[], W=[kcr])
        g.ld(cit.rearrange("p (g k) -> p g k", k=16), self.s5_c_im[oi, d].rearrange("(gp two) p k -> (two p) gp k", two=2), R=[], W=[kci])
        for part, (csrc, kcs, sgn) in enumerate(((crt, kcr, 1.0), (cit, kci, -1.0), (crt, kcr, -1.0))):
            c4 = csrc.rearrange("p (g two k) -> p g two k", two=2, k=16)
            Cv = CXv[:, d * 3 + part].rearrange("p (g two) c -> p g two c", two=2)
            for gpar in range(2):
                g.ts(Cv[0:64, :, gpar, gpar * 32:gpar * 32 + 16], c4[0:64, :, gpar, :], sgn, None, ALU.mult, None, R=[kcs], W=[kCX])
                g.ts(Cv[64:128, :, gpar, gpar * 32 + 16:gpar * 32 + 32], c4[64:128, :, gpar, :], sgn, None, ALU.mult, None, R=[kcs], W=[kCX])
    if self.cfg.get("s5_stop") == 1:
        return
    kb.barrier()
    A.off = mark
    iot, kio = A.alloc("iota", T)
    kb.op("pool", lambda e: e.iota(iot, pattern=[[1, T]], base=0, channel_multiplier=0, allow_small_or_imprecise_dtypes=True), W=[kio])
    uT = [A.alloc("uT0", T, BF16)] * 2
    ysb, kys = A.alloc("ysb", T)
    NB = 512
    TI, kti = A.alloc("TI", NB, I32)
    TFt = [A.alloc(f"TF{i}", NB) for i in range(2)]
    ST = [A.alloc(f"ST{i}", NB) for i in range(2)]
    CT = [A.alloc(f"CT{i}", NB) for i in range(2)]
    W1 = [A.alloc(f"w1_{i}", NB, BF16) for i in range(2)]
    W2 = [A.alloc(f"w2_{i}", NB, BF16) for i in range(2)]
    W3 = [A.alloc(f"w3_{i}", NB, BF16) for i in range(2)]
    W4 = [A.alloc(f"w4_{i}", NB, BF16) for i in range(2)]
    BTR = [A.alloc(f"btr{i}", NB, BF16) for i in range(2)]
    BTI = [A.alloc(f"bti{i}", NB, BF16) for i in range(2)]
    WR = [A.alloc(f"wr{i}", NB) for i in range(2)]
    WI = [A.alloc(f"wi{i}", NB) for i in range(2)]
    P1 = [A.alloc(f"p1_{i}", NB, BF16) for i in range(2)]
    P2 = [A.alloc(f"p2_{i}", NB, BF16) for i in range(2)]
    P3 = [A.alloc(f"p3_{i}", NB, BF16) for i in range(2)]
    P4 = [A.alloc(f"p4_{i}", NB, BF16) for i in range(2)]
    YE = [A.alloc(f"ye{i}", NB) for i in range(2)]
    CAR = [A.alloc(f"carry{i}", 4) for i in range(2)]
    zt, kzt = uT[0]
    lat = [(NCTX + i * NB, NCTX + (i + 1) * NB) for i in range(NLAT // NB)]
    fblocks = [(0, NCTX, False)] + [(lo, hi, False) for (lo, hi) in lat]
    bblocks = [(0, NCTX, True)] + [(lo, hi, True) for (lo, hi) in reversed(lat)]
    gcount = [0]
    for q in range(8):
        u_, ku = uT[q % 2]
        g.ld(u_, self.UT[q * 128:(q + 1) * 128, :], R=["UTall"], W=[ku])
        g.ts(ysb, u_, dcol[:, q:q + 1], None, ALU.mult, None, R=[ku, kdcol], W=[kys])
        blist = []
        for gl in range(4):
            for d in range(2):
                gi = gcount[0]
                gcount[0] += 1
                for bidx, (lo, hi, rv) in enumerate(fblocks if d == 0 else bblocks):
                    blist.append(dict(gl=gl, d=d, gi=gi, bidx=bidx, lo=lo, hi=hi, rv=rv, i=len(blist)))
        for i_, bl in enumerate(blist):
            bl["prev"] = blist[i_ - 1] if bl["bidx"] > 0 else None

        def tokf(bl):
            lo, hi = bl["lo"], bl["hi"]
            return (lambda ap: rev(ap, lo, hi)) if bl["rv"] else (lambda ap: ap[:, lo:hi])

        def stageA(bl):
            gl, d, gi, b = bl["gl"], bl["d"], bl["gi"], bl["i"] % 2
            gp = 4 * q + gl
            half, par = gl // 2, gl % 2
            rows = slice(64 * half, 64 * half + 64)
            n = bl["hi"] - bl["lo"]
            tb = gi % 2
            st, kst2 = ST[tb]
            ct, kct = CT[tb]
            if bl["bidx"] == 0:
                tf, ktf = TFt[tb]
                rcol = rph[d][0][:, gp:gp + 1]
                g.ts(TI, iot[:, 0:NB], rcol, None, ALU.mult, None, R=[kio, rph[d][1]], W=[kti])
                g.stt(tf, iot[:, 0:NB], rcol, TI, ALU.mult, ALU.subtract, R=[kio, rph[d][1], kti], W=[ktf])
                g.act(st, tf, AF.Sin, R=[ktf], W=[kst2], scale=TWO_PI)
                g.act(tf, tf, AF.Abs, R=[ktf], W=[ktf])
                g.act(ct, tf, AF.Sin, R=[ktf, khp], W=[kct], scale=-TWO_PI, bias=hp)
            psBr = self.ps[0][:, b * 512:b * 512 + 512]
            psBi = self.ps[1][:, b * 512:b * 512 + 512]
            k0, k1 = f"ps0.{b}", f"ps1.{b}"
            tok = tokf(bl)
            for part, (pp, kpp) in enumerate(((psBr, k0), (psBi, k1))):
                a0 = ((d * 2 + part) * 2 + par) * 8 + q
                g.mm(pp[:, 0:n], BTv[rows, a0, :], tok(u_[rows, :]), True, True, R=[kBT, ku], W=[kpp])
            brs, kbrs = psBr, k0
            bis, kbis = psBi, k1
            w1, kw1 = W1[b]
            w2, kw2 = W2[b]
            w3, kw3 = W3[b]
            w4, kw4 = W4[b]
            btr, kbtr = BTR[b]
            bti, kbti = BTI[b]
            g.tt(w1[:, 0:n], brs[:, 0:n], ct[:, 0:n], ALU.mult, R=[kbrs, kct], W=[kw1])
            g.tt(w2[:, 0:n], bis[:, 0:n], st[:, 0:n], ALU.mult, R=[kbis, kst2], W=[kw2])
            g.tt(btr[:, 0:n], w1[:, 0:n], w2[:, 0:n], ALU.add, R=[kw1, kw2], W=[kbtr], eng="pool")
            g.tt(w3[:, 0:n], bis[:, 0:n], ct[:, 0:n], ALU.mult, R=[kbis, kct], W=[kw3])
            g.tt(w4[:, 0:n], brs[:, 0:n], st[:, 0:n], ALU.mult, R=[kbrs, kst2], W=[kw4])
            g.tt(bti[:, 0:n], w3[:, 0:n], w4[:, 0:n], ALU.subtract, R=[kw3, kw4], W=[kbti], eng="pool")

        def stageB(bl):
            gl, d, gi, b = bl["gl"], bl["d"], bl["gi"], bl["i"] % 2
            gp = 4 * q + gl
            half = gl // 2
            rows = slice(64 * half, 64 * half + 64)
            n = bl["hi"] - bl["lo"]
            tb = gi % 2
            st, kst2 = ST[tb]
            ct, kct = CT[tb]
            btr, kbtr = BTR[b]
            bti, kbti = BTI[b]
            wr, kwr = WR[b]
            wi, kwi = WI[b]
            car, kcar = CAR[b]
            pv = bl["prev"]
            if pv is None:
                g.cp(self.ps[3][:, 0:512], st, R=[kst2], W=["ps3.S"], eng="act")
                g.cp(self.ps[3][:, 512:1024], ct, R=[kct], W=["ps3.C"], eng="act")
                ini_r, ini_i, Rc = 0.0, 0.0, []
            else:
                pb = pv["i"] % 2
                npv = pv["hi"] - pv["lo"]
                ni = 0 if npv == 256 else 1
                wrl = WR[pb][0][:, npv - 1:npv]
                wil = WI[pb][0][:, npv - 1:npv]
                cn = CN[d][ni][0][:, gp:gp + 1]
                sn = SN[d][ni][0][:, gp:gp + 1]
                nsn = NSN[d][ni][0][:, gp:gp + 1]
                Rk = [WR[pb][1], WI[pb][1], CN[d][ni][1], SN[d][ni][1], NSN[d][ni][1]]
                g.act(car[:, 2:3], wil, AF.Copy, R=Rk, W=[kcar], scale=nsn)
                g.act(car[:, 3:4], wil, AF.Copy, R=Rk + [kcar], W=[kcar], scale=cn)
                g.act(car[:, 0:1], wrl, AF.Identity, R=Rk + [kcar], W=[kcar], scale=cn, bias=car[:, 2:3])
                g.act(car[:, 1:2], wrl, AF.Identity, R=Rk + [kcar], W=[kcar], scale=sn, bias=car[:, 3:4])
                ini_r, ini_i, Rc = car[:, 0:1], car[:, 1:2], [kcar]
            rb = rho[d][0][:, gp:gp + 1].to_broadcast([128, n])
            kb.op("dve", lambda e: e.tensor_tensor_scan(out=wr[:, 0:n], data0=rb, data1=btr[:, 0:n], initial=ini_r,
                                                        op0=ALU.mult, op1=ALU.add), R=[rho[d][1], kbtr] + Rc, W=[kwr])
            kb.op("dve", lambda e: e.tensor_tensor_scan(out=wi[:, 0:n], data0=rb, data1=bti[:, 0:n], initial=ini_i,
                                                        op0=ALU.mult, op1=ALU.add), R=[rho[d][1], kbti] + Rc, W=[kwi])
            p1, kp1 = P1[b]
            p2, kp2 = P2[b]
            p3, kp3 = P3[b]
            p4, kp4 = P4[b]
            pS, pC = self.ps[3][:, 0:512], self.ps[3][:, 512:1024]
            g.tt(p1[:, 0:n], wr[:, 0:n], pC[:, 0:n], ALU.mult, R=[kwr, "ps3.C"], W=[kp1])
            g.tt(p2[:, 0:n], wi[:, 0:n], pS[:, 0:n], ALU.mult, R=[kwi, "ps3.S"], W=[kp2])
            g.tt(p3[:, 0:n], wr[:, 0:n], st[:, 0:n], ALU.mult, R=[kwr, kst2], W=[kp3], eng="pool")
            g.tt(p4[:, 0:n], wi[:, 0:n], ct[:, 0:n], ALU.mult, R=[kwi, kct], W=[kp4], eng="pool")
            psY = self.ps[2][:, b * 512:b * 512 + 512]
            k2 = f"ps2.{b}"
            g.mm(psY[rows, 0:n], CXv[:, d * 3 + 0, gp, :], p1[:, 0:n], True, False, R=[kCX, kp1], W=[k2])
            g.mm(psY[rows, 0:n], CXv[:, d * 3 + 2, gp, :], p2[:, 0:n], False, False, R=[kCX, kp2], W=[k2])
            g.mm(psY[rows, 0:n], CXv[:, d * 3 + 1, gp, :], p3[:, 0:n], False, False, R=[kCX, kp3], W=[k2])
            g.mm(psY[rows, 0:n], CXv[:, d * 3 + 1, gp, :], p4[:, 0:n], False, True, R=[kCX, kp4], W=[k2])

        def stageC(bl):
            gl, b = bl["gl"], bl["i"] % 2
            half = gl // 2
            rows = slice(64 * half, 64 * half + 64)
            n = bl["hi"] - bl["lo"]
            psY = self.ps[2][:, b * 512:b * 512 + 512]
            k2 = f"ps2.{b}"
            tok = tokf(bl)
            g.tt(tok(ysb[rows, :]), tok(ysb[rows, :]), psY[rows, 0:n], ALU.add, R=[kys, k2], W=[kys])

        stageA(blist[0])
        for i_ in range(len(blist)):
            if i_ + 1 < len(blist):
                stageA(blist[i_ + 1])
            stageB(blist[i_])
            if i_ >= 1:
                stageC(blist[i_ - 1])
        stageC(blist[-1])
        g.act(iot, ysb, AF.Square, R=[kys], W=[kio + "g"])
        g.ts(iot, iot, 0.044715, 1.0, ALU.mult, ALU.add, R=[kio + "g"], W=[kio + "g"])
        g.tt(iot, iot, ysb, ALU.mult, R=[kio + "g", kys], W=[kio + "g"])
        g.act(iot, iot, AF.Tanh, R=[kio + "g"], W=[kio + "g"], scale=math.sqrt(2.0 / math.pi))
        g.stt(iot, iot, 1.0, ysb, ALU.add, ALU.mult, R=[kio + "g", kys], W=[kio + "g"])
        g.act(zt, iot, AF.Copy, R=[kio + "g"], W=[kzt], scale=0.5)
        g.st(self.ZT[q * 128:(q + 1) * 128, :], zt, R=[kzt], W=[f"ZT{q}"])
        if q < 7:
            kb.op("pool", lambda e: e.iota(iot, pattern=[[1, T]], base=0, channel_multiplier=0, allow_small_or_imprecise_dtypes=True),
                  R=[kio + "g"], W=[kio, kio + "g"])
    if self.cfg.get("s5_stop") == 2:
        return
    A.reset()
    GW, kGW = A.alloc("GW", 8 * 2048, BF16)
    GWv = GW.rearrange("p (k n) -> p k n", n=2048)
    stg = [A.alloc(f"stgg{i}", 1024) for i in range(2)]
    for k in range(8):
        g.ld(GWv[:, k, :], self.s5_glu_w[oi, k * 128:(k + 1) * 128, :], R=[], W=[kGW], q="pool")
    gb, kgb = A.alloc("gb", 2048)
    g.load_bcast(self.s5_glu_b[oi], gb, kgb)
    zT = [A.alloc(f"zT{i}", 1024, BF16) for i in range(2)]
    asb, kasb = A.alloc("asb", 1024)
    gsb, kgsb = A.alloc("gsb", 1024)
    dt_, kdt = A.alloc("dtile", 1024)

    def d_fn(tt):
        z_, kz = zT[tt % 2]
        z3 = z_.rearrange("p (c t) -> p c t", t=128)
        g.ld(z3, ZTv[:, :, tt * 128:(tt + 1) * 128], R=["ZTall"], W=[kz])
        for ch in range(4):
            pp, kpp = (self.ps[0], "ps0") if ch < 2 else (self.ps[3], "ps3")
            for k in range(8):
                g.mm(pp[:, (ch % 2) * 512:(ch % 2 + 1) * 512], z3[:, k, :], GWv[:, k, ch * 512:(ch + 1) * 512], k == 0, k == 7,
                     R=[kz, kGW], W=[kpp])
        g.tt(asb, self.ps[0][:, :], gb[:, 0:1024], ALU.add, R=["ps0", kgb], W=[kasb])
        g.tt(gsb, self.ps[3][:, :], gb[:, 1024:2048], ALU.add, R=["ps3", kgb], W=[kgsb])
        g.act(gsb, gsb, AF.Sigmoid, R=[kgsb], W=[kgsb])
        g.tt(dt_, asb, gsb, ALU.mult, R=[kasb, kgsb], W=[kdt], eng="pool")
        return dt_, kdt
    self.post_stage(li, l, d_fn)


Prog.s5_mixer = s5_mixer


def shared_weights(inp, layers):
    ev = [l // 2 for l in layers if l % 2 == 0]
    od = [l // 2 for l in layers if l % 2 == 1]
    f = lambda a: np.ascontiguousarray(np.asarray(a, dtype=np.float32))
    w = {}
    w["mod_w"] = f(inp["mod_w"][layers])
    w["mod_b"] = f(inp["mod_b"][layers])
    w["norm_g"] = f(inp["norm_g"][layers])
    w["moe_router_w"] = f(inp["moe_router_w"][layers])
    w["moe_w1"] = f(inp["moe_w1"][layers])
    w["moe_w3"] = f(inp["moe_w3"][layers])
    w["moe_w2"] = f(inp["moe_w2"][layers])
    w["final_norm_g"] = f(inp["final_norm_g"])
    if ev:
        w["mix_in_w"] = f(inp["mix_in_w"][ev])
        w["mix_out_w"] = f(inp["mix_out_w"][ev])
        w["ret_log_rate"] = f(np.asarray(inp["ret_log_rate"])[ev].reshape(len(ev), 16))
        w["ret_gn_g"] = f(inp["ret_gn_g"][ev])
        w["qk_norm_g"] = f(np.asarray(inp["qk_norm_g"])[ev].reshape(len(ev), 128))
    else:
        w["mix_in_w"] = np.zeros((1, D, 2816), np.float32)
        w["mix_out_w"] = np.zeros((1, D, D), np.float32)
        w["ret_log_rate"] = np.zeros((1, 16), np.float32)
        w["ret_gn_g"] = np.zeros((1, 512), np.float32)
        w["qk_norm_g"] = np.zeros((1, 128), np.float32)
    if od:
        no = len(od)
        a_re = np.asarray(inp["s5_a_re"])[od]
        a_im = np.asarray(inp["s5_a_im"])[od]
        ldt = np.asarray(inp["s5_log_dt"])[od]
        pair = lambda a: a.reshape(no, 2, 32, 2, 64).transpose(0, 1, 3, 4, 2).reshape(no, 2, 128, 32)
        ldt_b = np.broadcast_to(ldt[..., None], (no, 2, 64, 64))
        w["s5p"] = f(np.stack([pair(a_re), pair(a_im), pair(ldt_b)], axis=2))
        w["s5_b_re"] = f(inp["s5_b_re"][od])
        w["s5_b_im"] = f(inp["s5_b_im"][od])
        w["s5_c_re"] = f(np.asarray(inp["s5_c_re"])[od].transpose(0, 1, 2, 4, 3))
        w["s5_c_im"] = f(np.asarray(inp["s5_c_im"])[od].transpose(0, 1, 2, 4, 3))
        w["s5_d"] = f(np.asarray(inp["s5_d"])[od].reshape(no, 8, 128).transpose(0, 2, 1))
        w["s5_glu_w"] = f(inp["s5_glu_w"][od])
        w["s5_glu_b"] = f(inp["s5_glu_b"][od])
    else:
        w["s5p"] = np.zeros((1, 2, 3, 128, 32), np.float32)
        w["s5_b_re"] = np.zeros((1, 64, 64, 16), np.float32)
        w["s5_b_im"] = np.zeros((1, 64, 64, 16), np.float32)
        w["s5_c_re"] = np.zeros((1, 2, 64, 64, 16), np.float32)
        w["s5_c_im"] = np.zeros((1, 2, 64, 64, 16), np.float32)
        w["s5_d"] = np.zeros((1, 128, 8), np.float32)
        w["s5_glu_w"] = np.zeros((1, D, 2 * D), np.float32)
        w["s5_glu_b"] = np.zeros((1, 2 * D), np.float32)
    return w


def core_inputs(inp, b, w):
    m = dict(w)
    x = np.asarray(inp["x"][b], dtype=np.float32)
    ctx = np.asarray(inp["ctx"][b], dtype=np.float32)
    m["xin"] = np.ascontiguousarray(np.concatenate([ctx, x], axis=0))
    c = np.asarray(inp["c"][b], dtype=np.float32).reshape(8, 128).T
    cc = np.asarray(inp["c_ctx"], dtype=np.float32).reshape(8, 128).T
    m["cin"] = np.ascontiguousarray(np.stack([c, cc], axis=2).reshape(128, 16))
    return m


def kernel(**inputs):
    layers = [0, 1, 2, 3]
    prog = Prog(dict(layers=layers))
    nc = prog.build()
    w = shared_weights(inputs, layers)
    in_maps = [core_inputs(inputs, b, w) for b in range(8)]
    res = run_bass_kernel_spmd(nc, in_maps, core_ids=list(range(8)))
    return np.stack([np.asarray(r["out"], dtype=np.float32) for r in res.results], axis=0)
```

```python
import contextlib
import math
import numpy as np
import concourse.bass as bass
import concourse.mybir as mybir
from concourse.bass_utils import run_bass_kernel_spmd

F32 = mybir.dt.float32
BF16 = mybir.dt.bfloat16
I32 = mybir.dt.int32
U32 = mybir.dt.uint32
ALU = mybir.AluOpType
AF = mybir.ActivationFunctionType
AX = mybir.AxisListType

SEM_LIMIT = 30000
NSLOT = 10

D = 1024
NCTX = 256
NLAT = 4096
T = NCTX + NLAT
NT = T // 128
EPS = 1e-6
TWO_PI = 2.0 * math.pi


class KB:
    ENG = ("pe", "act", "dve", "pool", "sp")

    def __init__(self, nc):
        self.nc = nc
        self.stack = contextlib.ExitStack()
        self.q = {e: [] for e in self.ENG}
        self.nsem = 0
        self.cur = {}
        for e in ("pe", "act", "dve", "pool"):
            self.cur[e] = [self._newsem(e), 0]
        self.slots = {}
        self.slot_i = {}
        for e in ("sp", "pool"):
            self.slots[e] = [[self._newsem("d" + e), 0] for _ in range(NSLOT)]
            self.slot_i[e] = 0
        self.known = {e: {} for e in self.ENG}
        self.last_w = {}
        self.reads = {}
        self.pending = {e: [] for e in self.ENG}
        self.ninst = 0

    def _newsem(self, tag):
        self.nsem += 1
        return self.stack.enter_context(self.nc.semaphore(f"s_{tag}_{self.nsem}"))

    def sbuf(self, name, shape, dtype):
        return self.stack.enter_context(self.nc.sbuf_tensor(name, list(shape), dtype))

    def psum(self, name, shape, dtype):
        return self.stack.enter_context(self.nc.psum_tensor(name, list(shape), dtype))

    def all_tokens(self):
        toks = []
        for e in ("pe", "act", "dve", "pool"):
            c = self.cur[e]
            if c[1] > 0:
                toks.append((c[0], c[1]))
        for e in self.slots:
            for s in self.slots[e]:
                if s[1] > 0:
                    toks.append((s[0], s[1]))
        return toks

    def barrier(self):
        toks = self.all_tokens()
        for e in self.ENG:
            self.pending[e] = list(toks)
        self.last_w = {}
        self.reads = {}

    def _deps(self, eng, R, W):
        need = {}

        def add(tok):
            if tok is None:
                return
            sem, val = tok
            k = id(sem)
            if k not in need or need[k][1] < val:
                need[k] = (sem, val)
        for tok in self.pending[eng]:
            add(tok)
        self.pending[eng] = []
        for r in R:
            add(self.last_w.get(r))
        for w in W:
            add(self.last_w.get(w))
            for t in self.reads.get(w, {}).values():
                add(t)
        out = []
        kn = self.known[eng]
        for k, (sem, val) in need.items():
            if kn.get(k, 0) >= val:
                continue
            kn[k] = val
            out.append((sem, val))
        return out

    def _commit(self, tok, R, W, tag):
        for w in W:
            self.last_w[w] = tok
            self.reads[w] = {}
        for r in R:
            if r in W:
                continue
            self.reads.setdefault(r, {})[tag] = tok

    def op(self, eng, fn, R=(), W=()):
        R = tuple(R)
        W = tuple(W)
        waits = self._deps(eng, R, W)
        c = self.cur[eng]
        if c[1] >= SEM_LIMIT:
            c[0] = self._newsem(eng)
            c[1] = 0
        c[1] += 1
        sem, val = c[0], c[1]
        if eng == "pe":
            self.known[eng][id(sem)] = val
        self.q[eng].append((waits, fn, sem, 1))
        self._commit((sem, val), R, W, eng)
        self.ninst += 1

    def dma(self, qeng, fn, R=(), W=()):
        R = tuple(R)
        W = tuple(W)
        waits = self._deps(qeng, R, W)
        i = self.slot_i[qeng]
        self.slot_i[qeng] = (i + 1) % NSLOT
        s = self.slots[qeng][i]
        kn = self.known[qeng]
        if s[1] > 0 and kn.get(id(s[0]), 0) < s[1]:
            waits.append((s[0], s[1]))
            kn[id(s[0])] = s[1]
        s[1] += 16
        self.q[qeng].append((waits, fn, s[0], 16))
        self._commit((s[0], s[1]), R, W, ("dma", qeng, i))
        self.ninst += 1

    def emit(self):
        nc = self.nc
        finals = self.all_tokens()
        with nc.Block() as block:
            def run(engname):
                def body(e):
                    for waits, fn, sem, inc in self.q[engname]:
                        for (ws, wv) in waits:
                            e.wait_ge(ws, wv)
                        fn(e).then_inc(sem, inc)
                    if engname == "sp":
                        for (ws, wv) in finals:
                            e.wait_ge(ws, wv)
                return body
            block.sync(run("sp"))
            block.tensor(run("pe"))
            block.scalar(run("act"))
            block.vector(run("dve"))
            block.gpsimd(run("pool"))


class Arena:
    def __init__(self, kb, words):
        self.kb = kb
        self.words = words
        self.t = kb.sbuf("arena", [128, words], F32)
        self.off = 0
        self.phase = 0

    def reset(self):
        self.kb.barrier()
        self.off = 0
        self.phase += 1

    def alloc(self, name, cols, dtype=F32, parts=128):
        w = cols if dtype in (F32, I32, U32) else (cols + 1) // 2
        w = (w + 7) // 8 * 8
        assert self.off + w <= self.words, f"arena overflow at {name}: {self.off}+{w}>{self.words}"
        ap = self.t[0:parts, self.off:self.off + w]
        self.off += w
        if dtype != F32:
            ap = ap.bitcast(dtype)
        ap = ap[:, 0:cols]
        return ap, f"p{self.phase}.{name}"


class Gen:
    def __init__(self, cfg):
        self.cfg = cfg
        self.nc = bass.Bass("TRN2", target_bir_lowering=False)
        self.kb = KB(self.nc)
        self.dbg = {}

    def mm(self, out, lhsT, rhs, start, stop, R, W):
        self.kb.op("pe", lambda e: e.matmul(out, lhsT=lhsT, rhs=rhs, start=start, stop=stop), R=R, W=W)

    def tr(self, out, in_, ident, R, W):
        self.kb.op("pe", lambda e: e.transpose(out=out, in_=in_, identity=ident), R=R, W=W)

    def act(self, out, in_, func, R, W, **kw):
        self.kb.op("act", lambda e: e.activation(out=out, in_=in_, func=func, **kw), R=R, W=W)

    def tt(self, out, a, b, op, R, W, eng="dve"):
        self.kb.op(eng, lambda e: e.tensor_tensor(out=out, in0=a, in1=b, op=op), R=R, W=W)

    def ts(self, out, in0, s1, s2, op0, op1, R, W, eng="dve", **kw):
        if s2 is None:
            self.kb.op(eng, lambda e: e.tensor_scalar(out=out, in0=in0, scalar1=s1, scalar2=None, op0=op0, **kw), R=R, W=W)
        else:
            self.kb.op(eng, lambda e: e.tensor_scalar(out=out, in0=in0, scalar1=s1, scalar2=s2, op0=op0, op1=op1, **kw), R=R, W=W)

    def stt(self, out, in0, scalar, in1, op0, op1, R, W):
        self.kb.op("dve", lambda e: e.scalar_tensor_tensor(out=out, in0=in0, scalar=scalar, in1=in1, op0=op0, op1=op1), R=R, W=W)

    def cp(self, out, in_, R, W, eng="dve"):
        if eng == "act":
            self.kb.op("act", lambda e: e.copy(out=out, in_=in_), R=R, W=W)
        else:
            self.kb.op(eng, lambda e: e.tensor_copy(out=out, in_=in_), R=R, W=W)

    def memset(self, out, val, W, eng="dve"):
        self.kb.op(eng, lambda e: e.memset(out, val), W=W)

    def ld(self, out, in_, R, W, q="sp"):
        self.kb.dma(q, lambda e: e.dma_start(out=out, in_=in_), R=R, W=W)

    def st(self, out, in_, R, W, q="pool"):
        self.kb.dma(q, lambda e: e.dma_start(out=out, in_=in_), R=R, W=W)

    def red(self, out, in_, op, R, W, axis=AX.X):
        self.kb.op("dve", lambda e: e.tensor_reduce(out=out, in_=in_, axis=axis, op=op), R=R, W=W)

    def recip(self, out, in_, R, W):
        self.kb.op("dve", lambda e: e.reciprocal(out=out, in_=in_), R=R, W=W)

    def rsqrt_small(self, out, in_, scale, tmpkey, R, W):
        self.ts(out, in_, scale, EPS, ALU.mult, ALU.add, R=R, W=W)
        self.act(out, out, AF.Sqrt, R=W, W=W)
        self.recip(out, out, R=W, W=W)

    def dram_in(self, name, shape, dtype=F32):
        return self.nc.dram_tensor(name, list(shape), dtype, kind="ExternalInput").ap()

    def dram_out(self, name, shape, dtype=F32):
        return self.nc.dram_tensor(name, list(shape), dtype, kind="ExternalOutput").ap()

    def dram_tmp(self, name, shape, dtype=F32):
        if self.cfg.get("dbg_" + name):
            ap = self.nc.dram_tensor(name, list(shape), dtype, kind="ExternalOutput").ap()
            self.dbg[name] = ap
            return ap
        return self.nc.dram_tensor(name, list(shape), dtype, kind="Internal").ap()


class Prog(Gen):
    def __init__(self, cfg):
        super().__init__(cfg)
        g = self
        layers = cfg["layers"]
        self.layers = layers
        nl = len(layers)
        ev = [l for l in layers if l % 2 == 0]
        od = [l for l in layers if l % 2 == 1]
        self.ev_idx = {l: i for i, l in enumerate(ev)}
        self.od_idx = {l: i for i, l in enumerate(od)}
        ne, no = max(len(ev), 1), max(len(od), 1)
        self.xin = g.dram_in("xin", [T, D])
        self.cin = g.dram_in("cin", [128, 16])
        self.mod_w = g.dram_in("mod_w", [nl, D, 6 * D])
        self.mod_b = g.dram_in("mod_b", [nl, 6 * D])
        self.norm_g = g.dram_in("norm_g", [nl, 2, D])
        self.mix_in_w = g.dram_in("mix_in_w", [ne, D, 2816])
        self.mix_out_w = g.dram_in("mix_out_w", [ne, D, D])
        self.ret_log_rate = g.dram_in("ret_log_rate", [ne, 16])
        self.ret_gn_g = g.dram_in("ret_gn_g", [ne, 512])
        self.qk_norm_g = g.dram_in("qk_norm_g", [ne, 128])
        self.s5p = g.dram_in("s5p", [no, 2, 3, 128, 32])
        self.s5_b_re = g.dram_in("s5_b_re", [no, 64, 64, 16])
        self.s5_b_im = g.dram_in("s5_b_im", [no, 64, 64, 16])
        self.s5_c_re = g.dram_in("s5_c_re", [no, 2, 64, 64, 16])
        self.s5_c_im = g.dram_in("s5_c_im", [no, 2, 64, 64, 16])
        self.s5_d = g.dram_in("s5_d", [no, 128, 8])
        self.s5_glu_w = g.dram_in("s5_glu_w", [no, D, 2 * D])
        self.s5_glu_b = g.dram_in("s5_glu_b", [no, 2 * D])
        self.moe_router_w = g.dram_in("moe_router_w", [nl, D, 16])
        self.moe_w1 = g.dram_in("moe_w1", [nl, 16, D, 2 * D])
        self.moe_w3 = g.dram_in("moe_w3", [nl, 16, D, 2 * D])
        self.moe_w2 = g.dram_in("moe_w2", [nl, 16, 2 * D, D])
        self.final_norm_g = g.dram_in("final_norm_g", [D])
        self.out = g.dram_out("out", [NLAT, D])
        self.X = g.dram_tmp("X", [T, D])
        self.Fb = g.dram_tmp("Fb", [T, D], BF16)
        self.MOD = g.dram_tmp("MOD", [nl, 2, 6 * D])
        self.AFF = g.dram_tmp("AFF", [16, T])
        kb = self.kb
        self.arena = Arena(kb, cfg.get("arena_words", 40448))
        self.ident_f = kb.sbuf("ident_f", [128, 128], F32)
        self.ident_b = kb.sbuf("ident_b", [128, 128], BF16)
        self.IDXI = kb.sbuf("IDXI", [128, 80], I32)
        self.GVT = kb.sbuf("GVT", [128, 80], F32)
        self.ps = [kb.psum(f"ps{i}", [128, 1024], F32) for i in range(4)]
        self.psk = [f"ps{i}" for i in range(4)]
        kb.op("pool", lambda e: e.iota(self.ident_f[:], pattern=[[1, 128]], base=0, channel_multiplier=-1,
                                       allow_small_or_imprecise_dtypes=True), W=["ident_f"])
        g.kb.op("dve", lambda e: e.tensor_single_scalar(out=self.ident_f[:], in_=self.ident_f[:], scalar=0.0, op=ALU.is_equal),
                R=["ident_f"], W=["ident_f"])
        g.cp(self.ident_b[:], self.ident_f[:], R=["ident_f"], W=["ident_b"])

    def phase0(self):
        g = self
        A = self.arena
        A.reset()
        for i in range(4):
            r0 = i * (T // 4)
            g.ld(self.X[r0:r0 + T // 4, :], self.xin[r0:r0 + T // 4, :], R=[], W=[f"Xinit{i}"])
        cs, kcs = A.alloc("cs", 16)
        g.ld(cs, self.cin, R=[], W=[kcs])
        g.act(cs, cs, AF.Silu, R=[kcs], W=[kcs])
        cs3 = cs.rearrange("p (k c) -> p k c", c=2)
        mb, kmb = A.alloc("mb", 6144, parts=2)
        m2, km2 = A.alloc("m2", 6144, parts=2)
        stg = [A.alloc(f"stg{i}", 4096) for i in range(2)]
        for li in range(len(self.layers)):
            g.ld(mb, self.mod_b[li].partition_broadcast(2), R=[], W=[kmb])
            for n in range(12):
                s, ks = stg[n % 2]
                s3 = s.rearrange("p (k n) -> p k n", n=512)
                g.ld(s3, self.mod_w[li][:, n * 512:(n + 1) * 512].rearrange("(k p) n -> p k n", p=128), R=[], W=[ks])
                for k in range(8):
                    g.mm(self.ps[0][0:2, 0:512], cs3[:, k, :], s3[:, k, :], k == 0, k == 7, R=[kcs, ks], W=["ps0"])
                g.tt(m2[:, n * 512:(n + 1) * 512], self.ps[0][0:2, 0:512], mb[:, n * 512:(n + 1) * 512], ALU.add,
                     R=["ps0", kmb], W=[km2])
            g.st(self.MOD[li], m2, R=[km2], W=[f"MOD{li}"])

    def load_mod(self, li, which, isctx, dst, kdst):
        self.ld(dst, self.MOD[li, isctx, which * D:(which + 1) * D].partition_broadcast(128), R=[], W=[kdst])

    def load_bcast(self, vec_ap, dst, kdst, n=128):
        self.ld(dst, vec_ap.partition_broadcast(n), R=[], W=[kdst])

    def norm_tile(self, x, kx, ssq, kss, junk, kjunk):
        g = self
        g.act(junk, x, AF.Square, R=[kx], W=[kjunk, kss], accum_out=ssq)
        g.rsqrt_small(ssq, ssq, 1.0 / D, None, R=[kss], W=[kss])

    def post_stage(self, li, l, d_fn, alloc_extra=None):
        g = self
        A = self.arena
        G1 = [A.alloc(f"G1_{c}", D) for c in range(2)]
        A2 = [A.alloc(f"A2_{c}", D) for c in range(2)]
        B2 = [A.alloc(f"B2_{c}", D) for c in range(2)]
        ng, kng = A.alloc("ng", D)
        g.load_bcast(self.norm_g[li, 1], ng, kng)
        for c in range(2):
            g.load_mod(li, 2, c, *G1[c])
            g.load_mod(li, 4, c, *A2[c])
            g.load_mod(li, 3, c, *B2[c])
            g.stt(A2[c][0], A2[c][0], 1.0, ng, ALU.add, ALU.mult, R=[A2[c][1], kng], W=[A2[c][1]])
        xt = [A.alloc(f"xt{i}", D) for i in range(2)]
        xn = [A.alloc(f"xn{i}", D) for i in range(2)]
        tmp, ktmp = A.alloc("tmp", D)
        ff = [A.alloc(f"ff{i}", D) for i in range(2)]
        fb = [A.alloc(f"fb{i}", D, BF16) for i in range(2)]
        fT, kfT = A.alloc("fT32", D)
        wr, kwr = A.alloc("wr", 128)
        st_, kst = A.alloc("stat", 8)
        ex, kex = A.alloc("ex", 16)
        aft, kaft = A.alloc("AFFT", T, parts=16)
        g.ld(wr.rearrange("p (k e) -> p k e", e=16), self.moe_router_w[li].rearrange("(k p) e -> p k e", p=128), R=[], W=[kwr])
        wr3 = wr.rearrange("p (k e) -> p k e", e=16)
        psd, pst, psl = self.ps[0], self.ps[1], self.ps[2]
        def part1(tt):
            c = 1 if tt < 2 else 0
            b = tt % 2
            x, kx = xt[b]
            g.ld(x, self.X[tt * 128:(tt + 1) * 128, :], R=[f"X{tt}"], W=[kx])
            d, kd = d_fn(tt)
            g.tt(tmp, d, G1[c][0], ALU.mult, R=[kd, G1[c][1]], W=[ktmp])
            xo, kxo = xn[b]
            g.tt(xo, x, tmp, ALU.add, R=[kx, ktmp], W=[kxo])
        part1(0)
        for tt in range(NT):
            c = 1 if tt < 2 else 0
            b = tt % 2
            xo, kxo = xn[b]
            if tt + 1 < NT:
                part1(tt + 1)
            g.st(self.X[tt * 128:(tt + 1) * 128, :], xo, R=[kxo], W=[f"X{tt}"])
            ssq = st_[:, 0:1]
            g.act(fT, xo, AF.Square, R=[kxo], W=[kfT, kst], accum_out=ssq)
            g.rsqrt_small(ssq, ssq, 1.0 / D, None, R=[kst], W=[kst])
            f, kf = ff[b]
            g.stt(f, xo, ssq, A2[c][0], ALU.mult, ALU.mult, R=[kxo, kst, A2[c][1]], W=[kf])
            g.tt(f, f, B2[c][0], ALU.add, R=[kf, B2[c][1]], W=[kf])
            fbt, kfb = fb[b]
            g.cp(fbt, f, R=[kf], W=[kfb], eng="act")
            g.st(self.Fb[tt * 128:(tt + 1) * 128, :], fbt, R=[kfb], W=[f"Fb{tt}"])
            for k in range(8):
                g.tr(pst[:, k * 128:(k + 1) * 128], f[:, k * 128:(k + 1) * 128], self.ident_f[:], R=[kf, "ident_f"], W=["ps1"])
            g.cp(fT, pst[:, :], R=["ps1"], W=[kfT], eng="act")
            for k in range(8):
                g.mm(psl[:, 0:16], fT[:, k * 128:(k + 1) * 128], wr3[:, k, :], k == 0, k == 7, R=[kfT, kwr], W=["ps2"])
            mx = st_[:, 1:2]
            sm = st_[:, 2:3]
            g.red(mx, psl[:, 0:16], ALU.max, R=["ps2"], W=[kst])
            g.ts(mx, mx, -1.0, None, ALU.mult, None, R=[kst], W=[kst])
            g.act(ex, psl[:, 0:16], AF.Exp, R=["ps2", kst], W=[kex, kst], bias=mx, accum_out=sm)
            g.recip(sm, sm, R=[kst], W=[kst])
            g.ts(ex, ex, sm, None, ALU.mult, None, R=[kex, kst], W=[kex])
            g.tr(psl[0:16, 512:640], ex, self.ident_f[:], R=[kex, "ident_f"], W=["ps2"])
            g.cp(aft[:, tt * 128:(tt + 1) * 128], psl[0:16, 512:640], R=["ps2"], W=[kaft], eng="act")
        g.st(self.AFF, aft, R=[kaft], W=["AFF"])

    def moe_topk(self):
        g = self
        A = self.arena
        A.reset()
        af, kaf = A.alloc("af", T, parts=16)
        wk = [A.alloc(f"wk{i}", NLAT, parts=16) for i in range(2)]
        mxv, kmx = A.alloc("mxv", 544, parts=16)
        ixv, kix = A.alloc("ixv", 544, U32, parts=16)
        idf, kidf = A.alloc("idf", 544, parts=16)
        tf, ktf = A.alloc("tf", 80)
        g.ld(af, self.AFF, R=["AFF"], W=[kaf])

        def rounds(src0, ksrc0, n, col0, nr):
            src, ksrc = src0, ksrc0
            for r in range(nr):
                c0 = col0 + 8 * r
                g.kb.op("dve", lambda e, c0=c0, src=src: e.max(out=mxv[:, c0:c0 + 8], in_=src), R=[ksrc], W=[kmx])
                g.kb.op("dve", lambda e, c0=c0, src=src: e.max_index(out=ixv[:, c0:c0 + 8], in_max=mxv[:, c0:c0 + 8], in_values=src),
                        R=[ksrc, kmx], W=[kix])
                if r < nr - 1:
                    dst, kdst = wk[r % 2]
                    dstv = dst[:, 0:n]
                    g.kb.op("dve", lambda e, c0=c0, src=src, dstv=dstv: e.match_replace(out=dstv, in_to_replace=mxv[:, c0:c0 + 8],
                                                                                         in_values=src, imm_value=-1.0),
                            R=[ksrc, kmx], W=[kdst])
                    src, ksrc = dstv, kdst
        rounds(af[:, NCTX:T], kaf, NLAT, 0, 64)
        rounds(af[:, 0:NCTX], kaf, NCTX, 512, 4)
        g.cp(idf, ixv, R=[kix], W=[kidf])
        g.ts(idf[:, 0:512], idf[:, 0:512], float(NCTX), None, ALU.add, None, R=[kidf], W=[kidf])
        psl = self.ps[2]
        for sc in range(5):
            n = 128 if sc < 4 else 32
            g.tr(psl[0:n, 0:16], idf[:, sc * 128:sc * 128 + n], self.ident_f[0:16, 0:16], R=[kidf, "ident_f"], W=["ps2"])
            g.cp(self.IDXI[0:n, sc * 16:(sc + 1) * 16], psl[0:n, 0:16], R=["ps2"], W=["IDXI"])
            g.tr(psl[0:n, 16:32], mxv[:, sc * 128:sc * 128 + n], self.ident_f[0:16, 0:16], R=[kmx, "ident_f"], W=["ps2"])
            g.cp(self.GVT[0:n, sc * 16:(sc + 1) * 16], psl[0:n, 16:32], R=["ps2"], W=["GVT"])

    def moe_experts(self, li):
        g = self
        A = self.arena
        A.reset()
        W1b, _ = A.alloc("W1b", 8 * 2048, BF16)
        W3b, _ = A.alloc("W3b", 8 * 2048, BF16)
        W2b, _ = A.alloc("W2b", 16 * 1024, BF16)
        W1v = W1b.rearrange("p (k f) -> p k f", f=2048)
        W3v = W3b.rearrange("p (k f) -> p k f", f=2048)
        W2v = W2b.rearrange("p (k f) -> p k f", f=1024)
        XSt, kXS = A.alloc("XS", 5 * 1024, BF16)
        XS = XSt.rearrange("p (s d) -> p s d", d=1024)
        HIDt, kHID = A.alloc("HID", 16 * 544, BF16)
        HID = HIDt.rearrange("p (f s) -> p f s", s=544)
        XST, kXST = A.alloc("XST", 8 * 544, BF16)
        XSTv = XST.rearrange("p (k s) -> p k s", s=544)
        SIL = [A.alloc(f"sil{i}", 544) for i in range(2)]
        YSb = [A.alloc(f"YS{i}", 1024) for i in range(2)]
        G2 = [A.alloc(f"G2_{c}", D) for c in range(2)]
        for c in range(2):
            g.load_mod(li, 5, c, *G2[c])

        def load13(e):
            for (wsrc, wv, nm) in ((self.moe_w1, W1v, "W1"), (self.moe_w3, W3v, "W3")):
                for k in range(8):
                    g.ld(wv[:, k, :], wsrc[li, e, k * 128:(k + 1) * 128, :], R=[], W=[f"{nm}.{k}"], q="pool")

        def load2(e):
            for fc in range(0, 16, 2):
                g.ld(W2v[:, fc:fc + 2, :], self.moe_w2[li, e, fc * 128:(fc + 2) * 128, :].rearrange("(a p) n -> p a n", p=128),
                     R=[], W=[f"W2.{fc}", f"W2.{fc + 1}"], q="pool")

        def gather(e):
            for sc in range(5):
                n = 128 if sc < 4 else 32
                col = sc * 16 + e
                g.kb.dma("pool", lambda en, n=n, sc=sc, col=col: en.indirect_dma_start(
                    out=XS[0:n, sc, :], out_offset=None, in_=self.Fb,
                    in_offset=bass.IndirectOffsetOnAxis(ap=self.IDXI[0:n, col:col + 1], axis=0)),
                    R=["IDXI", "Fball"], W=[kXS])

        def transposes(e):
            pT = self.ps[3].bitcast(BF16)
            for sc in range(5):
                n = 128 if sc < 4 else 32
                for k in range(8):
                    g.tr(pT[:, k * 128:k * 128 + n], XS[0:n, sc, k * 128:(k + 1) * 128], self.ident_b[0:n, 0:n],
                         R=[kXS, "ident_b"], W=["ps3"])
                g.cp(XSTv[:, :, sc * 128:sc * 128 + n], pT[:, 0:1024].rearrange("p (k s) -> p k s", s=128)[:, :, 0:n],
                     R=["ps3"], W=[kXST], eng="act" if sc % 2 == 0 else "dve")

        def hidden(e):
            for fc in range(16):
                p1, k1 = (self.ps[0], "ps0") if fc % 2 == 0 else (self.ps[2], "ps2")
                p3, k3 = (self.ps[1], "ps1") if fc % 2 == 0 else (self.ps[3], "ps3")
                for (wv, nm, pp, kp) in ((W1v, "W1", p1, k1), (W3v, "W3", p3, k3)):
                    for k in range(8):
                        for (n0, n1) in ((0, 512), (512, 544)):
                            g.mm(pp[:, n0:n1], wv[:, k, fc * 128:(fc + 1) * 128], XSTv[:, k, n0:n1], k == 0, k == 7,
                                 R=[f"{nm}.{k}", kXST], W=[kp])
                sil, ksil = SIL[fc % 2]
                g.act(sil, p1[:, 0:544], AF.Silu, R=[k1], W=[ksil])
                g.tt(HID[:, fc, :], sil, p3[:, 0:544], ALU.mult, R=[ksil, k3], W=[kHID])

        ysi = [0]

        def outscatter(e):
            for sc in range(5):
                n = 128 if sc < 4 else 32
                c = 0 if sc < 4 else 1
                col = sc * 16 + e
                py, ky = (self.ps[0], "ps0") if sc % 2 == 0 else (self.ps[1], "ps1")
                for hf in range(2):
                    for fc in range(16):
                        g.mm(py[0:n, hf * 512:(hf + 1) * 512], HID[:, fc, sc * 128:sc * 128 + n], W2v[:, fc, hf * 512:(hf + 1) * 512],
                             fc == 0, fc == 15, R=[kHID, f"W2.{fc}"], W=[ky])
                YS, kYS = YSb[ysi[0] % 2]
                ysi[0] += 1
                g.stt(YS[0:n, :], py[0:n, :], self.GVT[0:n, col:col + 1], G2[c][0][0:n, :], ALU.mult, ALU.mult,
                      R=[ky, "GVT", G2[c][1]], W=[kYS])
                g.kb.dma("pool", lambda en, n=n, col=col, YS=YS: en.indirect_dma_start(
                    out=self.X, out_offset=bass.IndirectOffsetOnAxis(ap=self.IDXI[0:n, col:col + 1], axis=0),
                    in_=YS[0:n, :], in_offset=None, compute_op=ALU.add),
                    R=["IDXI", kYS, "Xsc"], W=["Xsc"])

        gather(0)
        load13(0)
        load2(0)
        transposes(0)
        for e in range(16):
            if e + 1 < 16:
                gather(e + 1)
            hidden(e)
            if e + 1 < 16:
                load13(e + 1)
                transposes(e + 1)
            outscatter(e)
            if e + 1 < 16:
                load2(e + 1)

    def final_norm(self):
        g = self
        A = self.arena
        A.reset()
        fg, kfg = A.alloc("fg", D)
        g.load_bcast(self.final_norm_g, fg, kfg)
        xt = [A.alloc(f"xt{i}", D) for i in range(2)]
        ot = [A.alloc(f"ot{i}", D) for i in range(2)]
        junk, kj = A.alloc("junk", D)
        st_, kst = A.alloc("stat", 8)
        for tt in range(2, NT):
            b = tt % 2
            x, kx = xt[b]
            o, ko = ot[b]
            g.ld(x, self.X[tt * 128:(tt + 1) * 128, :], R=[f"X{tt}"], W=[kx])
            g.norm_tile(x, kx, st_[:, 0:1], kst, junk, kj)
            g.stt(o, x, st_[:, 0:1], fg, ALU.mult, ALU.mult, R=[kx, kst, kfg], W=[ko])
            g.st(self.out[(tt - 2) * 128:(tt - 1) * 128, :], o, R=[ko], W=[f"out{tt}"])

    def build(self):
        g = self
        cfg = self.cfg
        self.phase0()
        if cfg.get("mixer", True) and any(l % 2 == 0 for l in self.layers):
            self.setup_rope()
        for li, l in enumerate(self.layers):
            if cfg.get("mixer", True):
                if l % 2 == 0:
                    self.even_mixer(li, l)
                else:
                    self.s5_mixer(li, l)
            else:
                A = self.arena
                A.reset()
                z, kz = A.alloc("zero", D)
                g.memset(z, 0.0, W=[kz])
                self.post_stage(li, l, lambda tt: (z, kz))
            if cfg.get("moe", True):
                self.moe_topk()
                self.moe_experts(li)
        self.final_norm()
        self.kb.emit()
        return self.nc


def bc(ap, axis, n):
    a = ap.unsqueeze(axis)
    shp = list(a.shape)
    shp[axis] = n
    return a.broadcast_to(shp)


def setup_rope(self):
    g = self
    kb = self.kb
    self.cosT = kb.sbuf("cosT", [128, 1024], F32)
    self.sinT = kb.sbuf("sinT", [128, 1024], F32)
    A = self.arena
    A.reset()
    fi, kfi = A.alloc("fi", 16)
    inv, kinv = A.alloc("inv", 16)
    pidx, kp = A.alloc("pidx", 1)
    ph, kph = A.alloc("ph", 1)
    colv, kcol = A.alloc("colv", 1)
    rowv, krow = A.alloc("rowv", 32)
    ang, kang = A.alloc("ang", 1024)
    ri, kri = A.alloc("ri", 1024, I32)
    ab, kab = A.alloc("ab", 1024)
    kb.op("pool", lambda e: e.iota(fi, pattern=[[1, 16]], base=0, channel_multiplier=0, allow_small_or_imprecise_dtypes=True), W=[kfi])
    g.act(inv, fi, AF.Exp, R=[kfi], W=[kinv], scale=-(2.0 / 32.0) * math.log(10000.0))
    kb.op("pool", lambda e: e.iota(pidx, pattern=[[0, 1]], base=0, channel_multiplier=1, allow_small_or_imprecise_dtypes=True), W=[kp])
    g.ts(ph, pidx, 64.0, None, ALU.is_ge, None, R=[kp], W=[kph])
    g.stt(colv, ph, -64.0, pidx, ALU.mult, ALU.add, R=[kph, kp], W=[kcol])
    kb.op("pool", lambda e: e.iota(rowv, pattern=[[2, 32]], base=0, channel_multiplier=0, allow_small_or_imprecise_dtypes=True), W=[krow])
    g.ts(rowv, rowv, ph, None, ALU.add, None, R=[krow, kph], W=[krow])
    a4 = ang.rearrange("p (t a i) -> p t a i", a=2, i=16)
    g.tt(a4[:, :, 0, :], bc(rowv, 2, 16), bc(inv, 1, 32), ALU.mult, R=[krow, kinv], W=[kang])
    g.ts(a4[:, :, 1, :], bc(inv, 1, 32), colv, None, ALU.mult, None, R=[kinv, kcol, kang], W=[kang])
    g.ts(ang, ang, 1.0 / TWO_PI, None, ALU.mult, None, R=[kang], W=[kang])
    g.cp(ri, ang, R=[kang], W=[kri])
    g.tt(ang, ang, ri, ALU.subtract, R=[kang, kri], W=[kang])
    g.act(self.sinT[:], ang, AF.Sin, R=[kang], W=["sinT"], scale=TWO_PI)
    g.act(ab, ang, AF.Abs, R=[kang], W=[kab])
    hp, khp = A.alloc("halfpi", 1)
    g.memset(hp, math.pi / 2.0, W=[khp])
    g.act(self.cosT[:], ab, AF.Sin, R=[kab, khp], W=["cosT"], scale=-TWO_PI, bias=hp)


def rope(self, src, ksrc, dst, kdst, H, tt, tmps):
    g = self
    t = tt - 2
    sv = src.rearrange("p (h a s i) -> p h a s i", a=2, s=2, i=16)
    dv = dst.rearrange("p (h a s i) -> p h a s i", a=2, s=2, i=16)
    x1, x2 = sv[:, :, :, 0, :], sv[:, :, :, 1, :]
    cos = bc(self.cosT[:, t * 32:(t + 1) * 32].rearrange("p (a i) -> p a i", i=16), 1, H)
    sin = bc(self.sinT[:, t * 32:(t + 1) * 32].rearrange("p (a i) -> p a i", i=16), 1, H)
    (t1, k1), (t2, k2), (t3, k3), (t4, k4) = tmps
    v = lambda a: a[:, 0:H * 32].rearrange("p (h a i) -> p h a i", a=2, i=16)
    g.tt(v(t1), x1, cos, ALU.mult, R=[ksrc, "cosT"], W=[k1])
    g.tt(v(t2), x2, sin, ALU.mult, R=[ksrc, "sinT"], W=[k2])
    g.tt(dv[:, :, :, 0, :], v(t1), v(t2), ALU.subtract, R=[k1, k2], W=[kdst])
    g.tt(v(t3), x1, sin, ALU.mult, R=[ksrc, "sinT"], W=[k3], eng="pool")
    g.tt(v(t4), x2, cos, ALU.mult, R=[ksrc, "cosT"], W=[k4], eng="pool")
    g.tt(dv[:, :, :, 1, :], v(t3), v(t4), ALU.add, R=[k3, k4], W=[kdst], eng="pool")


def even_mixer(self, li, l):
    g = self
    kb = self.kb
    A = self.arena
    ei = self.ev_idx[l]
    if not hasattr(self, "QT"):
        self.QT = g.dram_tmp("QT", [1664, T], BF16)
        self.TMd = g.dram_tmp("TMd", [T, 2176], BF16)
        self.OF = g.dram_tmp("OF", [T, 512])
        self.MIXT = g.dram_tmp("MIXT", [1024, T], BF16)
    QTv = self.QT.rearrange("(c p) t -> p c t", p=128)
    MIXTv = self.MIXT.rearrange("(c p) t -> p c t", p=128)
    A.reset()
    Wb, _ = A.alloc("Wb", 8 * 2816, BF16)
    Wv = Wb.rearrange("p (k n) -> p k n", n=2816)
    stg = [A.alloc(f"stg{i}", 1408) for i in range(2)]
    ce = ["pool", "act", "dve"]
    for k in range(8):
        g.ld(Wv[:, k, :], self.mix_in_w[ei, k * 128:(k + 1) * 128, :], R=[], W=["Wb"], q="pool")
    A1 = [A.alloc(f"A1_{c}", D) for c in range(2)]
    B1 = [A.alloc(f"B1_{c}", D) for c in range(2)]
    ng, kng = A.alloc("ng", D)
    g.load_bcast(self.norm_g[li, 0], ng, kng)
    for c in range(2):
        g.load_mod(li, 1, c, *A1[c])
        g.load_mod(li, 0, c, *B1[c])
        g.stt(A1[c][0], A1[c][0], 1.0, ng, ALU.add, ALU.mult, R=[A1[c][1], kng], W=[A1[c][1]])
    gq, kgq = A.alloc("gq", 64)
    gk, kgk = A.alloc("gk", 64)
    g.load_bcast(self.qk_norm_g[ei, 0:64], gq, kgq)
    g.load_bcast(self.qk_norm_g[ei, 64:128], gk, kgk)
    lgt, klg = A.alloc("lgt", 16)
    g.load_bcast(self.ret_log_rate[ei], lgt, klg)
    g.act(lgt, lgt, AF.Exp, R=[klg], W=[klg])
    g.ts(lgt, lgt, -1.0, None, ALU.mult, None, R=[klg], W=[klg])
    pidx, kp = A.alloc("pidx", 1)
    pr_, kpr = A.alloc("prev", 1)
    kb.op("pool", lambda e: e.iota(pidx, pattern=[[0, 1]], base=0, channel_multiplier=1, allow_small_or_imprecise_dtypes=True), W=[kp])
    g.ts(pr_, pidx, -1.0, 127.0, ALU.mult, ALU.add, R=[kp], W=[kpr])
    DK, kDK = A.alloc("DK", 16)
    g.ts(DK[:, 0:8], lgt[:, 0:8], pr_, None, ALU.mult, None, R=[klg, kpr], W=[kDK])
    g.ts(DK[:, 8:16], lgt[:, 8:16], pidx, None, ALU.mult, None, R=[klg, kp, kDK], W=[kDK])
    g.act(DK, DK, AF.Exp, R=[kDK], W=[kDK])
    xt = [A.alloc(f"xt{i}", D) for i in range(2)]
    tmp, ktmp = A.alloc("tmp", D)
    hb, khb = A.alloc("hb", D, BF16)
    hT, khT = A.alloc("hT", D, BF16)
    P, kP = A.alloc("P", 2816)
    sqt, ksq = A.alloc("sqt", 640)
    st_, kst = A.alloc("stat", 16)
    QKb = [A.alloc(f"QKb{i}", 1664, BF16) for i in range(2)]
    TM = [A.alloc(f"TM{i}", 2176, BF16) for i in range(2)]
    QTs = [A.alloc(f"QTs{i}", 1664, BF16) for i in range(2)]
    tmps = [A.alloc(f"rt{i}", 512) for i in range(4)]
    psT = self.ps[3].bitcast(BF16)
    def e1_a(tt):
        c = 1 if tt < 2 else 0
        b = tt % 2
        x, kx = xt[b]
        g.ld(x, self.X[tt * 128:(tt + 1) * 128, :], R=[f"X{tt}"], W=[kx])
        ssq = st_[:, 0:1]
        g.act(tmp, x, AF.Square, R=[kx], W=[ktmp, kst], accum_out=ssq)
        g.rsqrt_small(ssq, ssq, 1.0 / D, None, R=[kst], W=[kst])
        g.stt(tmp, x, ssq, A1[c][0], ALU.mult, ALU.mult, R=[kx, kst, A1[c][1]], W=[ktmp])
        g.tt(hb, tmp, B1[c][0], ALU.add, R=[ktmp, B1[c][1]], W=[khb])
        for k in range(8):
            g.tr(psT[:, k * 128:(k + 1) * 128], hb[:, k * 128:(k + 1) * 128], self.ident_b[:], R=[khb, "ident_b"], W=["ps3"])
        g.cp(hT, psT[:, 0:1024], R=["ps3"], W=[khT], eng="act")
        for (n0, w, pp, kp_, off) in ((0, 512, 0, "ps0", 0), (512, 512, 0, "ps0", 512), (1024, 512, 1, "ps1", 0),
                                      (1536, 512, 1, "ps1", 512), (2048, 512, 2, "ps2", 0), (2560, 256, 2, "ps2", 512)):
            for k in range(8):
                g.mm(self.ps[pp][:, off:off + w], hT[:, k * 128:(k + 1) * 128], Wv[:, k, n0:n0 + w], k == 0, k == 7,
                     R=[khT, "Wb"], W=[kp_])
        g.cp(P[:, 0:1024], self.ps[0][:, :], R=["ps0"], W=[kP], eng="act")
        g.cp(P[:, 1024:2048], self.ps[1][:, :], R=["ps1"], W=[kP], eng="dve")
        g.cp(P[:, 2048:2816], self.ps[2][:, 0:768], R=["ps2"], W=[kP], eng="act")
    def e1_b(tt):
        c = 1 if tt < 2 else 0
        b = tt % 2
        qa = P[:, 2048:2688]
        qa3 = qa.rearrange("p (h d) -> p h d", d=64)
        g.act(sqt, qa, AF.Square, R=[kP], W=[ksq])
        ss10 = st_[:, 4:14]
        g.red(ss10, sqt.rearrange("p (h d) -> p h d", d=64), ALU.add, R=[ksq], W=[kst])
        g.rsqrt_small(ss10, ss10, 1.0 / 64.0, None, R=[kst], W=[kst])
        g.tt(qa3, qa3, bc(ss10, 2, 64), ALU.mult, R=[kP, kst], W=[kP])
        g.tt(qa3[:, 0:8, :], qa3[:, 0:8, :], bc(gq, 1, 8), ALU.mult, R=[kP, kgq], W=[kP])
        g.tt(qa3[:, 8:10, :], qa3[:, 8:10, :], bc(gk, 1, 2), ALU.mult, R=[kP, kgk], W=[kP])
        g.ts(P[:, 512:1024], P[:, 512:1024], 0.125, None, ALU.mult, None, R=[kP], W=[kP], eng="pool")
        qk, kqk = QKb[b]
        if c == 0:
            rope(self, P[:, 0:1024], kP, qk[:, 0:1024], kqk, 16, tt, tmps)
            rope(self, P[:, 2048:2688], kP, qk[:, 1024:1664], kqk, 10, tt, tmps)
        else:
            g.cp(qk[:, 0:1024], P[:, 0:1024], R=[kP], W=[kqk], eng="dve")
            g.cp(qk[:, 1024:1664], P[:, 2048:2688], R=[kP], W=[kqk], eng="pool")
        tm, ktm = TM[b]
        rk3 = qk[:, 512:1024].rearrange("p (h d) -> p h d", d=64)
        g.tt(tm[:, 0:512].rearrange("p (h d) -> p h d", d=64), rk3, bc(DK[:, 0:8], 2, 64), ALU.mult, R=[kqk, kDK], W=[ktm])
        g.tt(tm[:, 512:1024].rearrange("p (h d) -> p h d", d=64), rk3, bc(DK[:, 8:16], 2, 64), ALU.mult, R=[kqk, kDK], W=[ktm], eng="pool")
        g.cp(tm[:, 1024:1536], P[:, 1024:1536], R=[kP], W=[ktm], eng="pool")
        g.act(tm[:, 1536:2048], P[:, 1536:2048], AF.Silu, R=[kP], W=[ktm])
        g.cp(tm[:, 2048:2176], P[:, 2688:2816], R=[kP], W=[ktm], eng="act")
        g.st(self.TMd[tt * 128:(tt + 1) * 128, :], tm, R=[ktm], W=[f"TMd{tt}"])
    def e1_c(tt):
        c = 1 if tt < 2 else 0
        b = tt % 2
        qk, kqk = QKb[b]
        for ch in range(13):
            g.tr(psT[:, ch * 128:(ch + 1) * 128], qk[:, ch * 128:(ch + 1) * 128], self.ident_b[:], R=[kqk, "ident_b"], W=["ps3"])
        qs, kqs = QTs[b]
        g.cp(qs, psT[:, 0:1664], R=["ps3"], W=[kqs], eng="act")
        g.st(QTv[:, :, tt * 128:(tt + 1) * 128], qs.rearrange("p (c t) -> p c t", t=128), R=[kqs], W=[f"QT{tt}"])

    e1_a(0)
    for tt in range(NT):
        e1_b(tt)
        if tt + 1 < NT:
            e1_a(tt + 1)
        e1_c(tt)
    if self.cfg.get('even_stop') == 1:
        return
    A.reset()
    lgt, klg = A.alloc("lgt", 16)
    g.load_bcast(self.ret_log_rate[ei], lgt, klg)
    g.act(lgt, lgt, AF.Exp, R=[klg], W=[klg])
    g.ts(lgt, lgt, -1.0, None, ALU.mult, None, R=[klg], W=[klg])
    diff, kdf = A.alloc("diff", 128)
    ndiff, kndf = A.alloc("ndiff", 128)
    mk, kmk = A.alloc("mk", 128)
    i1, ki1 = A.alloc("i1", 128)
    i2, ki2 = A.alloc("i2", 128)
    kb.op("pool", lambda e: e.iota(diff, pattern=[[1, 128]], base=0, channel_multiplier=-1, allow_small_or_imprecise_dtypes=True), W=[kdf])
    kb.op("pool", lambda e: e.iota(ndiff, pattern=[[-1, 128]], base=0, channel_multiplier=1, allow_small_or_imprecise_dtypes=True), W=[kndf])
    kb.op("pool", lambda e: e.iota(i1, pattern=[[1, 128]], base=1, channel_multiplier=0, allow_small_or_imprecise_dtypes=True), W=[ki1])
    kb.op("pool", lambda e: e.iota(i2, pattern=[[-1, 128]], base=128, channel_multiplier=0, allow_small_or_imprecise_dtypes=True), W=[ki2])
    DT = [A.alloc(f"DT{d}", 1024) for d in range(2)]
    DQ = [A.alloc(f"DQ{d}", 512) for d in range(2)]
    dc = [A.alloc(f"dc{d}", 4) for d in range(2)]
    lgh = [A.alloc(f"lgh{d}", 4) for d in range(2)]
    for d in range(2):
        src_d, ksd = (diff, kdf) if d == 0 else (ndiff, kndf)
        g.ts(mk, diff, 0.0, None, ALU.is_ge if d == 0 else ALU.is_lt, None, R=[kdf], W=[kmk])
        dt_, kdt = DT[d]
        for h in range(8):
            sl = (h % 2) * 4 + h // 2
            g.act(dt_[:, sl * 128:(sl + 1) * 128], src_d, AF.Exp, R=[ksd, klg], W=[kdt], scale=lgt[:, d * 8 + h:d * 8 + h + 1])
        g.tt(dt_.rearrange("p (h i) -> p h i", i=128), dt_.rearrange("p (h i) -> p h i", i=128), bc(mk, 1, 8), ALU.mult,
             R=[kdt, kmk], W=[kdt])
        lh, klh = lgh[d]
        lsel = lgt[:, d * 8:(d + 1) * 8].rearrange("p (q two) -> p q two", two=2)
        g.cp(lh[0:64, :], lsel[0:64, :, 0], R=[klg], W=[klh])
        g.cp(lh[64:128, :], lsel[64:128, :, 1], R=[klg], W=[klh])
        dq, kdq = DQ[d]
        isrc, kis = (i1, ki1) if d == 0 else (i2, ki2)
        for q in range(4):
            g.act(dq[:, q * 128:(q + 1) * 128], isrc, AF.Exp, R=[kis, klh], W=[kdq], scale=lh[:, q:q + 1])
        g.act(dc[d][0], lh, AF.Exp, R=[klh], W=[dc[d][1]], scale=128.0)
    gng, kgng = A.alloc("gng", 512)
    g.load_bcast(self.ret_gn_g[ei], gng, kgng)
    QKT = [A.alloc(f"QKT{i}", 1024, BF16) for i in range(2)]
    TMt = [A.alloc(f"TMt{i}", 2176, BF16) for i in range(2)]
    PT, kPT = A.alloc("PT", 1024, BF16)
    qtl, kqtl = A.alloc("qtl", 512, BF16)
    S, kS = A.alloc("S", 256)
    Sb, kSb = A.alloc("Sb", 256, BF16)
    o32 = [A.alloc(f"o32_{i}", 512) for i in range(2)]
    oft = [A.alloc(f"of{i}", 512) for i in range(2)]
    sq2, ksq2 = A.alloc("sq2", 512)
    gs, kgs = A.alloc("gs", 32)
    mr = [A.alloc(f"mr{i}", 512, BF16) for i in range(2)]
    mrT = [A.alloc(f"mrT{i}", 512, BF16) for i in range(2)]
    psS, psO = self.ps[0], self.ps[1]
    S3 = S.rearrange("p (q e) -> p q e", e=64)
    if self.cfg.get('even_stop') == 21:
        return
    for d in range(2):
        if d == 1 and self.cfg.get('even_stop') == 22:
            return
        order = list(range(NT)) if d == 0 else [1, 0] + list(range(NT - 1, 1, -1))
        g.memset(S, 0.0, W=[kS])
        g.memset(Sb, 0.0, W=[kSb])
        koff = 0 if d == 0 else 512
        for n_i, tt in enumerate(order):
            b = n_i % 2
            qkt, kq = QKT[b]
            tm, ktm = TMt[b]
            qk3 = qkt.rearrange("p (c t) -> p c t", t=128)
            g.ld(qk3, QTv[:, 0:8, tt * 128:(tt + 1) * 128], R=[f"QT{tt}"], W=[kq])
            g.ld(tm, self.TMd[tt * 128:(tt + 1) * 128, :], R=[f"TMd{tt}"], W=[ktm])
            g.tt(qtl, qkt[:, 0:512], DQ[d][0], ALU.mult, R=[kq, DQ[d][1]], W=[kqtl])
            for h in range(8):
                par, pr = h % 2, h // 2
                sl = par * 4 + pr
                g.mm(psS[:, sl * 128:(sl + 1) * 128], qk3[par * 64:(par + 1) * 64, 4 + pr, :], qk3[par * 64:(par + 1) * 64, pr, :],
                     True, True, R=[kq], W=["ps0"])
            g.tt(PT, psS[:, :], DT[d][0], ALU.mult, R=["ps0", DT[d][1]], W=[kPT])
            for h in range(8):
                par, pr = h % 2, h // 2
                sl = par * 4 + pr
                g.mm(psO[:, h * 64:(h + 1) * 64], PT[:, sl * 128:(sl + 1) * 128], tm[:, 1024 + h * 64:1024 + (h + 1) * 64],
                     True, bool(self.cfg.get('no_acc')), R=[kPT, ktm], W=["ps1"])
                if self.cfg.get('no_acc'):
                    continue
                g.mm(psO[:, h * 64:(h + 1) * 64], qtl[par * 64:(par + 1) * 64, pr * 128:(pr + 1) * 128],
                     Sb[par * 64:(par + 1) * 64, pr * 64:(pr + 1) * 64], False, True, R=[kqtl, kSb], W=["ps1"])
            for pr in range(4):
                g.mm(psO[:, 512 + pr * 128:512 + (pr + 1) * 128], tm[:, koff + pr * 128:koff + (pr + 1) * 128],
                     tm[:, 1024 + pr * 128:1024 + (pr + 1) * 128], True, True, R=[ktm], W=["ps1u"])
            g.tt(S3, S3, bc(dc[d][0], 2, 64), ALU.mult, R=[kS, dc[d][1]], W=[kS])
            U3 = psO[:, 512:1024].rearrange("p (q e) -> p q e", e=128)
            g.tt(S3[0:64], S3[0:64], U3[0:64, :, 0:64], ALU.add, R=[kS, "ps1u"], W=[kS])
            g.tt(S3[64:128], S3[64:128], U3[64:128, :, 64:128], ALU.add, R=[kS, "ps1u"], W=[kS])
            g.cp(Sb, S, R=[kS], W=[kSb], eng="act")
            o, ko = o32[b]
            if d == 0:
                g.cp(o, psO[:, 0:512], R=["ps1"], W=[ko], eng="act")
                g.st(self.OF[tt * 128:(tt + 1) * 128, :], o, R=[ko], W=[f"OF{tt}"])
            else:
                of, kof = oft[b]
                g.ld(of, self.OF[tt * 128:(tt + 1) * 128, :], R=[f"OF{tt}"], W=[kof])
                g.tt(o, psO[:, 0:512], of, ALU.add, R=["ps1", kof], W=[ko])
                o3 = o.rearrange("p (h e) -> p h e", e=64)
                s1, s2, mean, msq, var = gs[:, 0:8], gs[:, 8:16], gs[:, 16:24], gs[:, 24:32], gs[:, 8:16]
                g.red(s1, o3, ALU.add, R=[ko], W=[kgs])
                g.act(sq2, o, AF.Square, R=[ko], W=[ksq2])
                g.red(s2, sq2.rearrange("p (h e) -> p h e", e=64), ALU.add, R=[ksq2], W=[kgs])
                g.ts(mean, s1, 1.0 / 64.0, None, ALU.mult, None, R=[kgs], W=[kgs])
                g.tt(msq, mean, mean, ALU.mult, R=[kgs], W=[kgs])
                g.stt(var, s2, 1.0 / 64.0, msq, ALU.mult, ALU.subtract, R=[kgs], W=[kgs])
                g.rsqrt_small(var, var, 1.0, None, R=[kgs], W=[kgs])
                g.tt(o3, o3, bc(mean, 2, 64), ALU.subtract, R=[ko, kgs], W=[ko])
                g.tt(o3, o3, bc(var, 2, 64), ALU.mult, R=[ko, kgs], W=[ko])
                g.tt(o, o, gng, ALU.mult, R=[ko, kgng], W=[ko], eng="pool")
                m_, km = mr[b]
                g.tt(m_, o, tm[:, 1536:2048], ALU.mult, R=[ko, ktm], W=[km], eng="pool")
                pT2 = self.ps[3].bitcast(BF16)
                for ch in range(4):
                    g.tr(pT2[:, ch * 128:(ch + 1) * 128], m_[:, ch * 128:(ch + 1) * 128], self.ident_b[:], R=[km, "ident_b"], W=["ps3"])
                mt, kmt = mrT[b]
                g.cp(mt, pT2[:, 0:512], R=["ps3"], W=[kmt], eng="act")
                g.st(MIXTv[:, 0:4, tt * 128:(tt + 1) * 128], mt.rearrange("p (c t) -> p c t", t=128), R=[kmt], W=[f"MIXT{tt}"])

    if self.cfg.get('even_stop') == 2:
        return
    A.reset()
    Kstd, kKs = A.alloc("Kstd", T, BF16)
    Kswp, kKw = A.alloc("Kswp", T, BF16)
    g.ld(Kstd, self.QT[1536:1664, :], R=["QTall"], W=[kKs])
    g.ld(Kswp[0:64, :], self.QT[1600:1664, :], R=["QTall"], W=[kKw])
    g.ld(Kswp[64:128, :], self.QT[1536:1600, :], R=["QTall"], W=[kKw])
    VA, kVA = A.alloc("VA", NT * 130, BF16)
    VA4 = VA.rearrange("p (t kv d) -> p t kv d", kv=2, d=65)
    g.memset(VA4[:, :, :, 64:65], 1.0, W=[kVA])
    for t in range(NT):
        g.ld(VA4[:, t, :, 0:64], self.TMd[t * 128:(t + 1) * 128, 2048:2176].rearrange("p (kv d) -> p kv d", kv=2), R=["TMdall"], W=[kVA])
    onesf, kon = A.alloc("onesf", 64)
    g.memset(onesf, 1.0, W=[kon])
    Qb = [A.alloc(f"Qb{i}", 2048, BF16) for i in range(2)]
    PTa = [A.alloc(f"PTa{i}", 512, BF16) for i in range(2)]
    rd, krd = A.alloc("rd", 512)
    rdb, krdb = A.alloc("rdb", 512)
    aT = [A.alloc(f"aT{i}", 512, BF16) for i in range(2)]
    blocks = [(0, 256, [0, 1])] + [(NCTX + qb * 512, 512, list(range(NT))) for qb in range(8)]
    PTa4 = PTa + [A.alloc(f"PTa{i}", 512, BF16) for i in range(2, 4)]
    cnt = [0]
    for bi, (q0, nq, ktiles) in enumerate(blocks):
        qb_, kqb = Qb[bi % 2]
        qb3 = qb_.rearrange("p (c t) -> p c t", t=512)
        g.ld(qb3[:, :, 0:nq], QTv[:, 8:12, q0:q0 + nq], R=["QTall"], W=[kqb])
        units = []
        for hp in range(4):
            for ki, kt in enumerate(ktiles):
                u = []
                for h in (2 * hp, 2 * hp + 1):
                    u.append(dict(h=h, ki=ki, kt=kt, last=(ki == len(ktiles) - 1), idx=cnt[0]))
                    cnt[0] += 1
                units.append(u)

        def sbuf_of(it):
            j = it["idx"] % 4
            t = self.ps[j % 2]
            off = (j // 2) * 512
            return t[:, off:off + 512], f"ps{j % 2}.{j // 2}"

        def emitS(it):
            h = it["h"]
            par, kv, pr = h % 2, h // 4, h // 2
            K_, kK = (Kstd, kKs) if par == kv else (Kswp, kKw)
            psSt, kps = sbuf_of(it)
            g.mm(psSt[:, 0:nq], K_[par * 64:(par + 1) * 64, it["kt"] * 128:(it["kt"] + 1) * 128], qb3[par * 64:(par + 1) * 64, pr, 0:nq],
                 True, True, R=[kK, kqb], W=[kps])

        def emitEP(it):
            h = it["h"]
            kv = h // 4
            psSt, kps = sbuf_of(it)
            psOt, kpo = (self.ps[2], "ps2") if h % 2 == 0 else (self.ps[3], "ps3")
            pa, kpa = PTa4[it["idx"] % 4]
            g.act(pa[:, 0:nq], psSt[:, 0:nq], AF.Exp, R=[kps], W=[kpa], scale=0.125)
            g.mm(psOt[0:65, 0:nq], VA4[:, it["kt"], kv, :], pa[:, 0:nq], it["ki"] == 0, it["last"], R=[kVA, kpa], W=[kpo])
            if it["last"]:
                g.recip(rd[64:65, 0:nq], psOt[64:65, 0:nq], R=[kpo], W=[krd])
                g.mm(psOt[0:64, 512:512 + nq], onesf[64:65, 0:64], rd[64:65, 0:nq], True, True, R=[kon, krd], W=[kpo + "b"])
                g.cp(rdb[0:64, 0:nq], psOt[0:64, 512:512 + nq], R=[kpo + "b"], W=[krdb], eng="act")
                at, kat = aT[h % 2]
                g.tt(at[0:64, 0:nq], psOt[0:64, 0:nq], rdb[0:64, 0:nq], ALU.mult, R=[kpo, krdb], W=[kat])
                g.st(self.MIXT[512 + h * 64:512 + (h + 1) * 64, q0:q0 + nq], at[0:64, 0:nq], R=[kat], W=[f"MIXTa{bi}_{h}"])
        for it in units[0]:
            emitS(it)
        for u_ in range(len(units)):
            if u_ + 1 < len(units):
                for it in units[u_ + 1]:
                    emitS(it)
            for it in units[u_]:
                emitEP(it)

    if self.cfg.get('even_stop') == 3:
        return
    A.reset()
    Wo, kWo = A.alloc("Wo", 8 * 1024, BF16)
    Wov = Wo.rearrange("p (k n) -> p k n", n=1024)
    stg2 = [A.alloc(f"stgo{i}", 1024) for i in range(2)]
    for k in range(0, 8, 2):
        g.ld(Wov[:, k:k + 2, :], self.mix_out_w[ei, k * 128:(k + 2) * 128, :].rearrange("(a p) n -> p a n", p=128), R=[], W=[kWo], q="pool")
    mixT = [A.alloc(f"mixT{i}", 1024, BF16) for i in range(2)]

    def d_fn(tt):
        m_, km = mixT[tt % 2]
        m3 = m_.rearrange("p (c t) -> p c t", t=128)
        g.ld(m3, MIXTv[:, :, tt * 128:(tt + 1) * 128], R=["MIXTall"], W=[km])
        for hf in range(2):
            for k in range(8):
                g.mm(self.ps[0][:, hf * 512:(hf + 1) * 512], m3[:, k, :], Wov[:, k, hf * 512:(hf + 1) * 512], k == 0, k == 7,
                     R=[km, kWo], W=["ps0"])
        return self.ps[0][:, :], "ps0"
    self.post_stage(li, l, d_fn)


Prog.setup_rope = setup_rope
Prog.even_mixer = even_mixer


def rev(ap, lo, hi):
    return ap[:, lo:hi][:, ::-1]


def s5_mixer(self, li, l):
    g = self
    kb = self.kb
    A = self.arena
    oi = self.od_idx[l]
    if not hasattr(self, "UT"):
        self.UT = g.dram_tmp("UT", [D, T], BF16)
        self.ZT = g.dram_tmp("ZT", [D, T], BF16)
    UTv = self.UT.rearrange("(c p) t -> p c t", p=128)
    ZTv = self.ZT.rearrange("(c p) t -> p c t", p=128)
    ce = ["pool", "act", "dve"]
    A.reset()
    A1 = [A.alloc(f"A1_{c}", D) for c in range(2)]
    B1 = [A.alloc(f"B1_{c}", D) for c in range(2)]
    ng, kng = A.alloc("ng", D)
    g.load_bcast(self.norm_g[li, 0], ng, kng)
    for c in range(2):
        g.load_mod(li, 1, c, *A1[c])
        g.load_mod(li, 0, c, *B1[c])
        g.stt(A1[c][0], A1[c][0], 1.0, ng, ALU.add, ALU.mult, R=[A1[c][1], kng], W=[A1[c][1]])
    xt = [A.alloc(f"xt{i}", D) for i in range(2)]
    tmp, ktmp = A.alloc("tmp", D)
    hb, khb = A.alloc("hb", D, BF16)
    hT = [A.alloc(f"hT{i}", D, BF16) for i in range(2)]
    st_, kst = A.alloc("stat", 8)
    psT = self.ps[3].bitcast(BF16)
    for tt in range(NT):
        c = 1 if tt < 2 else 0
        b = tt % 2
        x, kx = xt[b]
        g.ld(x, self.X[tt * 128:(tt + 1) * 128, :], R=[f"X{tt}"], W=[kx])
        ssq = st_[:, 0:1]
        g.act(tmp, x, AF.Square, R=[kx], W=[ktmp, kst], accum_out=ssq)
        g.rsqrt_small(ssq, ssq, 1.0 / D, None, R=[kst], W=[kst])
        g.stt(tmp, x, ssq, A1[c][0], ALU.mult, ALU.mult, R=[kx, kst, A1[c][1]], W=[ktmp])
        g.tt(hb, tmp, B1[c][0], ALU.add, R=[ktmp, B1[c][1]], W=[khb])
        for k in range(8):
            g.tr(psT[:, k * 128:(k + 1) * 128], hb[:, k * 128:(k + 1) * 128], self.ident_b[:], R=[khb, "ident_b"], W=["ps3"])
        h_, kh = hT[b]
        g.cp(h_, psT[:, 0:1024], R=["ps3"], W=[kh], eng="act")
        g.st(UTv[:, :, tt * 128:(tt + 1) * 128], h_.rearrange("p (c t) -> p c t", t=128), R=[kh], W=[f"UT{tt}"])

    A.reset()
    BT, kBT = A.alloc("BT", 64 * 128, BF16)
    BTv = BT.rearrange("p (a s) -> p a s", s=128)
    CX, kCX = A.alloc("CX", 6 * 32 * 64, BF16)
    CXv = CX.rearrange("p (a g c) -> p a g c", g=32, c=64)
    rho = [A.alloc(f"rho{d}", 32) for d in range(2)]
    rph = [A.alloc(f"rph{d}", 32) for d in range(2)]
    CN = [[A.alloc(f"cn{d}_{n}", 32) for n in range(2)] for d in range(2)]
    SN = [[A.alloc(f"sn{d}_{n}", 32) for n in range(2)] for d in range(2)]
    NSN = [[A.alloc(f"nsn{d}_{n}", 32) for n in range(2)] for d in range(2)]
    dcol, kdcol = A.alloc("dcol", 8)
    g.ld(dcol, self.s5_d[oi], R=[], W=[kdcol])
    hp, khp = A.alloc("halfpi", 1)
    g.memset(hp, math.pi / 2.0, W=[khp])
    mark = A.off
    g.memset(CX, 0.0, W=[kCX])
    brt, kbr = A.alloc("brt", 512)
    bit, kbi = A.alloc("bit", 512)
    g.ld(brt.rearrange("p (g j) -> p g j", j=16), self.s5_b_re[oi].rearrange("(gp two) p j -> (two p) gp j", two=2), R=[], W=[kbr])
    g.ld(bit.rearrange("p (g j) -> p g j", j=16), self.s5_b_im[oi].rearrange("(gp two) p j -> (two p) gp j", two=2), R=[], W=[kbi])
    br3 = brt.rearrange("p (g j) -> p g j", j=16)
    bi3 = bit.rearrange("p (g j) -> p g j", j=16)
    pt = {}
    for nm in ("are", "aim", "ldt", "lre", "dt", "mag", "ang", "r", "r2", "sn", "cs", "bre", "bim", "den", "nr", "t1", "t2", "kre", "kim"):
        pt[nm] = A.alloc("pp_" + nm, 32)
    ri, kri = A.alloc("pp_ri", 32, I32)
    bbr, kbbr = A.alloc("bbr", 512)
    bbi, kbbi = A.alloc("bbi", 512)
    tb1, ktb1 = A.alloc("tb1", 512)
    tb2, ktb2 = A.alloc("tb2", 512)
    MX, kMX = A.alloc("MX", 32 * 32, BF16)
    crt, kcr = A.alloc("crt", 512)
    cit, kci = A.alloc("cit", 512)
    psT = self.ps[3].bitcast(BF16)
    P_ = lambda n: pt[n][0]
    Kk = lambda n: pt[n][1]

    def T2(out, a, b, op):
        g.tt(P_(out), P_(a), P_(b), op, R=[Kk(a), Kk(b)], W=[Kk(out)])
    for d in range(2):
        for j, nm in enumerate(("are", "aim", "ldt")):
            g.ld(P_(nm), self.s5p[oi, d, j], R=[], W=[Kk(nm)])
        g.ts(P_("lre"), P_("are"), -1e-4, None, ALU.min, None, R=[Kk("are")], W=[Kk("lre")])
        g.act(P_("dt"), P_("ldt"), AF.Exp, R=[Kk("ldt")], W=[Kk("dt")])
        T2("mag", "lre", "dt", ALU.mult)
        g.act(P_("mag"), P_("mag"), AF.Exp, R=[Kk("mag")], W=[Kk("mag")])
        T2("ang", "aim", "dt", ALU.mult)
        g.ts(P_("r"), P_("ang"), 1.0 / TWO_PI, None, ALU.mult, None, R=[Kk("ang")], W=[Kk("r")])
        g.cp(ri, P_("r"), R=[Kk("r")], W=[kri])
        g.tt(P_("r"), P_("r"), ri, ALU.subtract, R=[Kk("r"), kri], W=[Kk("r")])
        g.act(P_("sn"), P_("r"), AF.Sin, R=[Kk("r")], W=[Kk("sn")], scale=TWO_PI)
        g.act(P_("r2"), P_("r"), AF.Abs, R=[Kk("r")], W=[Kk("r2")])
        g.act(P_("cs"), P_("r2"), AF.Sin, R=[Kk("r2"), khp], W=[Kk("cs")], scale=-TWO_PI, bias=hp)
        T2("bre", "mag", "cs", ALU.mult)
        T2("bim", "mag", "sn", ALU.mult)
        T2("den", "lre", "lre", ALU.mult)
        T2("t1", "aim", "aim", ALU.mult)
        T2("den", "den", "t1", ALU.add)
        g.recip(P_("den"), P_("den"), R=[Kk("den")], W=[Kk("den")])
        g.ts(P_("nr"), P_("bre"), -1.0, None, ALU.add, None, R=[Kk("bre")], W=[Kk("nr")])
        T2("t1", "nr", "lre", ALU.mult)
        T2("t2", "bim", "aim", ALU.mult)
        T2("kre", "t1", "t2", ALU.add)
        T2("kre", "kre", "den", ALU.mult)
        T2("t1", "bim", "lre", ALU.mult)
        T2("t2", "nr", "aim", ALU.mult)
        T2("kim", "t1", "t2", ALU.subtract)
        T2("kim", "kim", "den", ALU.mult)
        g.cp(rho[d][0], P_("mag"), R=[Kk("mag")], W=[rho[d][1]])
        g.cp(rph[d][0], P_("r"), R=[Kk("r")], W=[rph[d][1]])
        for ni, nn in enumerate((256, 512)):
            g.ts(P_("t1"), P_("r"), float(nn), None, ALU.mult, None, R=[Kk("r")], W=[Kk("t1")])
            g.cp(ri, P_("t1"), R=[Kk("t1")], W=[kri])
            g.tt(P_("t1"), P_("t1"), ri, ALU.subtract, R=[Kk("t1"), kri], W=[Kk("t1")])
            g.act(SN[d][ni][0], P_("t1"), AF.Sin, R=[Kk("t1")], W=[SN[d][ni][1]], scale=TWO_PI)
            g.act(P_("t2"), P_("t1"), AF.Abs, R=[Kk("t1")], W=[Kk("t2")])
            g.act(CN[d][ni][0], P_("t2"), AF.Sin, R=[Kk("t2"), khp], W=[CN[d][ni][1]], scale=-TWO_PI, bias=hp)
            g.ts(NSN[d][ni][0], SN[d][ni][0], -1.0, None, ALU.mult, None, R=[SN[d][ni][1]], W=[NSN[d][ni][1]])
        bb3r = bbr.rearrange("p (g j) -> p g j", j=16)
        bb3i = bbi.rearrange("p (g j) -> p g j", j=16)
        t13 = tb1.rearrange("p (g j) -> p g j", j=16)
        t23 = tb2.rearrange("p (g j) -> p g j", j=16)
        g.tt(t13, br3, bc(P_("kre"), 2, 16), ALU.mult, R=[kbr, Kk("kre")], W=[ktb1])
        g.tt(t23, bi3, bc(P_("kim"), 2, 16), ALU.mult, R=[kbi, Kk("kim")], W=[ktb2])
        g.tt(bbr, tb1, tb2, ALU.subtract, R=[ktb1, ktb2], W=[kbbr])
        g.tt(t13, bi3, bc(P_("kre"), 2, 16), ALU.mult, R=[kbi, Kk("kre")], W=[ktb1])
        g.tt(t23, br3, bc(P_("kim"), 2, 16), ALU.mult, R=[kbr, Kk("kim")], W=[ktb2])
        g.tt(bbi, tb1, tb2, ALU.add, R=[ktb1, ktb2], W=[kbbi])
        for part, (bsrc, kbs) in enumerate(((bbr, kbbr), (bbi, kbbi))):
            b4 = bsrc.rearrange("p (g two j) -> p g two j", two=2, j=16)
            for par in range(2):
                g.memset(MX, 0.0, W=[kMX])
                MX4 = MX.rearrange("p (g two c) -> p g two c", two=2, c=32)
                g.cp(MX4[0:64, :, par, 0:16], b4[0:64, :, par, :], R=[kbs], W=[kMX])
                g.cp(MX4[64:128, :, par, 16:32], b4[64:128, :, par, :], R=[kbs], W=[kMX])
                for q in range(8):
                    g.tr(psT[:, q * 128:(q + 1) * 128], MX[:, q * 128:(q + 1) * 128], self.ident_b[:], R=[kMX, "ident_b"], W=["ps3"])
                a0 = ((d * 2 + part) * 2 + par) * 8
                g.cp(BT[:, a0 * 128:(a0 + 8) * 128], psT[:, 0:1024], R=["ps3"], W=[kBT], eng="act")
        g.ld(crt.rearrange("p (g k) -> p g k", k=16), self.s5_c_re[oi, d].rearrange("(gp two) p k -> (two p) gp k", two=2), R=[], W=[kcr])
        g.ld(cit.rearrange("p (g k) -> p g k", k=16), self.s5_c_im[oi, d].rearrange("(gp two) p k -> (two p) gp k", two=2), R=[], W=[kci])
        for part, (csrc, kcs, sgn) in enumerate(((crt, kcr, 1.0), (cit, kci, -1.0), (crt, kcr, -1.0))):
            c4 = csrc.rearrange("p (g two k) -> p g two k", two=2, k=16)
            Cv = CXv[:, d * 3 + part].rearrange("p (g two) c -> p g two c", two=2)
            for gpar in range(2):
                g.ts(Cv[0:64, :, gpar, gpar * 32:gpar * 32 + 16], c4[0:64, :, gpar, :], sgn, None, ALU.mult, None, R=[kcs], W=[kCX])
                g.ts(Cv[64:128, :, gpar, gpar * 32 + 16:gpar * 32 + 32], c4[64:128, :, gpar, :], sgn, None, ALU.mult, None, R=[kcs], W=[kCX])
    if self.cfg.get("s5_stop") == 1:
        return
    kb.barrier()
    A.off = mark
    iot, kio = A.alloc("iota", T)
    kb.op("pool", lambda e: e.iota(iot, pattern=[[1, T]], base=0, channel_multiplier=0, allow_small_or_imprecise_dtypes=True), W=[kio])
    uT = [A.alloc("uT0", T, BF16)] * 2
    ysb, kys = A.alloc("ysb", T)
    NB = 512
    TI, kti = A.alloc("TI", NB, I32)
    TFt = [A.alloc(f"TF{i}", NB) for i in range(2)]
    ST = [A.alloc(f"ST{i}", NB) for i in range(2)]
    CT = [A.alloc(f"CT{i}", NB) for i in range(2)]
    W1 = [A.alloc(f"w1_{i}", NB, BF16) for i in range(2)]
    W2 = [A.alloc(f"w2_{i}", NB, BF16) for i in range(2)]
    W3 = [A.alloc(f"w3_{i}", NB, BF16) for i in range(2)]
    W4 = [A.alloc(f"w4_{i}", NB, BF16) for i in range(2)]
    BTR = [A.alloc(f"btr{i}", NB, BF16) for i in range(2)]
    BTI = [A.alloc(f"bti{i}", NB, BF16) for i in range(2)]
    WR = [A.alloc(f"wr{i}", NB) for i in range(2)]
    WI = [A.alloc(f"wi{i}", NB) for i in range(2)]
    P1 = [A.alloc(f"p1_{i}", NB, BF16) for i in range(2)]
    P2 = [A.alloc(f"p2_{i}", NB, BF16) for i in range(2)]
    P3 = [A.alloc(f"p3_{i}", NB, BF16) for i in range(2)]
    P4 = [A.alloc(f"p4_{i}", NB, BF16) for i in range(2)]
    YE = [A.alloc(f"ye{i}", NB) for i in range(2)]
    CAR = [A.alloc(f"carry{i}", 4) for i in range(2)]
    zt, kzt = uT[0]
    lat = [(NCTX + i * NB, NCTX + (i + 1) * NB) for i in range(NLAT // NB)]
    fblocks = [(0, NCTX, False)] + [(lo, hi, False) for (lo, hi) in lat]
    bblocks = [(0, NCTX, True)] + [(lo, hi, True) for (lo, hi) in reversed(lat)]
    gcount = [0]
    for q in range(8):
        u_, ku = uT[q % 2]
        g.ld(u_, self.UT[q * 128:(q + 1) * 128, :], R=["UTall"], W=[ku])
        g.ts(ysb, u_, dcol[:, q:q + 1], None, ALU.mult, None, R=[ku, kdcol], W=[kys])
        blist = []
        for gl in range(4):
            for d in range(2):
                gi = gcount[0]
                gcount[0] += 1
                for bidx, (lo, hi, rv) in enumerate(fblocks if d == 0 else bblocks):
                    blist.append(dict(gl=gl, d=d, gi=gi, bidx=bidx, lo=lo, hi=hi, rv=rv, i=len(blist)))
        for i_, bl in enumerate(blist):
            bl["prev"] = blist[i_ - 1] if bl["bidx"] > 0 else None

        def tokf(bl):
            lo, hi = bl["lo"], bl["hi"]
            return (lambda ap: rev(ap, lo, hi)) if bl["rv"] else (lambda ap: ap[:, lo:hi])

        def stageA(bl):
            gl, d, gi, b = bl["gl"], bl["d"], bl["gi"], bl["i"] % 2
            gp = 4 * q + gl
            half, par = gl // 2, gl % 2
            rows = slice(64 * half, 64 * half + 64)
            n = bl["hi"] - bl["lo"]
            tb = gi % 2
            st, kst2 = ST[tb]
            ct, kct = CT[tb]
            if bl["bidx"] == 0:
                tf, ktf = TFt[tb]
                rcol = rph[d][0][:, gp:gp + 1]
                g.ts(TI, iot[:, 0:NB], rcol, None, ALU.mult, None, R=[kio, rph[d][1]], W=[kti])
                g.stt(tf, iot[:, 0:NB], rcol, TI, ALU.mult, ALU.subtract, R=[kio, rph[d][1], kti], W=[ktf])
                g.act(st, tf, AF.Sin, R=[ktf], W=[kst2], scale=TWO_PI)
                g.act(tf, tf, AF.Abs, R=[ktf], W=[ktf])
                g.act(ct, tf, AF.Sin, R=[ktf, khp], W=[kct], scale=-TWO_PI, bias=hp)
            psBr = self.ps[0][:, b * 512:b * 512 + 512]
            psBi = self.ps[1][:, b * 512:b * 512 + 512]
            k0, k1 = f"ps0.{b}", f"ps1.{b}"
            tok = tokf(bl)
            for part, (pp, kpp) in enumerate(((psBr, k0), (psBi, k1))):
                a0 = ((d * 2 + part) * 2 + par) * 8 + q
                g.mm(pp[:, 0:n], BTv[rows, a0, :], tok(u_[rows, :]), True, True, R=[kBT, ku], W=[kpp])
            brs, kbrs = psBr, k0
            bis, kbis = psBi, k1
            w1, kw1 = W1[b]
            w2, kw2 = W2[b]
            w3, kw3 = W3[b]
            w4, kw4 = W4[b]
            btr, kbtr = BTR[b]
            bti, kbti = BTI[b]
            g.tt(w1[:, 0:n], brs[:, 0:n], ct[:, 0:n], ALU.mult, R=[kbrs, kct], W=[kw1])
            g.tt(w2[:, 0:n], bis[:, 0:n], st[:, 0:n], ALU.mult, R=[kbis, kst2], W=[kw2])
            g.tt(btr[:, 0:n], w1[:, 0:n], w2[:, 0:n], ALU.add, R=[kw1, kw2], W=[kbtr], eng="pool")
            g.tt(w3[:, 0:n], bis[:, 0:n], ct[:, 0:n], ALU.mult, R=[kbis, kct], W=[kw3])
            g.tt(w4[:, 0:n], brs[:, 0:n], st[:, 0:n], ALU.mult, R=[kbrs, kst2], W=[kw4])
            g.tt(bti[:, 0:n], w3[:, 0:n], w4[:, 0:n], ALU.subtract, R=[kw3, kw4], W=[kbti], eng="pool")

        def stageB(bl):
            gl, d, gi, b = bl["gl"], bl["d"], bl["gi"], bl["i"] % 2
            gp = 4 * q + gl
            half = gl // 2
            rows = slice(64 * half, 64 * half + 64)
            n = bl["hi"] - bl["lo"]
            tb = gi % 2
            st, kst2 = ST[tb]
            ct, kct = CT[tb]
            btr, kbtr = BTR[b]
            bti, kbti = BTI[b]
            wr, kwr = WR[b]
            wi, kwi = WI[b]
            car, kcar = CAR[b]
            pv = bl["prev"]
            if pv is None:
                g.cp(self.ps[3][:, 0:512], st, R=[kst2], W=["ps3.S"], eng="act")
                g.cp(self.ps[3][:, 512:1024], ct, R=[kct], W=["ps3.C"], eng="act")
                ini_r, ini_i, Rc = 0.0, 0.0, []
            else:
                pb = pv["i"] % 2
                npv = pv["hi"] - pv["lo"]
                ni = 0 if npv == 256 else 1
                wrl = WR[pb][0][:, npv - 1:npv]
                wil = WI[pb][0][:, npv - 1:npv]
                cn = CN[d][ni][0][:, gp:gp + 1]
                sn = SN[d][ni][0][:, gp:gp + 1]
                nsn = NSN[d][ni][0][:, gp:gp + 1]
                Rk = [WR[pb][1], WI[pb][1], CN[d][ni][1], SN[d][ni][1], NSN[d][ni][1]]
                g.act(car[:, 2:3], wil, AF.Copy, R=Rk, W=[kcar], scale=nsn)
                g.act(car[:, 3:4], wil, AF.Copy, R=Rk + [kcar], W=[kcar], scale=cn)
                g.act(car[:, 0:1], wrl, AF.Identity, R=Rk + [kcar], W=[kcar], scale=cn, bias=car[:, 2:3])
                g.act(car[:, 1:2], wrl, AF.Identity, R=Rk + [kcar], W=[kcar], scale=sn, bias=car[:, 3:4])
                ini_r, ini_i, Rc = car[:, 0:1], car[:, 1:2], [kcar]
            rb = rho[d][0][:, gp:gp + 1].to_broadcast([128, n])
            kb.op("dve", lambda e: e.tensor_tensor_scan(out=wr[:, 0:n], data0=rb, data1=btr[:, 0:n], initial=ini_r,
                                                        op0=ALU.mult, op1=ALU.add), R=[rho[d][1], kbtr] + Rc, W=[kwr])
            kb.op("dve", lambda e: e.tensor_tensor_scan(out=wi[:, 0:n], data0=rb, data1=bti[:, 0:n], initial=ini_i,
                                                        op0=ALU.mult, op1=ALU.add), R=[rho[d][1], kbti] + Rc, W=[kwi])
            p1, kp1 = P1[b]
            p2, kp2 = P2[b]
            p3, kp3 = P3[b]
            p4, kp4 = P4[b]
            pS, pC = self.ps[3][:, 0:512], self.ps[3][:, 512:1024]
            g.tt(p1[:, 0:n], wr[:, 0:n], pC[:, 0:n], ALU.mult, R=[kwr, "ps3.C"], W=[kp1])
            g.tt(p2[:, 0:n], wi[:, 0:n], pS[:, 0:n], ALU.mult, R=[kwi, "ps3.S"], W=[kp2])
            g.tt(p3[:, 0:n], wr[:, 0:n], st[:, 0:n], ALU.mult, R=[kwr, kst2], W=[kp3], eng="pool")
            g.tt(p4[:, 0:n], wi[:, 0:n], ct[:, 0:n], ALU.mult, R=[kwi, kct], W=[kp4], eng="pool")
            psY = self.ps[2][:, b * 512:b * 512 + 512]
            k2 = f"ps2.{b}"
            g.mm(psY[rows, 0:n], CXv[:, d * 3 + 0, gp, :], p1[:, 0:n], True, False, R=[kCX, kp1], W=[k2])
            g.mm(psY[rows, 0:n], CXv[:, d * 3 + 2, gp, :], p2[:, 0:n], False, False, R=[kCX, kp2], W=[k2])
            g.mm(psY[rows, 0:n], CXv[:, d * 3 + 1, gp, :], p3[:, 0:n], False, False, R=[kCX, kp3], W=[k2])
            g.mm(psY[rows, 0:n], CXv[:, d * 3 + 1, gp, :], p4[:, 0:n], False, True, R=[kCX, kp4], W=[k2])

        def stageC(bl):
            gl, b = bl["gl"], bl["i"] % 2
            half = gl // 2
            rows = slice(64 * half, 64 * half + 64)
            n = bl["hi"] - bl["lo"]
            psY = self.ps[2][:, b * 512:b * 512 + 512]
            k2 = f"ps2.{b}"
            tok = tokf(bl)
            g.tt(tok(ysb[rows, :]), tok(ysb[rows, :]), psY[rows, 0:n], ALU.add, R=[kys, k2], W=[kys])

        stageA(blist[0])
        for i_ in range(len(blist)):
            if i_ + 1 < len(blist):
                stageA(blist[i_ + 1])
            stageB(blist[i_])
            if i_ >= 1:
                stageC(blist[i_ - 1])
        stageC(blist[-1])
        g.act(iot, ysb, AF.Square, R=[kys], W=[kio + "g"])
        g.ts(iot, iot, 0.044715, 1.0, ALU.mult, ALU.add, R=[kio + "g"], W=[kio + "g"])
        g.tt(iot, iot, ysb, ALU.mult, R=[kio + "g", kys], W=[kio + "g"])
        g.act(iot, iot, AF.Tanh, R=[kio + "g"], W=[kio + "g"], scale=math.sqrt(2.0 / math.pi))
        g.stt(iot, iot, 1.0, ysb, ALU.add, ALU.mult, R=[kio + "g", kys], W=[kio + "g"])
        g.act(zt, iot, AF.Copy, R=[kio + "g"], W=[kzt], scale=0.5)
        g.st(self.ZT[q * 128:(q + 1) * 128, :], zt, R=[kzt], W=[f"ZT{q}"])
        if q < 7:
            kb.op("pool", lambda e: e.iota(iot, pattern=[[1, T]], base=0, channel_multiplier=0, allow_small_or_imprecise_dtypes=True),
                  R=[kio + "g"], W=[kio, kio + "g"])
    if self.cfg.get("s5_stop") == 2:
        return
    A.reset()
    GW, kGW = A.alloc("GW", 8 * 2048, BF16)
    GWv = GW.rearrange("p (k n) -> p k n", n=2048)
    stg = [A.alloc(f"stgg{i}", 1024) for i in range(2)]
    for k in range(8):
        g.ld(GWv[:, k, :], self.s5_glu_w[oi, k * 128:(k + 1) * 128, :], R=[], W=[kGW], q="pool")
    gb, kgb = A.alloc("gb", 2048)
    g.load_bcast(self.s5_glu_b[oi], gb, kgb)
    zT = [A.alloc(f"zT{i}", 1024, BF16) for i in range(2)]
    asb, kasb = A.alloc("asb", 1024)
    gsb, kgsb = A.alloc("gsb", 1024)
    dt_, kdt = A.alloc("dtile", 1024)

    def d_fn(tt):
        z_, kz = zT[tt % 2]
        z3 = z_.rearrange("p (c t) -> p c t", t=128)
        g.ld(z3, ZTv[:, :, tt * 128:(tt + 1) * 128], R=["ZTall"], W=[kz])
        for ch in range(4):
            pp, kpp = (self.ps[0], "ps0") if ch < 2 else (self.ps[3], "ps3")
            for k in range(8):
                g.mm(pp[:, (ch % 2) * 512:(ch % 2 + 1) * 512], z3[:, k, :], GWv[:, k, ch * 512:(ch + 1) * 512], k == 0, k == 7,
                     R=[kz, kGW], W=[kpp])
        g.tt(asb, self.ps[0][:, :], gb[:, 0:1024], ALU.add, R=["ps0", kgb], W=[kasb])
        g.tt(gsb, self.ps[3][:, :], gb[:, 1024:2048], ALU.add, R=["ps3", kgb], W=[kgsb])
        g.act(gsb, gsb, AF.Sigmoid, R=[kgsb], W=[kgsb])
        g.tt(dt_, asb, gsb, ALU.mult, R=[kasb, kgsb], W=[kdt], eng="pool")
        return dt_, kdt
    self.post_stage(li, l, d_fn)


Prog.s5_mixer = s5_mixer


def shared_weights(inp, layers):
    ev = [l // 2 for l in layers if l % 2 == 0]
    od = [l // 2 for l in layers if l % 2 == 1]
    f = lambda a: np.ascontiguousarray(np.asarray(a, dtype=np.float32))
    w = {}
    w["mod_w"] = f(inp["mod_w"][layers])
    w["mod_b"] = f(inp["mod_b"][layers])
    w["norm_g"] = f(inp["norm_g"][layers])
    w["moe_router_w"] = f(inp["moe_router_w"][layers])
    w["moe_w1"] = f(inp["moe_w1"][layers])
    w["moe_w3"] = f(inp["moe_w3"][layers])
    w["moe_w2"] = f(inp["moe_w2"][layers])
    w["final_norm_g"] = f(inp["final_norm_g"])
    if ev:
        w["mix_in_w"] = f(inp["mix_in_w"][ev])
        w["mix_out_w"] = f(inp["mix_out_w"][ev])
        w["ret_log_rate"] = f(np.asarray(inp["ret_log_rate"])[ev].reshape(len(ev), 16))
        w["ret_gn_g"] = f(inp["ret_gn_g"][ev])
        w["qk_norm_g"] = f(np.asarray(inp["qk_norm_g"])[ev].reshape(len(ev), 128))
    else:
        w["mix_in_w"] = np.zeros((1, D, 2816), np.float32)
        w["mix_out_w"] = np.zeros((1, D, D), np.float32)
        w["ret_log_rate"] = np.zeros((1, 16), np.float32)
        w["ret_gn_g"] = np.zeros((1, 512), np.float32)
        w["qk_norm_g"] = np.zeros((1, 128), np.float32)
    if od:
        no = len(od)
        a_re = np.asarray(inp["s5_a_re"])[od]
        a_im = np.asarray(inp["s5_a_im"])[od]
        ldt = np.asarray(inp["s5_log_dt"])[od]
        pair = lambda a: a.reshape(no, 2, 32, 2, 64).transpose(0, 1, 3, 4, 2).reshape(no, 2, 128, 32)
        ldt_b = np.broadcast_to(ldt[..., None], (no, 2, 64, 64))
        w["s5p"] = f(np.stack([pair(a_re), pair(a_im), pair(ldt_b)], axis=2))
        w["s5_b_re"] = f(inp["s5_b_re"][od])
        w["s5_b_im"] = f(inp["s5_b_im"][od])
        w["s5_c_re"] = f(np.asarray(inp["s5_c_re"])[od].transpose(0, 1, 2, 4, 3))
        w["s5_c_im"] = f(np.asarray(inp["s5_c_im"])[od].transpose(0, 1, 2, 4, 3))
        w["s5_d"] = f(np.asarray(inp["s5_d"])[od].reshape(no, 8, 128).transpose(0, 2, 1))
        w["s5_glu_w"] = f(inp["s5_glu_w"][od])
        w["s5_glu_b"] = f(inp["s5_glu_b"][od])
    else:
        w["s5p"] = np.zeros((1, 2, 3, 128, 32), np.float32)
        w["s5_b_re"] = np.zeros((1, 64, 64, 16), np.float32)
        w["s5_b_im"] = np.zeros((1, 64, 64, 16), np.float32)
        w["s5_c_re"] = np.zeros((1, 2, 64, 64, 16), np.float32)
        w["s5_c_im"] = np.zeros((1, 2, 64, 64, 16), np.float32)
        w["s5_d"] = np.zeros((1, 128, 8), np.float32)
        w["s5_glu_w"] = np.zeros((1, D, 2 * D), np.float32)
        w["s5_glu_b"] = np.zeros((1, 2 * D), np.float32)
    return w


def core_inputs(inp, b, w):
    m = dict(w)
    x = np.asarray(inp["x"][b], dtype=np.float32)
    ctx = np.asarray(inp["ctx"][b], dtype=np.float32)
    m["xin"] = np.ascontiguousarray(np.concatenate([ctx, x], axis=0))
    c = np.asarray(inp["c"][b], dtype=np.float32).reshape(8, 128).T
    cc = np.asarray(inp["c_ctx"], dtype=np.float32).reshape(8, 128).T
    m["cin"] = np.ascontiguousarray(np.stack([c, cc], axis=2).reshape(128, 16))
    return m


def kernel(**inputs):
    layers = [0, 1, 2, 3]
    prog = Prog(dict(layers=layers))
    nc = prog.build()
    w = shared_weights(inputs, layers)
    in_maps = [core_inputs(inputs, b, w) for b in range(8)]
    res = run_bass_kernel_spmd(nc, in_maps, core_ids=list(range(8)))
    return np.stack([np.asarray(r["out"], dtype=np.float32) for r in res.results], axis=0)
```
